# Optimizing a Trainium2 kernel written in Bass

```python
import jax, jax.numpy as jnp
from jax import lax
import numpy as np


D_MODEL = 2048
BATCH = 2
SEQ = 8192
DEPTH = 1

D_MIX = D_MODEL
REC_HEADS = 8
REC_DK = 128
REC_DV = 128
REC_WIDTH = REC_HEADS * REC_DV
REC_CHUNK = 64
ATT_HEADS = 8
ATT_DH = 128
ATT_KV_HEADS = 2
ATT_GROUP = ATT_HEADS // ATT_KV_HEADS
ATT_WIDTH = ATT_HEADS * ATT_DH
IDX_HEADS = 8
IDX_DIM = 64
TOPK_MAX = 256
Q_BLOCK = 128
N_GROUPS = 4
EXPERTS_PER_GROUP = 8
N_EXPERTS = N_GROUPS * EXPERTS_PER_GROUP
TOPK_IN_GROUP = 2
D_EXPERT = 512
EPS = 1e-6

IN_WIDTHS = (REC_HEADS * REC_DK,
             REC_HEADS * REC_DK,
             REC_WIDTH,
             REC_WIDTH,
             ATT_WIDTH,
             ATT_KV_HEADS * ATT_DH,
             ATT_KV_HEADS * ATT_DH,
             IDX_HEADS * IDX_DIM,
             IDX_DIM,
             IDX_HEADS)
IN_COLS = sum(IN_WIDTHS)

kernel_name = 'hymba_hgrn2_dsa_hmoe_adaln'


def _rmsnorm(x, g):
    xf = x.astype(jnp.float32)
    xf = xf * lax.rsqrt(jnp.mean(xf * xf, axis=-1, keepdims=True) + EPS)
    return xf.astype(x.dtype) * g


def _head_rmsnorm(o, g):
    B, S, H, Dh = o.shape
    of = o.astype(jnp.float32)
    of = of * lax.rsqrt(jnp.mean(of * of, axis=-1, keepdims=True) + EPS)
    return of.reshape(B, S, H * Dh) * g.astype(jnp.float32)


def _split_in(proj):
    pieces = []
    off = 0
    for w in IN_WIDTHS:
        pieces.append(proj[..., off:off + w])
        off += w
    return pieces


def _hgrn2_mixer(q, f_logit, inp, gate, lb, g_out):
    B, S, _ = q.shape
    nc = S // REC_CHUNK
    f32 = jnp.float32
    f = lb + (1.0 - lb) * jax.nn.sigmoid(f_logit.astype(f32))
    log_f = jnp.log(f)
    k = 1.0 - f
    qf = jax.nn.silu(q.astype(f32)) * REC_DK ** -0.5

    def chunked(t, d):
        return t.reshape(B, nc, REC_CHUNK, REC_HEADS, d).transpose(1, 0, 3, 2, 4)

    qc = chunked(qf, REC_DK)
    kc = chunked(k, REC_DK)
    vc = chunked(inp.astype(f32), REC_DV)
    bc = jnp.cumsum(chunked(log_f, REC_DK), axis=3)
    causal = jnp.tril(jnp.ones((REC_CHUNK, REC_CHUNK), dtype=bool))[:, :, None]

    def step(state, xs):
        q_, k_, v_, b_ = xs
        inter = jnp.einsum('bhtd,bhdv->bhtv', q_ * jnp.exp(b_), state)
        rel = jnp.where(causal, b_[:, :, :, None, :] - b_[:, :, None, :, :], -jnp.inf)
        scores = jnp.einsum('bhtd,bhtsd,bhsd->bhts', q_, jnp.exp(rel), k_)
        intra = jnp.einsum('bhts,bhsv->bhtv', scores, v_)
        b_end = b_[:, :, -1:, :]
        state = (jnp.exp(b_end[:, :, 0, :])[..., None] * state
                 + jnp.einsum('bhsd,bhsv->bhdv', k_ * jnp.exp(b_end - b_), v_))
        return state, inter + intra

    s0 = jnp.zeros((B, REC_HEADS, REC_DK, REC_DV), f32)
    _, o = lax.scan(step, s0, (qc, kc, vc, bc))
    o = o.transpose(1, 0, 3, 2, 4).reshape(B, S, REC_HEADS, REC_DV)
    o = _head_rmsnorm(o, g_out) * jax.nn.silu(gate.astype(f32))
    return o.astype(q.dtype)


def _dsa_mixer(q, k, v, q_idx, k_idx, w_idx, g_out):
    B, S, _ = q.shape
    f32 = jnp.float32
    k_sel = min(TOPK_MAX, S // 4)
    nb = S // Q_BLOCK
    qb_all = q.reshape(B, nb, Q_BLOCK, ATT_KV_HEADS, ATT_GROUP, ATT_DH)
    kh = k.reshape(B, S, ATT_KV_HEADS, ATT_DH)
    vh = v.reshape(B, S, ATT_KV_HEADS, ATT_DH)
    qi_all = q_idx.reshape(B, nb, Q_BLOCK, IDX_HEADS, IDX_DIM)
    wi_all = w_idx.reshape(B, nb, Q_BLOCK, IDX_HEADS)
    k_idx_f = k_idx.astype(f32)
    key_pos = jnp.arange(S)
    gather = jax.vmap(lambda kv, ix: kv[ix])

    def block(args):
        qb, qib, wb, blk = args
        t = blk * Q_BLOCK + jnp.arange(Q_BLOCK)
        causal = key_pos[None, :] <= t[:, None]
        rel = jax.nn.relu(jnp.einsum('bqhd,bsd->bqhs', qib.astype(f32), k_idx_f) * IDX_DIM ** -0.5)
        score = jnp.einsum('bqhs,bqh->bqs', rel, wb.astype(f32) * IDX_HEADS ** -0.5)
        score = jnp.where(causal[None], score, -jnp.inf)
        _, idx = lax.top_k(score, k_sel)
        valid = idx <= t[None, :, None]
        kg = gather(kh, idx)
        vg = gather(vh, idx)
        logits = jnp.einsum('bqhgd,bqnhd->bqhgn', qb, kg).astype(f32) * ATT_DH ** -0.5
        logits = jnp.where(valid[:, :, None, None, :], logits, -jnp.inf)
        p = jax.nn.softmax(logits, axis=-1).astype(v.dtype)
        return jnp.einsum('bqhgn,bqnhd->bqhgd', p, vg)

    xs = (jnp.moveaxis(qb_all, 1, 0), jnp.moveaxis(qi_all, 1, 0),
          jnp.moveaxis(wi_all, 1, 0), jnp.arange(nb))
    out = lax.map(block, xs)
    out = jnp.moveaxis(out, 0, 1).reshape(B, S, ATT_HEADS, ATT_DH)
    return _head_rmsnorm(out, g_out).astype(q.dtype)


def _hier_moe(h, w_rg, b_rg, w_re, b_re, w_gate, w_up, w_down):
    B, S, D = h.shape
    f32 = jnp.float32
    xt = h.reshape(B * S, D)
    n = xt.shape[0]
    g_prob = jax.nn.softmax((xt @ w_rg + b_rg).astype(f32), axis=-1)
    p_g, g_idx = lax.top_k(g_prob, 1)
    e_logits = (xt @ w_re + b_re).astype(f32).reshape(n, N_GROUPS, EXPERTS_PER_GROUP)
    e_in_group = jnp.take_along_axis(e_logits, g_idx[:, :, None], axis=1)[:, 0]
    top_v, top_i = lax.top_k(e_in_group, TOPK_IN_GROUP)
    w_sel = jax.nn.softmax(top_v, axis=-1) * p_g
    expert_id = g_idx * EXPERTS_PER_GROUP + top_i
    combine = jnp.sum(jax.nn.one_hot(expert_id, N_EXPERTS, dtype=f32) * w_sel[..., None], axis=1)
    combine = combine.astype(xt.dtype)
    y = jnp.zeros_like(xt)
    for e in range(N_EXPERTS):
        he = jax.nn.silu(xt @ w_gate[e]) * (xt @ w_up[e])
        y = y + combine[:, e:e + 1] * (he @ w_down[e])
    return y.reshape(B, S, D)


def setup_inputs(seed: int = 0) -> dict:
    key = jax.random.key(seed)
    ks = jax.random.split(key, 20)
    f32 = jnp.float32
    nrm = lambda k, shape, s: jax.random.normal(k, shape, f32) * s
    return {
        'x': nrm(ks[0], (BATCH, SEQ, D_MODEL), 1.0),
        'c': nrm(ks[1], (BATCH, D_MODEL), 1.0),
        'w_ada': nrm(ks[2], (DEPTH, D_MODEL, 6 * D_MODEL), 0.5 * D_MODEL ** -0.5),
        'b_ada': nrm(ks[3], (DEPTH, 6 * D_MODEL), 0.02),
        'g_norm_mix': 1.0 + nrm(ks[4], (DEPTH, D_MODEL), 0.02),
        'w_in': nrm(ks[5], (DEPTH, D_MODEL, IN_COLS), D_MODEL ** -0.5),
        'lb_logits': nrm(ks[6], (DEPTH + 1, REC_HEADS * REC_DK), 0.5),
        'g_rec_out': 1.0 + nrm(ks[7], (DEPTH, REC_WIDTH), 0.02),
        'g_att_out': 1.0 + nrm(ks[8], (DEPTH, ATT_WIDTH), 0.02),
        'w_out': nrm(ks[9], (DEPTH, D_MIX, D_MODEL), D_MIX ** -0.5),
        'g_norm_ffn': 1.0 + nrm(ks[10], (DEPTH, D_MODEL), 0.02),
        'w_router_group': nrm(ks[11], (DEPTH, D_MODEL, N_GROUPS), D_MODEL ** -0.5),
        'b_router_group': nrm(ks[12], (DEPTH, N_GROUPS), 0.01),
        'w_router_expert': nrm(ks[13], (DEPTH, D_MODEL, N_EXPERTS), D_MODEL ** -0.5),
        'b_router_expert': nrm(ks[14], (DEPTH, N_EXPERTS), 0.01),
        'w_expert_gate': nrm(ks[15], (DEPTH, N_EXPERTS, D_MODEL, D_EXPERT), D_MODEL ** -0.5),
        'w_expert_up': nrm(ks[16], (DEPTH, N_EXPERTS, D_MODEL, D_EXPERT), D_MODEL ** -0.5),
        'w_expert_down': nrm(ks[17], (DEPTH, N_EXPERTS, D_EXPERT, D_MODEL), D_EXPERT ** -0.5),
        'g_final': 1.0 + nrm(ks[18], (D_MODEL,), 0.02),
    }


def reference(x, c, w_ada, b_ada, g_norm_mix, w_in, lb_logits, g_rec_out, g_att_out, w_out,
              g_norm_ffn, w_router_group, b_router_group, w_router_expert, b_router_expert,
              w_expert_gate, w_expert_up, w_expert_down, g_final):
    lower_bounds = jnp.cumsum(jax.nn.softmax(lb_logits.astype(jnp.float32), axis=0), axis=0)
    c_act = jax.nn.silu(c)
    for layer in range(DEPTH):
        mod = (c_act @ w_ada[layer] + b_ada[layer])[:, None, :]
        sh1, sc1, gt1, sh2, sc2, gt2 = jnp.split(mod, 6, axis=-1)

        h = _rmsnorm(x, g_norm_mix[layer]) * (1.0 + sc1) + sh1
        (r_q, r_f, r_i, r_g, a_q, a_k, a_v, i_q, i_k, i_w) = _split_in(h @ w_in[layer])
        rec = _hgrn2_mixer(r_q, r_f, r_i, r_g, lower_bounds[layer], g_rec_out[layer])
        att = _dsa_mixer(a_q, a_k, a_v, i_q, i_k, i_w, g_att_out[layer])
        mixed = jnp.concatenate([rec, att], axis=-1) @ w_out[layer]
        x = x + gt1 * mixed

        h = _rmsnorm(x, g_norm_ffn[layer]) * (1.0 + sc2) + sh2
        y = _hier_moe(h, w_router_group[layer], b_router_group[layer], w_router_expert[layer],
                      b_router_expert[layer], w_expert_gate[layer], w_expert_up[layer],
                      w_expert_down[layer])
        x = x + gt2 * y
    return _rmsnorm(x, g_final)
```

```python
import contextlib
import numpy as np
import ml_dtypes
import concourse.bass as bass
import concourse.mybir as mybir
from concourse.bass_utils import run_bass_kernel_spmd

F32 = mybir.dt.float32
BF16 = mybir.dt.bfloat16
AF = mybir.ActivationFunctionType
ALU = mybir.AluOpType
AX = mybir.AxisListType

D = 2048
KC = 16
IN_COLS = 6216
NE = 32
DE = 512
EPS = 1e-6
NEG = -30000.0
C_RQ, C_RF, C_RI, C_RG, C_AQ, C_AK, C_AV, C_IQ, C_IK, C_IW = 0, 1024, 2048, 3072, 4096, 5120, 5376, 5632, 6144, 6208


class Buf:
    __slots__ = ("name", "w", "r", "sem", "cnt")

    def __init__(self, name):
        self.name = name
        self.w = None
        self.r = []
        self.sem = None
        self.cnt = 0


class Prog:
    ENGS = ("pe", "act", "dve", "pool", "sp")

    def __init__(self, nc, es):
        self.nc = nc
        self.es = es
        self.q = {e: [] for e in self.ENGS}
        self.cnt = {e: 0 for e in self.ENGS}
        self.esem = {e: es.enter_context(nc.semaphore("sem_" + e)) for e in ("pe", "act", "dve", "pool")}
        self.seen = {e: {} for e in self.ENGS}
        self.nsem = 0
        self.final = []
        self.dmabufs = []
        self.stopped = False
        self.barrier = []

    def set_barrier(self):
        toks = [(e, self.esem[e], self.cnt[e]) for e in ("pe", "act", "dve", "pool") if self.cnt[e] > 0]
        for b in self.dmabufs:
            toks.append(("d%d" % id(b), b.sem, b.cnt))
        self.barrier = toks

    def mkbuf(self, name):
        b = Buf(name)
        b.r = list(self.barrier)
        return b

    def newsem(self, name):
        self.nsem += 1
        return self.es.enter_context(self.nc.semaphore("d_%s_%d" % (name, self.nsem)))

    def _waits(self, eng, R, W):
        toks = []
        for b in R:
            if b.w is not None:
                toks.append(b.w)
        for b in W:
            if b.w is not None:
                toks.append(b.w)
            toks.extend(b.r)
        out = {}
        for (key, sem, v) in toks:
            if key == eng and eng == "pe":
                continue
            if self.seen[eng].get(key, 0) >= v:
                continue
            if out.get(key, (None, 0))[1] < v:
                out[key] = (sem, v)
        for key, (sem, v) in out.items():
            self.seen[eng][key] = v
        return list(out.values())

    def emit(self, eng, fn, R=(), W=(), inc=True):
        if self.stopped:
            return
        waits = self._waits(eng, R, W)
        sem = self.esem[eng]
        if inc:
            self.cnt[eng] += 1
            c = self.cnt[eng]
            self.q[eng].append((waits, fn, sem, 1))
        else:
            c = self.cnt[eng] + 1
            self.q[eng].append((waits, fn, sem, 0))
        tok = (eng, sem, c)
        for b in W:
            b.w = tok
            b.r = []
        for b in R:
            if b not in W:
                b.r.append(tok)

    def dma(self, out, in_, R, W, semb=None, q="sp"):
        semb = semb or W[0]
        if self.stopped:
            return (None, None, 0)
        if semb.sem is None:
            semb.sem = self.newsem(semb.name)
            self.dmabufs.append(semb)
        waits = self._waits(q, R, W)
        semb.cnt += 16
        nc = self.nc
        eng = {"sp": nc.sync, "act": nc.scalar, "pool": nc.gpsimd}[q]
        self.q[q].append((waits, lambda: eng.dma_start(out=out, in_=in_), semb.sem, 16))
        tok = ("d%d" % id(semb), semb.sem, semb.cnt)
        for b in W:
            b.w = tok
            b.r = []
        for b in R:
            b.r.append(tok)
        return tok

    def act(self, out, in_, func, R, W, **kw):
        nc = self.nc
        self.emit("act", lambda: nc.scalar.activation(out=out, in_=in_, func=func, **kw), R, W)

    def ts(self, eng, out, in0, s1, s2, op0, op1, R, W, **kw):
        e = self.nc.vector if eng == "dve" else self.nc.gpsimd
        if op1 is None:
            self.emit(eng, lambda: e.tensor_scalar(out=out, in0=in0, scalar1=s1, scalar2=None, op0=op0, **kw), R, W)
        else:
            self.emit(eng, lambda: e.tensor_scalar(out=out, in0=in0, scalar1=s1, scalar2=s2, op0=op0, op1=op1, **kw), R, W)

    def tt(self, eng, out, in0, in1, op, R, W):
        e = self.nc.vector if eng == "dve" else self.nc.gpsimd
        self.emit(eng, lambda: e.tensor_tensor(out=out, in0=in0, in1=in1, op=op), R, W)

    def stt(self, out, in0, scalar, in1, op0, op1, R, W):
        nc = self.nc
        self.emit("dve", lambda: nc.vector.scalar_tensor_tensor(out=out, in0=in0, scalar=scalar, in1=in1, op0=op0, op1=op1), R, W)

    def copy(self, eng, out, in_, R, W):
        nc = self.nc
        if eng == "act":
            self.emit("act", lambda: nc.scalar.copy(out=out, in_=in_), R, W)
        elif eng == "dve":
            self.emit("dve", lambda: nc.vector.tensor_copy(out=out, in_=in_), R, W)
        else:
            self.emit("pool", lambda: nc.gpsimd.tensor_copy(out=out, in_=in_), R, W)

    def mm(self, out, lhsT, rhs, start, stop, R, W, inc=None, **kw):
        nc = self.nc
        if inc is None:
            inc = bool(stop)
        self.emit("pe", lambda: nc.tensor.matmul(out, lhsT, rhs, start=start, stop=stop, **kw), R, W, inc=inc)

    def tr(self, out, in_, ident, R, W, inc=True):
        nc = self.nc
        self.emit("pe", lambda: nc.tensor.transpose(out, in_, ident), R, W, inc=inc)

    def replay(self):
        nc = self.nc
        engmap = {"pe": "tensor", "act": "scalar", "dve": "vector", "pool": "gpsimd", "sp": "sync"}
        with nc.Block() as blk:
            for e in self.ENGS:
                lst = self.q[e]
                final = self.final

                def body(eng, lst=lst, e=e):
                    for (waits, fn, sem, n) in lst:
                        for (s, v) in waits:
                            eng.wait_ge(s, v)
                        if n:
                            fn().then_inc(sem, n)
                        else:
                            fn()
                    if e == "sp":
                        for (s, v) in final:
                            eng.wait_ge(s, v)

                getattr(blk, engmap[e])(body)


class T:
    def __init__(self, P, kind, name, shape, dtype):
        nc = P.nc
        if kind == "sb":
            self.t = P.es.enter_context(nc.sbuf_tensor(name, shape, dtype))
        else:
            self.t = P.es.enter_context(nc.psum_tensor(name, shape, dtype))
        self.b = Buf(name)

    def __getitem__(self, k):
        return self.t[k]


def build(S, dbg=None):
    NB = S // 128
    NSB = NB // 4
    NO = NSB
    TO = NO * 128
    KSEL = min(256, S // 4)
    dbg = dbg or ()
    nc = bass.Bass("TRN2", target_bir_lowering=False)

    def din(name, shape, dt=F32):
        return nc.dram_tensor(name, list(shape), dt, kind="ExternalInput").ap()

    def dscr(name, shape, dt):
        return nc.dram_tensor(name, list(shape), dt, kind="Internal").ap()

    x_sh = din("x_sh", [S, D])
    cT = din("cT", [128, KC])
    w_ada = din("w_ada", [D, 6 * D])
    badaT = din("badaT", [128, 96])
    gnmT = din("gnmT", [128, KC])
    gnfT = din("gnfT", [128, KC])
    w_in = din("w_in", [D, IN_COLS])
    lbl = din("lbl", [2, 1024])
    g_rec = din("g_rec", [1, 1024])
    g_att = din("g_att", [1, 1024])
    w_out = din("w_out", [D, D])
    w_rt = din("w_rt", [D, 36])
    b_rt = din("b_rt", [1, 36])
    ne_decl = 1 if any(d.startswith("stop") for d in dbg) else NE
    w_eg = din("w_eg", [ne_decl, D, DE])
    w_eu = din("w_eu", [ne_decl, D, DE])
    w_ed = din("w_ed", [ne_decl, DE, D])
    g_fin = din("g_fin", [1, D])
    validT = din("validT", [128, NB])
    padb = din("padb", [1, 512])
    nvalT = din("nvalT", [128, NO])
    cst = din("cst", [128, 8 * 128])
    out_d = nc.dram_tensor("out", [TO, D], F32, kind="ExternalOutput").ap()

    win_bf = dscr("win_bf", [D, IN_COLS], BF16)
    mod_d = dscr("mod_d", [96 * 128], F32)
    KT_d = dscr("KT_d", [2, 128, S], BF16)
    V_d = dscr("V_d", [S, 256], BF16)
    kiT_d = dscr("kiT_d", [128, S], BF16)
    aqT_d = dscr("aqT_d", [8, 128, TO], BF16)
    iqT_d = dscr("iqT_d", [4, 128, TO], BF16)
    sgn_d = dscr("sgn_d", [TO, 8], F32)
    mixT_d = dscr("mixT_d", [D, TO], BF16)
    x1_d = dscr("x1_d", [TO, D], F32)

    dbg_out = {}

    class _Stop(Exception):
        pass

    es = contextlib.ExitStack()
    with es:
        P = Prog(nc, es)
        allbufs = []

        def stop(tag):
            if tag in dbg:
                P.stopped = True

        def body():

            def sb(name, shape, dt=F32):
                return T(P, "sb", name, shape, dt)

            PS = [T(P, "ps", "ps%d" % i, [128, 512], F32) for i in range(8)]
            psrot = [0]
            psmod = [6]

            def psbank():
                t = PS[psrot[0] % psmod[0]]
                psrot[0] += 1
                return t

            def bfv(ps):
                return ps.t[:, :].bitcast(BF16)

            def dump(name, tile, shape, dt=F32):
                if name not in dbg:
                    return
                o = nc.dram_tensor("dbg_" + name, list(shape), dt, kind="ExternalOutput").ap()
                b = Buf("dbg_" + name)
                tok = P.dma(o, tile.t[:] if isinstance(tile, T) else tile[0], [tile.b if isinstance(tile, T) else tile[1]], [b])
                P.final.append((tok[1], tok[2]))
                dbg_out[name] = (shape, dt)

            cstf = sb("cstf", [128, 8 * 128])
            P.dma(cstf[:], cst[:, :], [], [cstf.b])
            ident_f = cstf[:, 0:128]
            TL = cstf[:, 128:256]
            TU = cstf[:, 256:384]
            CI = cstf[:, 384:386]
            CB = cstf[:, 512:640]
            cstb = sb("cstb", [128, 8 * 128], BF16)
            P.copy("dve", cstb[:], cstf[:], [cstf.b], [cstb.b])
            ident = cstb[:, 0:128]
            MBD = cstf[:, 128:256]
            I4 = cstb[:, 640:640 + 128]

            cTt = sb("cTt", [128, KC])
            P.dma(cTt[:], cT[:, :], [], [cTt.b])
            cact = sb("cact", [128, KC])
            P.act(cact[:], cTt[:], AF.Silu, [cTt.b], [cact.b])
            modps = psbank()
            modT = sb("modT", [128, 96])
            bT = sb("bT", [128, 96])
            modTT = sb("modTT", [96, 128])
            g1t = sb("g1t", [128, KC]); g2t = sb("g2t", [128, KC])
            G1s = sb("G1s", [128, KC]); G2s = sb("G2s", [128, KC])
            with contextlib.ExitStack() as es0:
                wad = [T.__new__(T) for _ in range(2)]
                for i, w in enumerate(wad):
                    w.t = es0.enter_context(nc.sbuf_tensor("wad%d" % i, [128, KC, 512], F32))
                    w.b = Buf("wad%d" % i)
                w_ada_v = w_ada.rearrange("(kc p) c -> p kc c", p=128)
                for n in range(24):
                    w = wad[n % 2]
                    P.dma(w[:], w_ada_v[:, :, n * 512:(n + 1) * 512], [], [w.b])
                    for mm_ in range(4):
                        m = n * 4 + mm_
                        for kc in range(KC):
                            P.mm(modps[:, m:m + 1], w[:, kc, mm_ * 128:(mm_ + 1) * 128], cact[:, kc:kc + 1],
                                 kc == 0, kc == KC - 1, [w.b, cact.b], [modps.b])
                P.dma(bT[:], badaT[:, :], [], [bT.b])
                P.tt("dve", modT[:], modps[:, 0:96], bT[:], ALU.add, [modps.b, bT.b], [modT.b])
                dump("modT", modT, [128, 96])
                P.dma(g1t[:], gnmT[:, :], [], [g1t.b])
                P.dma(g2t[:], gnfT[:, :], [], [g2t.b])
                P.stt(G1s[:], modT[:, 16:32], 1.0, g1t[:], ALU.add, ALU.mult, [modT.b, g1t.b], [G1s.b])
                P.stt(G2s[:], modT[:, 64:80], 1.0, g2t[:], ALU.add, ALU.mult, [modT.b, g2t.b], [G2s.b])
                SH1s = modT[:, 0:16]
                SH2s = modT[:, 48:64]
                modD = Buf("mod_d")
                pmt = psbank()
                P.tr(pmt[0:96, 0:128], modT[:], ident_f, [modT.b, cstf.b], [pmt.b])
                P.copy("dve", modTT[:], pmt[0:96, 0:128], [pmt.b], [modTT.b])
                P.dma(mod_d.rearrange("(m p) -> m p", p=128), modTT[:], [modTT.b], [modD])

                winD = Buf("win_bf")
                wst = []
                wsb = []
                for i in range(2):
                    a = T.__new__(T); a.t = es0.enter_context(nc.sbuf_tensor("wst%d" % i, [128, IN_COLS], F32)); a.b = Buf("wst%d" % i)
                    c_ = T.__new__(T); c_.t = es0.enter_context(nc.sbuf_tensor("wsb%d" % i, [128, IN_COLS], BF16)); c_.b = Buf("wsb%d" % i)
                    wst.append(a); wsb.append(c_)
                for kc in range(KC):
                    a = wst[kc % 2]; c_ = wsb[kc % 2]
                    P.dma(a[:], w_in[kc * 128:(kc + 1) * 128, :], [], [a.b])
                    h = IN_COLS // 2
                    P.copy("act", c_[:, 0:h], a[:, 0:h], [a.b], [c_.b])
                    P.copy("pool", c_[:, h:], a[:, h:], [a.b], [c_.b])
                    P.dma(win_bf[kc * 128:(kc + 1) * 128, :], c_[:], [c_.b], [winD], semb=winD)

            stop("stop0")
            P.set_barrier()
            with contextlib.ExitStack() as es1:
                def sb1(name, shape, dt=F32):
                    t = T.__new__(T)
                    t.t = es1.enter_context(nc.sbuf_tensor(name, shape, dt))
                    t.b = P.mkbuf(name)
                    return t

                LB = sb1("LB", [128, 1024]); OMLB = sb1("OMLB", [128, 1024]); GS = sb1("GS", [128, 1024]); l1 = GS
                P.dma(LB[:], lbl[0:1, :].partition_broadcast(128), [], [LB.b])
                P.dma(l1[:], lbl[1:2, :].partition_broadcast(128), [], [l1.b])
                P.tt("dve", LB[:], LB[:], l1[:], ALU.subtract, [LB.b, l1.b], [LB.b])
                P.act(LB[:], LB[:], AF.Sigmoid, [LB.b], [LB.b])
                P.ts("dve", OMLB[:], LB[:], -1.0, 1.0, ALU.mult, ALU.add, [LB.b], [OMLB.b])
                GR = sb1("GR", [128, 1024])
                P.dma(GR[:], g_rec[0:1, :].partition_broadcast(128), [], [GR.b])
                vT = sb1("vT", [128, NB])
                P.dma(vT[:], validT[:, :], [], [vT.b])

                xt = [sb1("xt%d" % i, [128, D]) for i in range(2)]
                junk = sb1("junk", [128, 128], BF16)
                xn = [sb1("xn%d" % i, [128, D], BF16) for i in range(2)]
                st = sb1("st", [128, 8])
                hT = sb1("hT", [128, KC, 512], BF16)
                wt = [sb1("wt%d" % i, [128, KC, 512], BF16) for i in range(2)]
                wrot = [0]
                sgf = sb1("sgf", [128, 4, 1024])
                lgf = sb1("lgf", [128, 4, 1024])
                vbf = sb1("vbf", [128, 4, 1024], BF16)
                kvt = sb1("kvt", [128, 4, 512], BF16)
                ikt = sb1("ikt", [128, 4, 128], BF16)
                qs = sb1("qs", [128, 1024]); gs = sb1("gs", [128, 1024])
                aqt = sb1("aqt", [128, 1024], BF16)
                iqt = sb1("iqt", [128, 512]); iwt = sb1("iwt", [128, 8])
                eR = sb1("eR", [128, 1024]); eB = sb1("eB", [128, 1024])
                ktb = sb1("ktb", [128, 1024], BF16)
                qtb = sb1("qtb", [128, 1024], BF16); qtb1 = sb1("qtb1", [128, 1024], BF16); khb = sb1("khb", [128, 1024], BF16)
                dec = sb1("dec", [128, 16])
                Sst = sb1("Sst", [128, 1024])
                Sbf = [sb1("Sbf%d" % i, [128, 1024], BF16) for i in range(2)]
                qT0 = sb1("qT0", [128, 1024], BF16); qT1 = sb1("qT1", [128, 1024], BF16)
                khT = sb1("khT", [128, 1024], BF16)
                pT = [sb1("pT%d" % i, [128, 128], BF16) for i in range(2)]
                ssq = sb1("ssq", [128, 8]); rsq = sb1("rsq", [128, 8])
                recb = sb1("recb", [128, 1024], BF16)
                trs = sb1("trs", [128, 1536], BF16)
                recT = sb1("recT", [128, 1024], BF16)
                aqT = sb1("aqT", [128, 1024], BF16)
                iqs = sb1("iqs", [128, 512], BF16)
                iqT = sb1("iqT", [128, 512], BF16)
                aw = sb1("aw", [128, 8]); sgn = sb1("sgn", [128, 8])
                P.emit("pool", lambda: nc.gpsimd.memset(Sst[:], 0.0), [], [Sst.b])
                P.emit("pool", lambda: nc.gpsimd.memset(qtb[:], 0.0), [], [qtb.b])
                P.emit("pool", lambda: nc.gpsimd.memset(qtb1[:], 0.0), [], [qtb1.b])

                KTD = Buf("KT_d"); VD = Buf("V_d"); KID = Buf("kiT_d"); AQD = Buf("aqT_d"); IQD = Buf("iqT_d")
                SGD = Buf("sgn_d"); MIXD = Buf("mixT_d")
                win_v = win_bf.rearrange("(kc p) c -> p kc c", p=128)

                def rms_rstd(ssap, outap, n, scale, R, W):
                    P.ts("dve", outap, ssap, scale, EPS, ALU.mult, ALU.add, R, W)
                    P.act(outap, outap, AF.Sqrt, W, W)
                    P.emit("dve", lambda: nc.vector.reciprocal(out=outap, in_=outap), W, W)

                def proj_tile(c0, ncols, blks, evac):
                    w = wt[wrot[0] % 2]; wrot[0] += 1
                    P.dma(w[:, :, 0:ncols], win_v[:, :, c0:c0 + ncols], [winD], [w.b])
                    for blk in blks:
                        ps = psbank()
                        for kc in range(KC):
                            P.mm(ps[:, 0:ncols], hT[:, kc, blk * 128:(blk + 1) * 128], w[:, kc, 0:ncols],
                                 kc == 0, kc == KC - 1, [hT.b, w.b], [ps.b])
                        evac(ps, blk)

                for sbi in range(NSB):
                    for blk in range(4):
                        p = sbi * 4 + blk
                        x_ = xt[p % 2]; xn_ = xn[p % 2]
                        P.dma(x_[:], x_sh[p * 128:(p + 1) * 128, :], [], [x_.b])
                        P.act(xn_[:], x_[:], AF.Square, [x_.b], [xn_.b, st.b], accum_out=st[:, 0:1])
                        rms_rstd(st[:, 0:1], st[:, 1:2], 1, 1.0 / D, [st.b], [st.b])
                        P.ts("dve", xn_[:], x_[:], st[:, 1:2], None, ALU.mult, None, [x_.b, st.b], [xn_.b])
                        for half in range(2):
                            ps = psbank()
                            pv = bfv(ps)
                            for k8 in range(8):
                                kc = half * 8 + k8
                                P.tr(pv[:, k8 * 128:(k8 + 1) * 128], xn_[:, kc * 128:(kc + 1) * 128], ident, [xn_.b, cstb.b], [ps.b])
                            for k8 in range(8):
                                kc = half * 8 + k8
                                o = hT[:, kc, blk * 128:(blk + 1) * 128]
                                i_ = pv[:, k8 * 128:(k8 + 1) * 128]
                                if k8 % 2 == 0:
                                    P.act(o, i_, AF.Identity, [ps.b, G1s.b, modT.b], [hT.b], scale=G1s[:, kc:kc + 1], bias=SH1s[:, kc:kc + 1])
                                else:
                                    P.ts("dve", o, i_, G1s[:, kc:kc + 1], SH1s[:, kc:kc + 1], ALU.mult, ALU.add, [ps.b, G1s.b, modT.b], [hT.b])
                    if sbi == 0:
                        dump("hT", hT, [128, KC, 512], BF16)
                    stop("stopA")
                    for ti in range(2):
                        proj_tile(C_RF + ti * 512, 512, range(4),
                                  lambda ps, blk, ti=ti: P.act(sgf[:, blk, ti * 512:(ti + 1) * 512], ps[:, :], AF.Sigmoid, [ps.b], [sgf.b]))
                    for blk in range(4):
                        P.tt("dve", sgf[:, blk, :], sgf[:, blk, :], OMLB[:], ALU.mult, [sgf.b, OMLB.b], [sgf.b])
                        P.tt("dve", sgf[:, blk, :], sgf[:, blk, :], LB[:], ALU.add, [sgf.b, LB.b], [sgf.b])
                        P.act(lgf[:, blk, :], sgf[:, blk, :], AF.Ln, [sgf.b], [lgf.b])
                        P.ts("pool", sgf[:, blk, :], sgf[:, blk, :], -1.0, 1.0, ALU.mult, ALU.add, [sgf.b, lgf.b], [sgf.b])
                    for ti in range(2):
                        proj_tile(C_RI + ti * 512, 512, range(4),
                                  lambda ps, blk, ti=ti: P.copy("act", vbf[:, blk, ti * 512:(ti + 1) * 512], ps[:, :], [ps.b], [vbf.b]))
                    proj_tile(C_AK, 512, range(4), lambda ps, blk: P.copy("dve", kvt[:, blk, :], ps[:, :], [ps.b], [kvt.b]))

                    def ev_ik(ps, blk):
                        P.copy("act", ikt[:, blk, 0:64], ps[:, 0:64], [ps.b], [ikt.b])
                        P.copy("act", ikt[:, blk, 64:128], ps[:, 0:64], [ps.b], [ikt.b])
                    proj_tile(C_IK, 64, range(4), ev_ik)
                    for ti in range(2):
                        proj_tile(C_RQ + ti * 512, 512, [3],
                                  lambda ps, blk, ti=ti: P.act(qs[:, ti * 512:(ti + 1) * 512], ps[:, :], AF.Silu, [ps.b], [qs.b]))
                    for ti in range(2):
                        proj_tile(C_RG + ti * 512, 512, [3],
                                  lambda ps, blk, ti=ti: P.act(gs[:, ti * 512:(ti + 1) * 512], ps[:, :], AF.Silu, [ps.b], [gs.b]))
                    for ti in range(2):
                        proj_tile(C_AQ + ti * 512, 512, [3],
                                  lambda ps, blk, ti=ti: P.copy("dve", aqt[:, ti * 512:(ti + 1) * 512], ps[:, :], [ps.b], [aqt.b]))
                    proj_tile(C_IQ, 512, [3], lambda ps, blk: P.copy("dve", iqt[:], ps[:, :], [ps.b], [iqt.b]))
                    proj_tile(C_IW, 8, [3], lambda ps, blk: P.copy("dve", iwt[:], ps[:, 0:8], [ps.b], [iwt.b]))

                    stop("stopB")
                    for blk in range(4):
                        p = sbi * 4 + blk
                        own = (blk == 3)
                        for ti in range(2):
                            ps = psbank()
                            P.mm(ps[:, :], TU, lgf[:, blk, ti * 512:(ti + 1) * 512], True, True, [cstf.b, lgf.b], [ps.b])
                            P.act(eR[:, ti * 512:(ti + 1) * 512], ps[:, :], AF.Exp, [ps.b], [eR.b])
                        P.stt(ktb[:], sgf[:, blk, :], vT[:, p:p + 1], eR[:], ALU.mult, ALU.mult, [sgf.b, vT.b, eR.b], [ktb.b])
                        psd = psbank()
                        for hd in range(8):
                            P.mm(psd[:, 2 * hd:2 * hd + 2], lgf[:, blk, hd * 128:(hd + 1) * 128], CI, True, True, [lgf.b, cstf.b], [psd.b])
                        P.act(dec[:], psd[:, 0:16], AF.Exp, [psd.b], [dec.b])
                        stop("stopD1")
                        if own:
                            for ti in range(2):
                                ps = psbank()
                                P.mm(ps[:, :], TL, lgf[:, blk, ti * 512:(ti + 1) * 512], True, True, [cstf.b, lgf.b], [ps.b])
                                P.act(eB[:, ti * 512:(ti + 1) * 512], ps[:, :], AF.Exp, [ps.b], [eB.b])
                                P.act(eR[:, ti * 512:(ti + 1) * 512], ps[:, :], AF.Exp, [ps.b, ktb.b], [eR.b], scale=-1.0)
                            P.stt(qtb[0:64, :], qs[0:64, :], 128.0 ** -0.5, eB[0:64, :], ALU.mult, ALU.mult, [qs.b, eB.b], [qtb.b])
                            P.stt(qtb1[64:128, :], qs[64:128, :], 128.0 ** -0.5, eB[64:128, :], ALU.mult, ALU.mult, [qs.b, eB.b], [qtb1.b])
                            P.tt("dve", khb[:], sgf[:, blk, :], eR[:], ALU.mult, [sgf.b, eR.b], [khb.b])
                            stop("stopD3a")
                            psq = psbank(); psk = psbank()
                            pq = bfv(psq); pk = bfv(psk)
                            for hd in range(8):
                                P.tr(pq[:, hd * 128:(hd + 1) * 128], qtb[:, hd * 128:(hd + 1) * 128], ident, [qtb.b, cstb.b], [psq.b])
                            for hd in range(8):
                                P.tr(pk[:, hd * 128:(hd + 1) * 128], khb[:, hd * 128:(hd + 1) * 128], ident, [khb.b, cstb.b], [psk.b])
                            psq1 = psbank(); pq1 = bfv(psq1)
                            for hd in range(8):
                                P.tr(pq1[:, hd * 128:(hd + 1) * 128], qtb1[:, hd * 128:(hd + 1) * 128], ident, [qtb1.b, cstb.b], [psq1.b])
                            stop("stopD3b")
                            P.copy("act", qT0[:], pq[:, :], [psq.b], [qT0.b])
                            P.copy("dve", qT1[:], pq1[:, :], [psq1.b], [qT1.b])
                            P.copy("act", khT[:], pk[:, :], [psk.b], [khT.b])
                            stop("stopD3")
                        for c in range(2):
                            if own:
                                P.copy("act" if c == 0 else "pool", Sbf[c][:], Sst[:], [Sst.b], [Sbf[c].b])
                            for hh in range(2):
                                ps = psbank()
                                for h4 in range(4):
                                    hd = hh * 4 + h4
                                    P.mm(ps[:, h4 * 128:(h4 + 1) * 128], ktb[c * 64:(c + 1) * 64, hd * 128:(hd + 1) * 128],
                                         vbf[c * 64:(c + 1) * 64, blk, hd * 128:(hd + 1) * 128], True, True, [ktb.b, vbf.b], [ps.b])
                                for h4 in range(4):
                                    hd = hh * 4 + h4
                                    P.stt(Sst[:, hd * 128:(hd + 1) * 128], Sst[:, hd * 128:(hd + 1) * 128], dec[:, 2 * hd + c:2 * hd + c + 1], ps[:, h4 * 128:(h4 + 1) * 128],
                                          ALU.mult, ALU.add, [Sst.b, dec.b, ps.b], [Sst.b])
                        stop("stopD2")
                        if own:
                            ob = sbi
                            pso = [PS[6], PS[7]]
                            for hd in range(8):
                                pss = psbank()
                                P.mm(pss[:, 0:128], khT[:, hd * 128:(hd + 1) * 128], qT0[:, hd * 128:(hd + 1) * 128], True, False, [khT.b, qT0.b], [pss.b])
                                P.mm(pss[:, 0:128], khT[:, hd * 128:(hd + 1) * 128], qT1[:, hd * 128:(hd + 1) * 128], False, True, [khT.b, qT1.b], [pss.b])
                                pt = pT[hd % 2]
                                P.tt("dve", pt[:], pss[:, 0:128], MBD, ALU.mult, [pss.b, cstf.b], [pt.b])
                                po = pso[hd // 4]
                                oo = po[:, (hd % 4) * 128:(hd % 4 + 1) * 128]
                                P.mm(oo, pt[:], vbf[:, blk, hd * 128:(hd + 1) * 128], True, False, [pt.b, vbf.b], [po.b])
                                P.mm(oo, qT0[:, hd * 128:(hd + 1) * 128], Sbf[0][:, hd * 128:(hd + 1) * 128], False, False, [qT0.b, Sbf[0].b], [po.b])
                                P.mm(oo, qT1[:, hd * 128:(hd + 1) * 128], Sbf[1][:, hd * 128:(hd + 1) * 128], False, True, [qT1.b, Sbf[1].b], [po.b])
                            stop("stopD3c")
                            for hd in range(8):
                                po = pso[hd // 4]
                                P.act(junk[:, 0:128], po[:, (hd % 4) * 128:(hd % 4 + 1) * 128], AF.Square, [po.b], [junk.b, ssq.b],
                                      accum_out=ssq[:, hd:hd + 1])
                            stop("stopD3d")
                            rms_rstd(ssq[:], rsq[:], 8, 1.0 / 128, [ssq.b], [rsq.b])
                            P.tt("dve", GS[:], gs[:], GR[:], ALU.mult, [gs.b, GR.b], [GS.b])
                            for hd in range(8):
                                po = pso[hd // 4]
                                P.stt(recb[:, hd * 128:(hd + 1) * 128], po[:, (hd % 4) * 128:(hd % 4 + 1) * 128], rsq[:, hd:hd + 1],
                                      GS[:, hd * 128:(hd + 1) * 128], ALU.mult, ALU.mult, [po.b, rsq.b, GS.b], [recb.b])
                            if sbi == 0:
                                dump("rec0", recb, [128, 1024], BF16)
                            stop("stopD4")
                            psr = psbank(); pr = bfv(psr)
                            for hd in range(8):
                                P.tr(pr[:, hd * 128:(hd + 1) * 128], recb[:, hd * 128:(hd + 1) * 128], ident, [recb.b, cstb.b], [psr.b])
                            P.copy("act", recT[:], pr[:, :], [psr.b], [recT.b])
                            P.dma(mixT_d[0:1024, ob * 128:(ob + 1) * 128].rearrange("(h p) t -> p h t", p=128), recT[:].rearrange("p (h t) -> p h t", h=8), [recT.b], [MIXD], semb=MIXD)
                            psa = psbank(); pa = bfv(psa)
                            for hd in range(8):
                                P.tr(pa[:, hd * 128:(hd + 1) * 128], aqt[:, hd * 128:(hd + 1) * 128], ident, [aqt.b, cstb.b], [psa.b])
                            P.copy("act", aqT[:], pa[:, :], [psa.b], [aqT.b])
                            P.dma(aqT_d[:, :, ob * 128:(ob + 1) * 128].rearrange("h p t -> p h t"), aqT[:].rearrange("p (h t) -> p h t", h=8), [aqT.b], [AQD], semb=AQD)
                            P.act(aw[:], iwt[:], AF.Abs, [iwt.b], [aw.b], scale=64.0 ** -0.5 * 8.0 ** -0.5)
                            P.ts("dve", sgn[:], iwt[:], 0.0, 2.0, ALU.is_ge, ALU.mult, [iwt.b], [sgn.b])
                            P.ts("dve", sgn[:], sgn[:], -1.0, None, ALU.add, None, [sgn.b], [sgn.b])
                            for h in range(8):
                                P.ts("pool", iqs[:, h * 64:(h + 1) * 64], iqt[:, h * 64:(h + 1) * 64], aw[:, h:h + 1], None, ALU.mult, None,
                                     [iqt.b, aw.b], [iqs.b])
                            psi = psbank(); pi = bfv(psi)
                            for c4 in range(4):
                                P.tr(pi[:, c4 * 128:(c4 + 1) * 128], iqs[:, c4 * 128:(c4 + 1) * 128], ident, [iqs.b, cstb.b], [psi.b])
                            P.copy("act", iqT[:], pi[:, 0:512], [psi.b], [iqT.b])
                            P.dma(iqT_d[:, :, ob * 128:(ob + 1) * 128].rearrange("h p t -> p h t"), iqT[:].rearrange("p (h t) -> p h t", h=4), [iqT.b], [IQD], semb=IQD)
                            P.dma(sgn_d[ob * 128:(ob + 1) * 128, :], sgn[:], [sgn.b], [SGD], semb=SGD)
                    stop("stopD")
                    pst = [psbank(), psbank()]
                    for blk in range(4):
                        for w3 in range(3):
                            idx = blk * 3 + w3
                            pv = bfv(pst[idx // 8])
                            src = kvt[:, blk, w3 * 128:(w3 + 1) * 128] if w3 < 2 else ikt[:, blk, :]
                            P.tr(pv[:, (idx % 8) * 128:(idx % 8 + 1) * 128], src, ident, [kvt.b, ikt.b, cstb.b], [pst[idx // 8].b])
                    P.copy("act", trs[:, 0:1024], bfv(pst[0])[:, :], [pst[0].b], [trs.b])
                    P.copy("dve", trs[:, 1024:1536], bfv(pst[1])[:, 0:512], [pst[1].b], [trs.b])
                    trv = trs[:].rearrange("p (b w t) -> p b w t", w=3, t=128)
                    t0 = sbi * 512
                    for kv in range(2):
                        P.dma(KT_d[kv, :, t0:t0 + 512].rearrange("p (b t) -> p b t", b=4), trv[:, :, kv, :], [trs.b], [KTD], semb=KTD)
                    P.dma(kiT_d[:, t0:t0 + 512].rearrange("p (b t) -> p b t", b=4), trv[:, :, 2, :], [trs.b], [KID], semb=KID)
                    P.dma(V_d[t0:t0 + 512, :].rearrange("(b p) c -> p b c", p=128), kvt[:, :, 256:512], [kvt.b], [VD], semb=VD)
                dump("Sst", Sst, [128, 1024])

            stop("stop1")

            psmod[0] = 3
            NIT = 22
            P.set_barrier()
            with contextlib.ExitStack() as es2:
                def sb2(name, shape, dt=F32):
                    t = T.__new__(T)
                    t.t = es2.enter_context(nc.sbuf_tensor(name, shape, dt))
                    t.b = P.mkbuf(name)
                    return t
                KT = sb2("KT", [128, 2 * S], BF16)
                kiT = sb2("kiT", [128, S], BF16)
                Vaug = sb2("Vaug", [128, NB * 2 * 132], BF16)
                scoreL = [sb2("score%d" % k, [128, S]) for k in range(2)]
                nbiasL = [sb2("nbias%d" % k, [128, S], BF16) for k in range(2)]
                rbuf = [sb2("rbuf%d" % i, [128, 512], BF16) for i in range(2)]
                pbuf = [sb2("pbuf%d" % i, [128, 512], BF16) for i in range(2)]
                aqTiL = [sb2("aqTi%d" % k, [128, 1024], BF16) for k in range(2)]
                iqTi = sb2("iqTi", [128, 512], BF16)
                sgni = sb2("sgni", [128, 8])
                Dh = sb2("Dh", [128, 1024], BF16)
                CBf = sb2("CBf", [128, 512], BF16)
                PBt = sb2("PBt", [128, 512], BF16)
                BIGI4 = sb2("BIGI4", [128, 512], BF16)
                GA = sb2("GA", [128, 1024])
                nv = sb2("nv", [128, NO])
                smL = [sb2("sm%d" % k, [128, 16]) for k in range(2)]
                sm2 = sb2("sm2", [128, 8]); sm3 = sb2("sm3", [128, 8]); sm4 = sb2("sm4", [128, 8])
                junk2 = sb2("junk2", [128, 128], BF16)
                attb = sb2("attb", [128, 1024], BF16)
                attT = sb2("attT", [128, 1024], BF16)
                P.dma(KT[:, 0:S], KT_d[0], [KTD], [KT.b])
                P.dma(KT[:, S:2 * S], KT_d[1], [KTD], [KT.b])
                P.dma(kiT[:], kiT_d[:, :], [KID], [kiT.b])
                P.emit("pool", lambda: nc.gpsimd.memset(Vaug[:], 1.0), [], [Vaug.b])
                Vv = Vaug[:].rearrange("p (b k c) -> p b k c", k=2, c=132)
                V_dv = V_d.rearrange("(b p) c -> p b c", p=128)
                for b0 in range(0, NB, 16):
                    b1 = min(NB, b0 + 16)
                    for kv in range(2):
                        P.dma(Vv[:, b0:b1, kv, 0:128], V_dv[:, b0:b1, kv * 128:(kv + 1) * 128], [VD], [Vaug.b])
                P.emit("pool", lambda: nc.gpsimd.memset(CBf[:], 0.0), [], [CBf.b])
                P.copy("pool", CBf[:, 384:512], CB, [cstf.b], [CBf.b])
                P.dma(scoreL[0][:, 0:512], padb[0:1, :].partition_broadcast(128), [], [scoreL[0].b])
                P.copy("pool", PBt[:], scoreL[0][:, 0:512], [scoreL[0].b], [PBt.b])
                for r4 in range(4):
                    P.ts("dve", BIGI4[:, r4 * 128:(r4 + 1) * 128], ident_f, 29952.0, None, ALU.mult, None, [cstf.b], [BIGI4.b])
                P.dma(GA[:], g_att[0:1, :].partition_broadcast(128), [], [GA.b])
                P.dma(nv[:], nvalT[:, :], [], [nv.b])
                acc = [PS[3], PS[4], PS[5]]
                def stageA(i):
                    score = scoreL[i % 2]; nbias = nbiasL[i % 2]; junkb = nbias; aqTi = aqTiL[i % 2]; sm = smL[i % 2]
                    LO, HI, TH, CNT, GE, DD, NGE, SEL, AA, BB, THF = [sm[:, k:k + 1] for k in range(11)]
                    SMB = [sm.b]
                    nkt = i + 1
                    n = nkt * 512
                    nk = 4 * (i + 1)
                    P.dma(aqTi[:].rearrange("p (h t) -> p h t", h=8), aqT_d[:, :, i * 128:(i + 1) * 128].rearrange("h p t -> p h t"), [AQD], [aqTi.b])
                    P.dma(iqTi[:].rearrange("p (h t) -> p h t", h=4), iqT_d[:, :, i * 128:(i + 1) * 128].rearrange("h p t -> p h t"), [IQD], [iqTi.b])
                    P.dma(sgni[:], sgn_d[i * 128:(i + 1) * 128, :], [SGD], [sgni.b])
                    for h in range(8):
                        P.ts("dve", Dh[:, h * 128:(h + 1) * 128], ident_f, sgni[:, h:h + 1], None, ALU.mult, None, [cstf.b, sgni.b], [Dh.b])
                    for kt in range(nkt):
                        psc = PS[6 + kt % 2]
                        for h in range(8):
                            ps = psbank()
                            pb = (h % 2) * 64
                            P.mm(ps[:, :], iqTi[pb:pb + 64, (h // 2) * 128:(h // 2 + 1) * 128], kiT[pb:pb + 64, kt * 512:(kt + 1) * 512],
                                 True, True, [iqTi.b, kiT.b], [ps.b])
                            rb = rbuf[h % 2]
                            P.act(rb[:], ps[:, :], AF.Relu, [ps.b], [rb.b])
                            P.mm(psc[:, :], Dh[:, h * 128:(h + 1) * 128], rb[:], h == 0, h == 7, [Dh.b, rb.b], [psc.b])
                        dst = score[:, kt * 512:(kt + 1) * 512]
                        if kt == nkt - 1:
                            P.tt("dve", dst, psc[:, :], CBf[:], ALU.add, [psc.b, CBf.b], [score.b])
                            if kt == 0:
                                P.tt("dve", dst, dst, PBt[:], ALU.add, [score.b, PBt.b], [score.b])
                        elif kt == 0:
                            P.tt("dve", dst, psc[:, :], PBt[:], ALU.add, [psc.b, PBt.b], [score.b])
                        else:
                            P.copy("act", dst, psc[:, :], [psc.b], [score.b])
                    sc = score[:, 0:n]
                    P.emit("dve", lambda sc=sc: nc.vector.reduce_max(out=HI, in_=sc, axis=AX.X), [score.b], SMB)
                    P.ts("dve", LO, HI, -64.0, None, ALU.add, None, SMB, SMB)
                    for it in range(NIT):
                        cw = 64.0 / (2.0 ** (it + 1))
                        P.ts("dve", TH, LO, cw, None, ALU.add, None, SMB, SMB)
                        P.ts("dve", junkb[:, 0:n], sc, TH, 0.0, ALU.is_ge, ALU.add, [score.b] + SMB, [junkb.b] + SMB, accum_out=CNT)
                        P.ts("dve", GE, CNT, float(KSEL), cw, ALU.is_ge, ALU.mult, SMB, SMB)
                        P.tt("dve", LO, LO, GE, ALU.add, SMB, SMB)
                    P.ts("dve", SEL, nv[:, i:i + 1], float(KSEL), None, ALU.is_gt, None, [nv.b], SMB)
                    P.ts("dve", AA, SEL, 1e20, -1e20, ALU.mult, ALU.add, SMB, SMB)
                    P.tt("dve", BB, LO, SEL, ALU.mult, SMB, SMB)
                    P.tt("dve", THF, AA, BB, ALU.add, SMB, SMB)
                    P.ts("dve", nbias[:, 0:n], sc, THF, 1.0, ALU.is_ge, ALU.subtract, [score.b] + SMB, [nbias.b])
                def stageB(i):
                    nbias = nbiasL[i % 2]; aqTi = aqTiL[i % 2]
                    nk = 4 * (i + 1)
                    for a in acc:
                        P.emit("dve", lambda a=a: nc.vector.memset(a[:, :], 0.0), [], [a.b])
                    for kb in range(nk):
                        for kvh in range(2):
                            psl = psbank()
                            P.mm(psl[:, :], KT[:, kvh * S + kb * 128:kvh * S + (kb + 1) * 128], aqTi[:, kvh * 512:(kvh + 1) * 512],
                                 True, False, [KT.b, aqTi.b], [psl.b])
                            P.mm(psl[:, :], nbias[:, kb * 128:(kb + 1) * 128], BIGI4[:], False, True, [nbias.b, BIGI4.b], [psl.b])
                            pb_ = pbuf[(kb * 2 + kvh) % 2]
                            P.act(pb_[:], psl[:, :], AF.Exp, [psl.b], [pb_.b], scale=128.0 ** -0.5)
                            vo = (kb * 2 + kvh) * 132
                            for g in range(4):
                                hd = kvh * 4 + g
                                a = acc[hd // 3]
                                off = (hd % 3) * 132
                                P.mm(a[:, off:off + 129], pb_[:, g * 128:(g + 1) * 128], Vaug[:, vo:vo + 129], False, False,
                                     [pb_.b, Vaug.b], [a.b], inc=(g == 3), skip_group_check=True)
                    for hd in range(8):
                        a = acc[hd // 3]
                        off = (hd % 3) * 132
                        P.emit("dve", lambda a=a, off=off, hd=hd: nc.vector.reciprocal(out=sm2[:, hd:hd + 1], in_=a[:, off + 128:off + 129]), [a.b], [sm2.b])
                        P.act(junk2[:], a[:, off:off + 128], AF.Square, [a.b, sm2.b], [junk2.b, sm3.b], scale=sm2[:, hd:hd + 1],
                              accum_out=sm3[:, hd:hd + 1])
                    rms_rstd(sm3[:], sm4[:], 8, 1.0 / 128, [sm3.b], [sm4.b])
                    P.tt("dve", sm4[:], sm4[:], sm2[:], ALU.mult, [sm4.b, sm2.b], [sm4.b])
                    for hd in range(8):
                        a = acc[hd // 3]
                        off = (hd % 3) * 132
                        P.stt(attb[:, hd * 128:(hd + 1) * 128], a[:, off:off + 128], sm4[:, hd:hd + 1], GA[:, hd * 128:(hd + 1) * 128],
                              ALU.mult, ALU.mult, [a.b, sm4.b, GA.b], [attb.b])
                    if i == 0:
                        dump("att0", attb, [128, 1024], BF16)
                    if i == 1:
                        dump("att1", attb, [128, 1024], BF16)
                    psr = psbank(); pr = bfv(psr)
                    for hd in range(8):
                        P.tr(pr[:, hd * 128:(hd + 1) * 128], attb[:, hd * 128:(hd + 1) * 128], ident, [attb.b, cstb.b], [psr.b])
                    P.copy("act", attT[:], pr[:, :], [psr.b], [attT.b])
                    P.dma(mixT_d[1024:2048, i * 128:(i + 1) * 128].rearrange("(h p) t -> p h t", p=128), attT[:].rearrange("p (h t) -> p h t", h=8),
                          [attT.b], [MIXD], semb=MIXD)
                stageA(0)
                for i in range(NO):
                    if i + 1 < NO:
                        stageA(i + 1)
                    stageB(i)
            psmod[0] = 6
            stop("stop2")

            h2T_d = dscr("h2T_d", [D, TO], BF16)
            H2D = Buf("h2T_d"); X1D = Buf("x1_d"); OUTD = Buf("out")
            P.set_barrier()
            with contextlib.ExitStack() as es34:
                def sb34(name, shape, dt=F32, st=es34):
                    t = T.__new__(T)
                    t.t = st.enter_context(nc.sbuf_tensor(name, shape, dt))
                    t.b = P.mkbuf(name)
                    return t
                comb = sb34("comb", [128, NO * 32])
                with contextlib.ExitStack() as es3:
                    sb3 = lambda name, shape, dt=F32: sb34(name, shape, dt, es3)
                    Wo = sb3("Wo", [128, KC * 2048], BF16)
                    wst3 = [sb3("wst3_%d" % i, [128, 2048]) for i in range(2)]
                    for kc in range(KC):
                        w = wst3[kc % 2]
                        P.dma(w[:], w_out[kc * 128:(kc + 1) * 128, :], [], [w.b])
                        P.copy("act" if kc % 2 == 0 else "pool", Wo[:, kc * 2048:(kc + 1) * 2048], w[:], [w.b], [Wo.b])
                    GT1b = sb3("GT1b", [128, 2048])
                    mod6 = mod_d.rearrange("(a n) -> a n", a=6)
                    P.dma(GT1b[:], mod6[2:3, :].partition_broadcast(128), [modD], [GT1b.b])
                    Wrt = sb3("Wrt", [128, KC * 36]); Wrtb = sb3("Wrtb", [128, KC * 36], BF16)
                    P.dma(Wrt[:].rearrange("p (k c) -> p k c", c=36), w_rt.rearrange("(k p) c -> p k c", p=128), [], [Wrt.b])
                    P.copy("dve", Wrtb[:], Wrt[:], [Wrt.b], [Wrtb.b])
                    brt = sb3("brt", [128, 36])
                    P.dma(brt[:], b_rt[0:1, :].partition_broadcast(128), [], [brt.b])
                    xo = [sb3("xo%d" % i, [128, D]) for i in range(2)]
                    x1t = [sb3("x1t%d" % i, [128, D]) for i in range(2)]
                    xn2 = sb3("xn2", [128, D], BF16)
                    mixTi = sb3("mixTi", [128, KC * 128], BF16)
                    h2Tb = sb3("h2Tb", [128, KC * 128], BF16)
                    st3 = sb3("st3", [128, 8])
                    lg = sb3("lg", [128, 36])
                    rs = sb3("rs", [128, 64])
                    for i in range(NO):
                        x_ = xo[i % 2]; x1 = x1t[i % 2]
                        p = 4 * i + 3
                        P.dma(x_[:], x_sh[p * 128:(p + 1) * 128, :], [], [x_.b])
                        P.dma(mixTi[:].rearrange("p (k t) -> p k t", t=128), mixT_d[:, i * 128:(i + 1) * 128].rearrange("(k p) t -> p k t", p=128),
                              [MIXD], [mixTi.b])
                        for dt in range(4):
                            ps = psbank()
                            for kc in range(KC):
                                P.mm(ps[:, :], mixTi[:, kc * 128:(kc + 1) * 128], Wo[:, kc * 2048 + dt * 512:kc * 2048 + (dt + 1) * 512],
                                     kc == 0, kc == KC - 1, [mixTi.b, Wo.b], [ps.b])
                            P.tt("dve", x1[:, dt * 512:(dt + 1) * 512], ps[:, :], GT1b[:, dt * 512:(dt + 1) * 512], ALU.mult, [ps.b, GT1b.b], [x1.b])
                        P.tt("dve", x1[:], x1[:], x_[:], ALU.add, [x1.b, x_.b], [x1.b])
                        if i == 0:
                            dump("x1", x1, [128, D])
                        P.dma(x1_d[i * 128:(i + 1) * 128, :], x1[:], [x1.b], [X1D], semb=X1D)
                        P.act(xn2[:], x1[:], AF.Square, [x1.b], [xn2.b, st3.b], accum_out=st3[:, 0:1])
                        rms_rstd(st3[:, 0:1], st3[:, 1:2], 1, 1.0 / D, [st3.b], [st3.b])
                        P.ts("dve", xn2[:], x1[:], st3[:, 1:2], None, ALU.mult, None, [x1.b, st3.b], [xn2.b])
                        for half in range(2):
                            ps = psbank()
                            pv = bfv(ps)
                            for k8 in range(8):
                                kc = half * 8 + k8
                                P.tr(pv[:, k8 * 128:(k8 + 1) * 128], xn2[:, kc * 128:(kc + 1) * 128], ident, [xn2.b, cstb.b], [ps.b])
                            for k8 in range(8):
                                kc = half * 8 + k8
                                o = h2Tb[:, kc * 128:(kc + 1) * 128]
                                i_ = pv[:, k8 * 128:(k8 + 1) * 128]
                                if k8 % 2 == 0:
                                    P.act(o, i_, AF.Identity, [ps.b, G2s.b, modT.b], [h2Tb.b], scale=G2s[:, kc:kc + 1], bias=SH2s[:, kc:kc + 1])
                                else:
                                    P.ts("dve", o, i_, G2s[:, kc:kc + 1], SH2s[:, kc:kc + 1], ALU.mult, ALU.add, [ps.b, G2s.b, modT.b], [h2Tb.b])
                        P.dma(h2T_d[:, i * 128:(i + 1) * 128].rearrange("(k p) t -> p k t", p=128), h2Tb[:].rearrange("p (k t) -> p k t", t=128),
                              [h2Tb.b], [H2D], semb=H2D)
                        psr = psbank()
                        for kc in range(KC):
                            P.mm(psr[:, 0:36], h2Tb[:, kc * 128:(kc + 1) * 128], Wrtb[:, kc * 36:(kc + 1) * 36], kc == 0, kc == KC - 1,
                                 [h2Tb.b, Wrtb.b], [psr.b])
                        P.tt("dve", lg[:], psr[:, 0:36], brt[:], ALU.add, [psr.b, brt.b], [lg.b])
                        RB = [rs.b]
                        GMAX, NGM, SE, PG, M1, M2, DLT, EX, DEN, W1, W2 = [rs[:, k:k + 1] for k in range(11)]
                        OHG = rs[:, 12:16]; ESEL = rs[:, 16:24]; MK1 = rs[:, 24:32]; E2 = rs[:, 32:40]; MK2 = rs[:, 40:48]; CIG = rs[:, 48:56]; EG = rs[:, 56:60]
                        P.emit("dve", lambda: nc.vector.reduce_max(out=GMAX, in_=lg[:, 0:4], axis=AX.X), [lg.b], RB)
                        P.ts("dve", NGM, GMAX, -1.0, None, ALU.mult, None, RB, RB)
                        P.act(EG, lg[:, 0:4], AF.Exp, [lg.b] + RB, RB, bias=NGM, accum_out=SE)
                        P.emit("dve", lambda: nc.vector.reciprocal(out=PG, in_=SE), RB, RB)
                        P.ts("dve", OHG, lg[:, 0:4], GMAX, None, ALU.is_ge, None, [lg.b] + RB, RB)
                        P.ts("dve", ESEL, lg[:, 4:12], rs[:, 12:13], None, ALU.mult, None, [lg.b] + RB, RB)
                        for g in range(1, 4):
                            P.stt(ESEL, lg[:, 4 + 8 * g:12 + 8 * g], rs[:, 12 + g:13 + g], ESEL, ALU.mult, ALU.add, [lg.b] + RB, RB)
                        P.emit("dve", lambda: nc.vector.reduce_max(out=M1, in_=ESEL, axis=AX.X), RB, RB)
                        P.ts("dve", MK1, ESEL, M1, None, ALU.is_ge, None, RB, RB)
                        P.stt(E2, MK1, -1e30, ESEL, ALU.mult, ALU.add, RB, RB)
                        P.emit("dve", lambda: nc.vector.reduce_max(out=M2, in_=E2, axis=AX.X), RB, RB)
                        P.ts("dve", MK2, E2, M2, None, ALU.is_ge, None, RB, RB)
                        P.tt("dve", DLT, M2, M1, ALU.subtract, RB, RB)
                        P.act(EX, DLT, AF.Exp, RB, RB)
                        P.ts("dve", DEN, EX, 1.0, None, ALU.add, None, RB, RB)
                        P.emit("dve", lambda: nc.vector.reciprocal(out=DEN, in_=DEN), RB, RB)
                        P.tt("dve", W1, DEN, PG, ALU.mult, RB, RB)
                        P.tt("dve", W2, W1, EX, ALU.mult, RB, RB)
                        P.ts("dve", CIG, MK1, W1, None, ALU.mult, None, RB, RB)
                        P.stt(CIG, MK2, W2, CIG, ALU.mult, ALU.add, RB, RB)
                        for g in range(4):
                            P.ts("dve", comb[:, i * 32 + g * 8:i * 32 + (g + 1) * 8], CIG, rs[:, 12 + g:13 + g], None, ALU.mult, None, RB, [comb.b])
                stop("stop3")
                P.set_barrier()
                with contextlib.ExitStack() as es4:
                    sb4 = lambda name, shape, dt=F32: sb34(name, shape, dt, es4)
                    CH = min(TO, 1024)
                    NCH = TO // CH
                    NT = CH // 128
                    SUB = min(512, CH)
                    h2c = sb4("h2c", [128, KC * CH], BF16)
                    yacc = sb4("yacc", [128, NT * 2048])
                    WGb = sb4("WGb", [128, KC * 512], BF16); WUb = sb4("WUb", [128, KC * 512], BF16); WDb = sb4("WDb", [128, 4 * 2048], BF16)
                    stg = [sb4("stg%d" % k, [128, 2048]) for k in range(2)]
                    sgt = [sb4("sgt%d" % k, [128, 512]) for k in range(2)]
                    heT = [sb4("heT%d" % k, [128, 4 * 512], BF16) for k in range(2)]
                    GT2b = sb4("GT2b", [128, 2048]); GFb = sb4("GFb", [128, 2048])
                    xf = sb4("xf", [128, 2048])
                    st4 = sb4("st4", [128, 8])
                    P.dma(GT2b[:], mod6[5:6, :].partition_broadcast(128), [modD], [GT2b.b])
                    P.dma(GFb[:], g_fin[0:1, :].partition_broadcast(128), [], [GFb.b])
                    srot = [0]

                    def load_cast(dst_ap, dstb, src_ap, three=None):
                        w = stg[srot[0] % 2]
                        eng = "act" if srot[0] % 2 == 0 else "pool"
                        srot[0] += 1
                        if three is None:
                            P.dma(w[:], src_ap, [], [w.b])
                        else:
                            P.dma(w[:].rearrange("p (k c) -> p k c", c=512), src_ap, [], [w.b])
                        P.copy(eng, dst_ap, w[:], [w.b], [dstb])

                    for ch in range(NCH):
                        P.dma(h2c[:].rearrange("p (k t) -> p k t", t=CH), h2T_d[:, ch * CH:(ch + 1) * CH].rearrange("(k p) t -> p k t", p=128),
                              [H2D], [h2c.b])
                        P.emit("pool", lambda: nc.gpsimd.memset(yacc[:], 0.0), [], [yacc.b])
                        for e in range(NE):
                            for q in range(4):
                                load_cast(WGb[:, q * 2048:(q + 1) * 2048], WGb.b, w_eg[e % ne_decl, q * 512:(q + 1) * 512, :].rearrange("(k p) c -> p k c", p=128), 1)
                            for q in range(4):
                                load_cast(WUb[:, q * 2048:(q + 1) * 2048], WUb.b, w_eu[e % ne_decl, q * 512:(q + 1) * 512, :].rearrange("(k p) c -> p k c", p=128), 1)
                            for c in range(4):
                                load_cast(WDb[:, c * 2048:(c + 1) * 2048], WDb.b, w_ed[e % ne_decl, c * 128:(c + 1) * 128, :])
                            for st_ in range(CH // SUB):
                                tok0 = st_ * SUB
                                he = heT[st_ % 2]
                                for c in range(4):
                                    psg = psbank(); psu = psbank()
                                    for kc in range(KC):
                                        P.mm(psg[:, 0:SUB], WGb[:, kc * 512 + c * 128:kc * 512 + (c + 1) * 128], h2c[:, kc * CH + tok0:kc * CH + tok0 + SUB],
                                             kc == 0, kc == KC - 1, [WGb.b, h2c.b], [psg.b])
                                    for kc in range(KC):
                                        P.mm(psu[:, 0:SUB], WUb[:, kc * 512 + c * 128:kc * 512 + (c + 1) * 128], h2c[:, kc * CH + tok0:kc * CH + tok0 + SUB],
                                             kc == 0, kc == KC - 1, [WUb.b, h2c.b], [psu.b])
                                    sg = sgt[c % 2]
                                    P.act(sg[:, 0:SUB], psg[:, 0:SUB], AF.Silu, [psg.b], [sg.b])
                                    P.tt("dve", he[:, c * 512:c * 512 + SUB], sg[:, 0:SUB], psu[:, 0:SUB], ALU.mult, [sg.b, psu.b], [he.b])
                                for t_ in range(SUB // 128):
                                    tile = st_ * (SUB // 128) + t_
                                    gt = ch * NT + tile
                                    for dt in range(4):
                                        psd = psbank()
                                        for c in range(4):
                                            P.mm(psd[:, :], he[:, c * 512 + t_ * 128:c * 512 + (t_ + 1) * 128], WDb[:, c * 2048 + dt * 512:c * 2048 + (dt + 1) * 512],
                                                 c == 0, c == 3, [he.b, WDb.b], [psd.b])
                                        ya = yacc[:, tile * 2048 + dt * 512:tile * 2048 + (dt + 1) * 512]
                                        P.stt(ya, psd[:, :], comb[:, gt * 32 + e:gt * 32 + e + 1], ya, ALU.mult, ALU.add, [psd.b, comb.b, yacc.b], [yacc.b])
                        for tile in range(NT):
                            gt = ch * NT + tile
                            P.dma(xf[:], x1_d[gt * 128:(gt + 1) * 128, :], [X1D], [xf.b])
                            ya = yacc[:, tile * 2048:(tile + 1) * 2048]
                            P.tt("dve", ya, ya, GT2b[:], ALU.mult, [yacc.b, GT2b.b], [yacc.b])
                            P.tt("dve", xf[:], xf[:], ya, ALU.add, [xf.b, yacc.b], [xf.b])
                            P.act(ya, xf[:], AF.Square, [xf.b], [yacc.b, st4.b], accum_out=st4[:, 0:1])
                            rms_rstd(st4[:, 0:1], st4[:, 1:2], 1, 1.0 / D, [st4.b], [st4.b])
                            P.stt(xf[:], xf[:], st4[:, 1:2], GFb[:], ALU.mult, ALU.mult, [xf.b, st4.b, GFb.b], [xf.b])
                            P.dma(out_d[gt * 128:(gt + 1) * 128, :], xf[:], [xf.b], [OUTD], semb=OUTD)


        try:
            body()
        except _Stop:
            pass
        for b_ in P.dmabufs:
            P.final.append((b_.sem, b_.cnt))
        P.replay()
    return nc, dbg_out


def host_consts():
    c = np.zeros((128, 1024), np.float32)
    i = np.arange(128)
    c[:, 0:128] = np.eye(128)
    same = (i[:, None] // 64) == (i[None, :] // 64)
    c[:, 128:256] = (same & (i[:, None] <= i[None, :])).astype(np.float32)
    c[:, 256:384] = (same & (i[:, None] > i[None, :])).astype(np.float32)
    c[:, 384] = (i < 64)
    c[:, 385] = (i >= 64)
    c[:, 512:640] = np.where(i[None, :] <= i[:, None], 0.0, -1e30)
    c[:, 640:768] = np.eye(128)
    return c


def make_in_maps(inputs, S, ne=NE):
    f = lambda a: np.ascontiguousarray(np.asarray(a, dtype=np.float32))
    x = f(inputs["x"]); c = f(inputs["c"])
    NB = S // 128; NO = NB // 4
    pT = lambda v: np.ascontiguousarray(v.reshape(-1, 128).T)
    shared = {
        "w_ada": f(inputs["w_ada"][0]), "badaT": pT(f(inputs["b_ada"][0])),
        "gnmT": pT(f(inputs["g_norm_mix"][0])), "gnfT": pT(f(inputs["g_norm_ffn"][0])),
        "w_in": f(inputs["w_in"][0]), "lbl": f(inputs["lb_logits"]),
        "g_rec": f(inputs["g_rec_out"]), "g_att": f(inputs["g_att_out"]),
        "w_out": f(inputs["w_out"][0]),
        "w_rt": np.ascontiguousarray(np.concatenate([f(inputs["w_router_group"][0]), f(inputs["w_router_expert"][0])], axis=1)),
        "b_rt": np.ascontiguousarray(np.concatenate([f(inputs["b_router_group"][0]), f(inputs["b_router_expert"][0])])[None, :]),
        "w_eg": f(inputs["w_expert_gate"][0][:ne]), "w_eu": f(inputs["w_expert_up"][0][:ne]), "w_ed": f(inputs["w_expert_down"][0][:ne]),
        "g_fin": f(inputs["g_final"])[None, :], "cst": host_consts(),
    }
    maps = []
    for core in range(8):
        b, j = core // 4, core % 4
        npad = (3 - j) * 128
        xs = np.zeros((S, D), np.float32)
        xs[npad:] = x[b, :S - npad]
        valid = np.ones(S, np.float32); valid[:npad] = 0
        padb = np.zeros((1, 512), np.float32); padb[0, :npad] = -1e30
        nval = np.zeros((128, NO), np.float32)
        for i in range(NO):
            nval[:, i] = (4 * i + 3) * 128 + np.arange(128) - npad + 1
        m = dict(shared)
        m.update({"x_sh": xs, "cT": pT(c[b]), "validT": pT(valid), "padb": padb, "nvalT": nval})
        maps.append(m)
    return maps


def assemble(results, S, B=2):
    NB = S // 128; NO = NB // 4
    out = np.zeros((B, S, D), np.float32)
    for core in range(8):
        b, j = core // 4, core % 4
        o = np.asarray(results[core]["out"]).reshape(NO, 128, D)
        for i in range(NO):
            g = 4 * i + j
            out[b, g * 128:(g + 1) * 128] = o[i]
    return out


_CACHE = {}


def kernel(**inputs):
    S = int(np.asarray(inputs["x"]).shape[1])
    if S not in _CACHE:
        _CACHE[S] = build(S)[0]
    nc = _CACHE[S]
    maps = make_in_maps(inputs, S)
    res = run_bass_kernel_spmd(nc, maps, core_ids=list(range(8)))
    return assemble(res.results, S)
```

```python
import contextlib
import numpy as np
import ml_dtypes
import concourse.bass as bass
import concourse.mybir as mybir
from concourse.bass_utils import run_bass_kernel_spmd

F32 = mybir.dt.float32
BF16 = mybir.dt.bfloat16
AF = mybir.ActivationFunctionType
ALU = mybir.AluOpType
AX = mybir.AxisListType

D = 2048
KC = 16
IN_COLS = 6216
NE = 32
DE = 512
EPS = 1e-6
NEG = -30000.0
SAME_ENGINE_SYNC = True
C_RQ, C_RF, C_RI, C_RG, C_AQ, C_AK, C_AV, C_IQ, C_IK, C_IW = 0, 1024, 2048, 3072, 4096, 5120, 5376, 5632, 6144, 6208


class Buf:
    __slots__ = ("name", "w", "r", "sem", "cnt")

    def __init__(self, name):
        self.name = name
        self.w = None
        self.r = []
        self.sem = None
        self.cnt = 0


class Prog:
    ENGS = ("pe", "act", "dve", "pool", "sp")

    def __init__(self, nc, es):
        self.nc = nc
        self.es = es
        self.q = {e: [] for e in self.ENGS}
        self.cnt = {e: 0 for e in self.ENGS}
        self.esem = {e: es.enter_context(nc.semaphore("sem_" + e)) for e in ("pe", "act", "dve", "pool")}
        self.seen = {e: {} for e in self.ENGS}
        self.nsem = 0
        self.final = []
        self.dmabufs = []
        self.stopped = False
        self.barrier = []

    def set_barrier(self):
        toks = [(e, self.esem[e], self.cnt[e]) for e in ("pe", "act", "dve", "pool") if self.cnt[e] > 0]
        for b in self.dmabufs:
            toks.append(("d%d" % id(b), b.sem, b.cnt))
        self.barrier = toks

    def mkbuf(self, name):
        b = Buf(name)
        b.r = list(self.barrier)
        return b

    def newsem(self, name):
        self.nsem += 1
        return self.es.enter_context(self.nc.semaphore("d_%s_%d" % (name, self.nsem)))

    def _waits(self, eng, R, W):
        toks = []
        for b in R:
            if b.w is not None:
                toks.append(b.w)
        for b in W:
            if b.w is not None:
                toks.append(b.w)
            toks.extend(b.r)
        out = {}
        for (key, sem, v) in toks:
            if key == eng and (eng == "pe" or not SAME_ENGINE_SYNC):
                continue
            if self.seen[eng].get(key, 0) >= v:
                continue
            if out.get(key, (None, 0))[1] < v:
                out[key] = (sem, v)
        for key, (sem, v) in out.items():
            self.seen[eng][key] = v
        return list(out.values())

    def emit(self, eng, fn, R=(), W=(), inc=True):
        if self.stopped:
            return
        waits = self._waits(eng, R, W)
        sem = self.esem[eng]
        if inc:
            self.cnt[eng] += 1
            c = self.cnt[eng]
            self.q[eng].append((waits, fn, sem, 1))
        else:
            c = self.cnt[eng] + 1
            self.q[eng].append((waits, fn, sem, 0))
        tok = (eng, sem, c)
        for b in W:
            b.w = tok
            b.r = []
        for b in R:
            if b not in W:
                b.r.append(tok)

    def dma(self, out, in_, R, W, semb=None, q="sp"):
        semb = semb or W[0]
        if self.stopped:
            return (None, None, 0)
        if semb.sem is None:
            semb.sem = self.newsem(semb.name)
            self.dmabufs.append(semb)
        waits = self._waits(q, R, W)
        semb.cnt += 16
        nc = self.nc
        eng = {"sp": nc.sync, "act": nc.scalar, "pool": nc.gpsimd}[q]
        self.q[q].append((waits, lambda: eng.dma_start(out=out, in_=in_), semb.sem, 16))
        tok = ("d%d" % id(semb), semb.sem, semb.cnt)
        for b in W:
            b.w = tok
            b.r = []
        for b in R:
            b.r.append(tok)
        return tok

    def act(self, out, in_, func, R, W, **kw):
        nc = self.nc
        self.emit("act", lambda: nc.scalar.activation(out=out, in_=in_, func=func, **kw), R, W)

    def ts(self, eng, out, in0, s1, s2, op0, op1, R, W, **kw):
        e = self.nc.vector if eng == "dve" else self.nc.gpsimd
        if op1 is None:
            self.emit(eng, lambda: e.tensor_scalar(out=out, in0=in0, scalar1=s1, scalar2=None, op0=op0, **kw), R, W)
        else:
            self.emit(eng, lambda: e.tensor_scalar(out=out, in0=in0, scalar1=s1, scalar2=s2, op0=op0, op1=op1, **kw), R, W)

    def tt(self, eng, out, in0, in1, op, R, W):
        e = self.nc.vector if eng == "dve" else self.nc.gpsimd
        self.emit(eng, lambda: e.tensor_tensor(out=out, in0=in0, in1=in1, op=op), R, W)

    def stt(self, out, in0, scalar, in1, op0, op1, R, W):
        nc = self.nc
        self.emit("dve", lambda: nc.vector.scalar_tensor_tensor(out=out, in0=in0, scalar=scalar, in1=in1, op0=op0, op1=op1), R, W)

    def copy(self, eng, out, in_, R, W):
        nc = self.nc
        if eng == "act":
            self.emit("act", lambda: nc.scalar.copy(out=out, in_=in_), R, W)
        elif eng == "dve":
            self.emit("dve", lambda: nc.vector.tensor_copy(out=out, in_=in_), R, W)
        else:
            self.emit("pool", lambda: nc.gpsimd.tensor_copy(out=out, in_=in_), R, W)

    def mm(self, out, lhsT, rhs, start, stop, R, W, inc=None, **kw):
        nc = self.nc
        if inc is None:
            inc = bool(stop)
        self.emit("pe", lambda: nc.tensor.matmul(out, lhsT, rhs, start=start, stop=stop, **kw), R, W, inc=inc)

    def tr(self, out, in_, ident, R, W, inc=True):
        nc = self.nc
        self.emit("pe", lambda: nc.tensor.transpose(out, in_, ident), R, W, inc=inc)

    def replay(self):
        nc = self.nc
        engmap = {"pe": "tensor", "act": "scalar", "dve": "vector", "pool": "gpsimd", "sp": "sync"}
        with nc.Block() as blk:
            for e in self.ENGS:
                lst = self.q[e]
                final = self.final

                def body(eng, lst=lst, e=e):
                    for (waits, fn, sem, n) in lst:
                        for (s, v) in waits:
                            eng.wait_ge(s, v)
                        if n:
                            fn().then_inc(sem, n)
                        else:
                            fn()
                    if e == "sp":
                        for (s, v) in final:
                            eng.wait_ge(s, v)

                getattr(blk, engmap[e])(body)


class T:
    def __init__(self, P, kind, name, shape, dtype):
        nc = P.nc
        if kind == "sb":
            self.t = P.es.enter_context(nc.sbuf_tensor(name, shape, dtype))
        else:
            self.t = P.es.enter_context(nc.psum_tensor(name, shape, dtype))
        self.b = Buf(name)

    def __getitem__(self, k):
        return self.t[k]


def build(S, dbg=None):
    NB = S // 128
    NSB = NB // 4
    NO = NSB
    TO = NO * 128
    KSEL = min(256, S // 4)
    dbg = dbg or ()
    nc = bass.Bass("TRN2", target_bir_lowering=False)

    def din(name, shape, dt=F32):
        return nc.dram_tensor(name, list(shape), dt, kind="ExternalInput").ap()

    def dscr(name, shape, dt):
        return nc.dram_tensor(name, list(shape), dt, kind="Internal").ap()

    x_sh = din("x_sh", [S, D])
    cT = din("cT", [128, KC])
    w_ada = din("w_ada", [D, 6 * D])
    badaT = din("badaT", [128, 96])
    gnmT = din("gnmT", [128, KC])
    gnfT = din("gnfT", [128, KC])
    w_in = din("w_in", [D, IN_COLS])
    lbl = din("lbl", [2, 1024])
    g_rec = din("g_rec", [1, 1024])
    g_att = din("g_att", [1, 1024])
    w_out = din("w_out", [D, D])
    w_rt = din("w_rt", [D, 36])
    b_rt = din("b_rt", [1, 36])
    ne_decl = 1 if any(d.startswith("stop") for d in dbg) else NE
    w_eg = din("w_eg", [ne_decl, D, DE])
    w_eu = din("w_eu", [ne_decl, D, DE])
    w_ed = din("w_ed", [ne_decl, DE, D])
    g_fin = din("g_fin", [1, D])
    validT = din("validT", [128, NB])
    padb = din("padb", [1, 512])
    nvalT = din("nvalT", [128, NO])
    cst = din("cst", [128, 8 * 128])
    out_d = nc.dram_tensor("out", [TO, D], F32, kind="ExternalOutput").ap()

    win_bf = dscr("win_bf", [D, IN_COLS], BF16)
    mod_d = dscr("mod_d", [96 * 128], F32)
    KT_d = dscr("KT_d", [2, 128, S], BF16)
    V_d = dscr("V_d", [S, 256], BF16)
    kiT_d = dscr("kiT_d", [128, S], BF16)
    aqT_d = dscr("aqT_d", [8, 128, TO], BF16)
    iqT_d = dscr("iqT_d", [4, 128, TO], BF16)
    sgn_d = dscr("sgn_d", [TO, 8], F32)
    mixT_d = dscr("mixT_d", [D, TO], BF16)
    x1_d = dscr("x1_d", [TO, D], F32)

    dbg_out = {}

    class _Stop(Exception):
        pass

    es = contextlib.ExitStack()
    with es:
        P = Prog(nc, es)
        allbufs = []

        def stop(tag):
            if tag in dbg:
                P.stopped = True

        def body():

            def sb(name, shape, dt=F32):
                return T(P, "sb", name, shape, dt)

            PS = [T(P, "ps", "ps%d" % i, [128, 512], F32) for i in range(8)]
            psrot = [0]
            psmod = [6]

            def psbank():
                t = PS[psrot[0] % psmod[0]]
                psrot[0] += 1
                return t

            def bfv(ps):
                return ps.t[:, :].bitcast(BF16)

            def dump(name, tile, shape, dt=F32):
                if name not in dbg:
                    return
                o = nc.dram_tensor("dbg_" + name, list(shape), dt, kind="ExternalOutput").ap()
                b = Buf("dbg_" + name)
                tok = P.dma(o, tile.t[:] if isinstance(tile, T) else tile[0], [tile.b if isinstance(tile, T) else tile[1]], [b])
                P.final.append((tok[1], tok[2]))
                dbg_out[name] = (shape, dt)

            cstf = sb("cstf", [128, 8 * 128])
            P.dma(cstf[:], cst[:, :], [], [cstf.b])
            ident_f = cstf[:, 0:128]
            TL = cstf[:, 128:256]
            TU = cstf[:, 256:384]
            CI = cstf[:, 384:386]
            CB = cstf[:, 512:640]
            cstb = sb("cstb", [128, 8 * 128], BF16)
            P.copy("dve", cstb[:], cstf[:], [cstf.b], [cstb.b])
            ident = cstb[:, 0:128]
            MBD = cstf[:, 128:256]
            I4 = cstb[:, 640:640 + 128]

            cTt = sb("cTt", [128, KC])
            P.dma(cTt[:], cT[:, :], [], [cTt.b])
            cact = sb("cact", [128, KC])
            P.act(cact[:], cTt[:], AF.Silu, [cTt.b], [cact.b])
            modps = psbank()
            modT = sb("modT", [128, 96])
            bT = sb("bT", [128, 96])
            modTT = sb("modTT", [96, 128])
            g1t = sb("g1t", [128, KC]); g2t = sb("g2t", [128, KC])
            G1s = sb("G1s", [128, KC]); G2s = sb("G2s", [128, KC])
            with contextlib.ExitStack() as es0:
                wad = [T.__new__(T) for _ in range(2)]
                for i, w in enumerate(wad):
                    w.t = es0.enter_context(nc.sbuf_tensor("wad%d" % i, [128, KC, 512], F32))
                    w.b = Buf("wad%d" % i)
                w_ada_v = w_ada.rearrange("(kc p) c -> p kc c", p=128)
                for n in range(24):
                    w = wad[n % 2]
                    P.dma(w[:], w_ada_v[:, :, n * 512:(n + 1) * 512], [], [w.b])
                    for mm_ in range(4):
                        m = n * 4 + mm_
                        for kc in range(KC):
                            P.mm(modps[:, m:m + 1], w[:, kc, mm_ * 128:(mm_ + 1) * 128], cact[:, kc:kc + 1],
                                 kc == 0, kc == KC - 1, [w.b, cact.b], [modps.b])
                P.dma(bT[:], badaT[:, :], [], [bT.b])
                P.tt("dve", modT[:], modps[:, 0:96], bT[:], ALU.add, [modps.b, bT.b], [modT.b])
                dump("modT", modT, [128, 96])
                P.dma(g1t[:], gnmT[:, :], [], [g1t.b])
                P.dma(g2t[:], gnfT[:, :], [], [g2t.b])
                P.stt(G1s[:], modT[:, 16:32], 1.0, g1t[:], ALU.add, ALU.mult, [modT.b, g1t.b], [G1s.b])
                P.stt(G2s[:], modT[:, 64:80], 1.0, g2t[:], ALU.add, ALU.mult, [modT.b, g2t.b], [G2s.b])
                SH1s = modT[:, 0:16]
                SH2s = modT[:, 48:64]
                modD = Buf("mod_d")
                pmt = psbank()
                P.tr(pmt[0:96, 0:128], modT[:], ident_f, [modT.b, cstf.b], [pmt.b])
                P.copy("dve", modTT[:], pmt[0:96, 0:128], [pmt.b], [modTT.b])
                P.dma(mod_d.rearrange("(m p) -> m p", p=128), modTT[:], [modTT.b], [modD])

                winD = Buf("win_bf")
                wst = []
                wsb = []
                for i in range(2):
                    a = T.__new__(T); a.t = es0.enter_context(nc.sbuf_tensor("wst%d" % i, [128, IN_COLS], F32)); a.b = Buf("wst%d" % i)
                    c_ = T.__new__(T); c_.t = es0.enter_context(nc.sbuf_tensor("wsb%d" % i, [128, IN_COLS], BF16)); c_.b = Buf("wsb%d" % i)
                    wst.append(a); wsb.append(c_)
                for kc in range(KC):
                    a = wst[kc % 2]; c_ = wsb[kc % 2]
                    P.dma(a[:], w_in[kc * 128:(kc + 1) * 128, :], [], [a.b])
                    h = IN_COLS // 2
                    P.copy("act", c_[:, 0:h], a[:, 0:h], [a.b], [c_.b])
                    P.copy("pool", c_[:, h:], a[:, h:], [a.b], [c_.b])
                    P.dma(win_bf[kc * 128:(kc + 1) * 128, :], c_[:], [c_.b], [winD], semb=winD)

            stop("stop0")
            P.set_barrier()
            with contextlib.ExitStack() as es1:
                def sb1(name, shape, dt=F32):
                    t = T.__new__(T)
                    t.t = es1.enter_context(nc.sbuf_tensor(name, shape, dt))
                    t.b = P.mkbuf(name)
                    return t

                LB = sb1("LB", [128, 1024]); OMLB = sb1("OMLB", [128, 1024]); GS = sb1("GS", [128, 1024]); l1 = GS
                P.dma(LB[:], lbl[0:1, :].partition_broadcast(128), [], [LB.b])
                P.dma(l1[:], lbl[1:2, :].partition_broadcast(128), [], [l1.b])
                P.tt("dve", LB[:], LB[:], l1[:], ALU.subtract, [LB.b, l1.b], [LB.b])
                P.act(LB[:], LB[:], AF.Sigmoid, [LB.b], [LB.b])
                P.ts("dve", OMLB[:], LB[:], -1.0, 1.0, ALU.mult, ALU.add, [LB.b], [OMLB.b])
                GR = sb1("GR", [128, 1024])
                P.dma(GR[:], g_rec[0:1, :].partition_broadcast(128), [], [GR.b])
                vT = sb1("vT", [128, NB])
                P.dma(vT[:], validT[:, :], [], [vT.b])

                xt = [sb1("xt%d" % i, [128, D]) for i in range(2)]
                junk = sb1("junk", [128, 128], BF16)
                xn = [sb1("xn%d" % i, [128, D], BF16) for i in range(2)]
                st = sb1("st", [128, 8])
                hT = sb1("hT", [128, KC, 512], BF16)
                wt = [sb1("wt%d" % i, [128, KC, 512], BF16) for i in range(2)]
                wrot = [0]
                sgf = sb1("sgf", [128, 4, 1024])
                lgf = sb1("lgf", [128, 4, 1024])
                vbf = sb1("vbf", [128, 4, 1024], BF16)
                kvt = sb1("kvt", [128, 4, 512], BF16)
                ikt = sb1("ikt", [128, 4, 128], BF16)
                qs = sb1("qs", [128, 1024]); gs = sb1("gs", [128, 1024])
                aqt = sb1("aqt", [128, 1024], BF16)
                iqt = sb1("iqt", [128, 512]); iwt = sb1("iwt", [128, 8])
                eR = sb1("eR", [128, 1024]); eB = sb1("eB", [128, 1024])
                ktb = sb1("ktb", [128, 1024], BF16)
                qtb = sb1("qtb", [128, 1024], BF16); qtb1 = sb1("qtb1", [128, 1024], BF16); khb = sb1("khb", [128, 1024], BF16)
                dec = sb1("dec", [128, 16])
                Sst = sb1("Sst", [128, 1024])
                Sbf = [sb1("Sbf%d" % i, [128, 1024], BF16) for i in range(2)]
                qT0 = sb1("qT0", [128, 1024], BF16); qT1 = sb1("qT1", [128, 1024], BF16)
                khT = sb1("khT", [128, 1024], BF16)
                pT = [sb1("pT%d" % i, [128, 128], BF16) for i in range(2)]
                ssq = sb1("ssq", [128, 8]); rsq = sb1("rsq", [128, 8])
                recb = sb1("recb", [128, 1024], BF16)
                trs = sb1("trs", [128, 1536], BF16)
                recT = sb1("recT", [128, 1024], BF16)
                aqT = sb1("aqT", [128, 1024], BF16)
                iqs = sb1("iqs", [128, 512], BF16)
                iqT = sb1("iqT", [128, 512], BF16)
                aw = sb1("aw", [128, 8]); sgn = sb1("sgn", [128, 8])
                P.emit("pool", lambda: nc.gpsimd.memset(Sst[:], 0.0), [], [Sst.b])
                P.emit("pool", lambda: nc.gpsimd.memset(qtb[:], 0.0), [], [qtb.b])
                P.emit("pool", lambda: nc.gpsimd.memset(qtb1[:], 0.0), [], [qtb1.b])

                KTD = Buf("KT_d"); VD = Buf("V_d"); KID = Buf("kiT_d"); AQD = Buf("aqT_d"); IQD = Buf("iqT_d")
                SGD = Buf("sgn_d"); MIXD = Buf("mixT_d")
                win_v = win_bf.rearrange("(kc p) c -> p kc c", p=128)

                def rms_rstd(ssap, outap, n, scale, R, W):
                    P.ts("dve", outap, ssap, scale, EPS, ALU.mult, ALU.add, R, W)
                    P.act(outap, outap, AF.Sqrt, W, W)
                    P.emit("dve", lambda: nc.vector.reciprocal(out=outap, in_=outap), W, W)

                def proj_tile(c0, ncols, blks, evac):
                    w = wt[wrot[0] % 2]; wrot[0] += 1
                    P.dma(w[:, :, 0:ncols], win_v[:, :, c0:c0 + ncols], [winD], [w.b])
                    for blk in blks:
                        ps = psbank()
                        for kc in range(KC):
                            P.mm(ps[:, 0:ncols], hT[:, kc, blk * 128:(blk + 1) * 128], w[:, kc, 0:ncols],
                                 kc == 0, kc == KC - 1, [hT.b, w.b], [ps.b])
                        evac(ps, blk)

                for sbi in range(NSB):
                    for blk in range(4):
                        p = sbi * 4 + blk
                        x_ = xt[p % 2]; xn_ = xn[p % 2]
                        P.dma(x_[:], x_sh[p * 128:(p + 1) * 128, :], [], [x_.b])
                        P.act(xn_[:], x_[:], AF.Square, [x_.b], [xn_.b, st.b], accum_out=st[:, 0:1])
                        rms_rstd(st[:, 0:1], st[:, 1:2], 1, 1.0 / D, [st.b], [st.b])
                        P.ts("dve", xn_[:], x_[:], st[:, 1:2], None, ALU.mult, None, [x_.b, st.b], [xn_.b])
                        for half in range(2):
                            ps = psbank()
                            pv = bfv(ps)
                            for k8 in range(8):
                                kc = half * 8 + k8
                                P.tr(pv[:, k8 * 128:(k8 + 1) * 128], xn_[:, kc * 128:(kc + 1) * 128], ident, [xn_.b, cstb.b], [ps.b])
                            for k8 in range(8):
                                kc = half * 8 + k8
                                o = hT[:, kc, blk * 128:(blk + 1) * 128]
                                i_ = pv[:, k8 * 128:(k8 + 1) * 128]
                                if k8 % 2 == 0:
                                    P.act(o, i_, AF.Identity, [ps.b, G1s.b, modT.b], [hT.b], scale=G1s[:, kc:kc + 1], bias=SH1s[:, kc:kc + 1])
                                else:
                                    P.ts("dve", o, i_, G1s[:, kc:kc + 1], SH1s[:, kc:kc + 1], ALU.mult, ALU.add, [ps.b, G1s.b, modT.b], [hT.b])
                    if sbi == 0:
                        dump("hT", hT, [128, KC, 512], BF16)
                    stop("stopA")
                    for ti in range(2):
                        proj_tile(C_RF + ti * 512, 512, range(4),
                                  lambda ps, blk, ti=ti: P.act(sgf[:, blk, ti * 512:(ti + 1) * 512], ps[:, :], AF.Sigmoid, [ps.b], [sgf.b]))
                    for blk in range(4):
                        P.tt("dve", sgf[:, blk, :], sgf[:, blk, :], OMLB[:], ALU.mult, [sgf.b, OMLB.b], [sgf.b])
                        P.tt("dve", sgf[:, blk, :], sgf[:, blk, :], LB[:], ALU.add, [sgf.b, LB.b], [sgf.b])
                        P.act(lgf[:, blk, :], sgf[:, blk, :], AF.Ln, [sgf.b], [lgf.b])
                        P.ts("pool", sgf[:, blk, :], sgf[:, blk, :], -1.0, 1.0, ALU.mult, ALU.add, [sgf.b, lgf.b], [sgf.b])
                    for ti in range(2):
                        proj_tile(C_RI + ti * 512, 512, range(4),
                                  lambda ps, blk, ti=ti: P.copy("act", vbf[:, blk, ti * 512:(ti + 1) * 512], ps[:, :], [ps.b], [vbf.b]))
                    proj_tile(C_AK, 512, range(4), lambda ps, blk: P.copy("dve", kvt[:, blk, :], ps[:, :], [ps.b], [kvt.b]))

                    def ev_ik(ps, blk):
                        P.copy("act", ikt[:, blk, 0:64], ps[:, 0:64], [ps.b], [ikt.b])
                        P.copy("act", ikt[:, blk, 64:128], ps[:, 0:64], [ps.b], [ikt.b])
                    proj_tile(C_IK, 64, range(4), ev_ik)
                    for ti in range(2):
                        proj_tile(C_RQ + ti * 512, 512, [3],
                                  lambda ps, blk, ti=ti: P.act(qs[:, ti * 512:(ti + 1) * 512], ps[:, :], AF.Silu, [ps.b], [qs.b]))
                    for ti in range(2):
                        proj_tile(C_RG + ti * 512, 512, [3],
                                  lambda ps, blk, ti=ti: P.act(gs[:, ti * 512:(ti + 1) * 512], ps[:, :], AF.Silu, [ps.b], [gs.b]))
                    for ti in range(2):
                        proj_tile(C_AQ + ti * 512, 512, [3],
                                  lambda ps, blk, ti=ti: P.copy("dve", aqt[:, ti * 512:(ti + 1) * 512], ps[:, :], [ps.b], [aqt.b]))
                    proj_tile(C_IQ, 512, [3], lambda ps, blk: P.copy("dve", iqt[:], ps[:, :], [ps.b], [iqt.b]))
                    proj_tile(C_IW, 8, [3], lambda ps, blk: P.copy("dve", iwt[:], ps[:, 0:8], [ps.b], [iwt.b]))

                    stop("stopB")
                    for blk in range(4):
                        p = sbi * 4 + blk
                        own = (blk == 3)
                        for ti in range(2):
                            ps = psbank()
                            P.mm(ps[:, :], TU, lgf[:, blk, ti * 512:(ti + 1) * 512], True, True, [cstf.b, lgf.b], [ps.b])
                            P.act(eR[:, ti * 512:(ti + 1) * 512], ps[:, :], AF.Exp, [ps.b], [eR.b])
                        P.stt(ktb[:], sgf[:, blk, :], vT[:, p:p + 1], eR[:], ALU.mult, ALU.mult, [sgf.b, vT.b, eR.b], [ktb.b])
                        psd = psbank()
                        for hd in range(8):
                            P.mm(psd[:, 2 * hd:2 * hd + 2], lgf[:, blk, hd * 128:(hd + 1) * 128], CI, True, True, [lgf.b, cstf.b], [psd.b])
                        P.act(dec[:], psd[:, 0:16], AF.Exp, [psd.b], [dec.b])
                        stop("stopD1")
                        if own:
                            for ti in range(2):
                                ps = psbank()
                                P.mm(ps[:, :], TL, lgf[:, blk, ti * 512:(ti + 1) * 512], True, True, [cstf.b, lgf.b], [ps.b])
                                P.act(eB[:, ti * 512:(ti + 1) * 512], ps[:, :], AF.Exp, [ps.b], [eB.b])
                                P.act(eR[:, ti * 512:(ti + 1) * 512], ps[:, :], AF.Exp, [ps.b, ktb.b], [eR.b], scale=-1.0)
                            P.stt(qtb[0:64, :], qs[0:64, :], 128.0 ** -0.5, eB[0:64, :], ALU.mult, ALU.mult, [qs.b, eB.b], [qtb.b])
                            P.stt(qtb1[64:128, :], qs[64:128, :], 128.0 ** -0.5, eB[64:128, :], ALU.mult, ALU.mult, [qs.b, eB.b], [qtb1.b])
                            P.tt("dve", khb[:], sgf[:, blk, :], eR[:], ALU.mult, [sgf.b, eR.b], [khb.b])
                            stop("stopD3a")
                            psq = psbank(); psk = psbank()
                            pq = bfv(psq); pk = bfv(psk)
                            for hd in range(8):
                                P.tr(pq[:, hd * 128:(hd + 1) * 128], qtb[:, hd * 128:(hd + 1) * 128], ident, [qtb.b, cstb.b], [psq.b])
                            for hd in range(8):
                                P.tr(pk[:, hd * 128:(hd + 1) * 128], khb[:, hd * 128:(hd + 1) * 128], ident, [khb.b, cstb.b], [psk.b])
                            psq1 = psbank(); pq1 = bfv(psq1)
                            for hd in range(8):
                                P.tr(pq1[:, hd * 128:(hd + 1) * 128], qtb1[:, hd * 128:(hd + 1) * 128], ident, [qtb1.b, cstb.b], [psq1.b])
                            stop("stopD3b")
                            P.copy("act", qT0[:], pq[:, :], [psq.b], [qT0.b])
                            P.copy("dve", qT1[:], pq1[:, :], [psq1.b], [qT1.b])
                            P.copy("act", khT[:], pk[:, :], [psk.b], [khT.b])
                            stop("stopD3")
                        for c in range(2):
                            if own:
                                P.copy("act" if c == 0 else "pool", Sbf[c][:], Sst[:], [Sst.b], [Sbf[c].b])
                            for hh in range(2):
                                ps = psbank()
                                for h4 in range(4):
                                    hd = hh * 4 + h4
                                    P.mm(ps[:, h4 * 128:(h4 + 1) * 128], ktb[c * 64:(c + 1) * 64, hd * 128:(hd + 1) * 128],
                                         vbf[c * 64:(c + 1) * 64, blk, hd * 128:(hd + 1) * 128], True, True, [ktb.b, vbf.b], [ps.b])
                                for h4 in range(4):
                                    hd = hh * 4 + h4
                                    P.stt(Sst[:, hd * 128:(hd + 1) * 128], Sst[:, hd * 128:(hd + 1) * 128], dec[:, 2 * hd + c:2 * hd + c + 1], ps[:, h4 * 128:(h4 + 1) * 128],
                                          ALU.mult, ALU.add, [Sst.b, dec.b, ps.b], [Sst.b])
                        stop("stopD2")
                        if own:
                            ob = sbi
                            pso = [PS[6], PS[7]]
                            for hd in range(8):
                                pss = psbank()
                                P.mm(pss[:, 0:128], khT[:, hd * 128:(hd + 1) * 128], qT0[:, hd * 128:(hd + 1) * 128], True, False, [khT.b, qT0.b], [pss.b])
                                P.mm(pss[:, 0:128], khT[:, hd * 128:(hd + 1) * 128], qT1[:, hd * 128:(hd + 1) * 128], False, True, [khT.b, qT1.b], [pss.b])
                                pt = pT[hd % 2]
                                P.tt("dve", pt[:], pss[:, 0:128], MBD, ALU.mult, [pss.b, cstf.b], [pt.b])
                                po = pso[hd // 4]
                                oo = po[:, (hd % 4) * 128:(hd % 4 + 1) * 128]
                                P.mm(oo, pt[:], vbf[:, blk, hd * 128:(hd + 1) * 128], True, False, [pt.b, vbf.b], [po.b])
                                P.mm(oo, qT0[:, hd * 128:(hd + 1) * 128], Sbf[0][:, hd * 128:(hd + 1) * 128], False, False, [qT0.b, Sbf[0].b], [po.b])
                                P.mm(oo, qT1[:, hd * 128:(hd + 1) * 128], Sbf[1][:, hd * 128:(hd + 1) * 128], False, True, [qT1.b, Sbf[1].b], [po.b])
                            stop("stopD3c")
                            for hd in range(8):
                                po = pso[hd // 4]
                                P.act(junk[:, 0:128], po[:, (hd % 4) * 128:(hd % 4 + 1) * 128], AF.Square, [po.b], [junk.b, ssq.b],
                                      accum_out=ssq[:, hd:hd + 1])
                            stop("stopD3d")
                            rms_rstd(ssq[:], rsq[:], 8, 1.0 / 128, [ssq.b], [rsq.b])
                            P.tt("dve", GS[:], gs[:], GR[:], ALU.mult, [gs.b, GR.b], [GS.b])
                            for hd in range(8):
                                po = pso[hd // 4]
                                P.stt(recb[:, hd * 128:(hd + 1) * 128], po[:, (hd % 4) * 128:(hd % 4 + 1) * 128], rsq[:, hd:hd + 1],
                                      GS[:, hd * 128:(hd + 1) * 128], ALU.mult, ALU.mult, [po.b, rsq.b, GS.b], [recb.b])
                            if sbi == 0:
                                dump("rec0", recb, [128, 1024], BF16)
                            stop("stopD4")
                            psr = psbank(); pr = bfv(psr)
                            for hd in range(8):
                                P.tr(pr[:, hd * 128:(hd + 1) * 128], recb[:, hd * 128:(hd + 1) * 128], ident, [recb.b, cstb.b], [psr.b])
                            P.copy("act", recT[:], pr[:, :], [psr.b], [recT.b])
                            P.dma(mixT_d[0:1024, ob * 128:(ob + 1) * 128].rearrange("(h p) t -> p h t", p=128), recT[:].rearrange("p (h t) -> p h t", h=8), [recT.b], [MIXD], semb=MIXD)
                            psa = psbank(); pa = bfv(psa)
                            for hd in range(8):
                                P.tr(pa[:, hd * 128:(hd + 1) * 128], aqt[:, hd * 128:(hd + 1) * 128], ident, [aqt.b, cstb.b], [psa.b])
                            P.copy("act", aqT[:], pa[:, :], [psa.b], [aqT.b])
                            P.dma(aqT_d[:, :, ob * 128:(ob + 1) * 128].rearrange("h p t -> p h t"), aqT[:].rearrange("p (h t) -> p h t", h=8), [aqT.b], [AQD], semb=AQD)
                            P.act(aw[:], iwt[:], AF.Abs, [iwt.b], [aw.b], scale=64.0 ** -0.5 * 8.0 ** -0.5)
                            P.ts("dve", sgn[:], iwt[:], 0.0, 2.0, ALU.is_ge, ALU.mult, [iwt.b], [sgn.b])
                            P.ts("dve", sgn[:], sgn[:], -1.0, None, ALU.add, None, [sgn.b], [sgn.b])
                            for h in range(8):
                                P.ts("pool", iqs[:, h * 64:(h + 1) * 64], iqt[:, h * 64:(h + 1) * 64], aw[:, h:h + 1], None, ALU.mult, None,
                                     [iqt.b, aw.b], [iqs.b])
                            psi = psbank(); pi = bfv(psi)
                            for c4 in range(4):
                                P.tr(pi[:, c4 * 128:(c4 + 1) * 128], iqs[:, c4 * 128:(c4 + 1) * 128], ident, [iqs.b, cstb.b], [psi.b])
                            P.copy("act", iqT[:], pi[:, 0:512], [psi.b], [iqT.b])
                            P.dma(iqT_d[:, :, ob * 128:(ob + 1) * 128].rearrange("h p t -> p h t"), iqT[:].rearrange("p (h t) -> p h t", h=4), [iqT.b], [IQD], semb=IQD)
                            P.dma(sgn_d[ob * 128:(ob + 1) * 128, :], sgn[:], [sgn.b], [SGD], semb=SGD)
                    stop("stopD")
                    pst = [psbank(), psbank()]
                    for blk in range(4):
                        for w3 in range(3):
                            idx = blk * 3 + w3
                            pv = bfv(pst[idx // 8])
                            src = kvt[:, blk, w3 * 128:(w3 + 1) * 128] if w3 < 2 else ikt[:, blk, :]
                            P.tr(pv[:, (idx % 8) * 128:(idx % 8 + 1) * 128], src, ident, [kvt.b, ikt.b, cstb.b], [pst[idx // 8].b])
                    P.copy("act", trs[:, 0:1024], bfv(pst[0])[:, :], [pst[0].b], [trs.b])
                    P.copy("dve", trs[:, 1024:1536], bfv(pst[1])[:, 0:512], [pst[1].b], [trs.b])
                    trv = trs[:].rearrange("p (b w t) -> p b w t", w=3, t=128)
                    t0 = sbi * 512
                    for kv in range(2):
                        P.dma(KT_d[kv, :, t0:t0 + 512].rearrange("p (b t) -> p b t", b=4), trv[:, :, kv, :], [trs.b], [KTD], semb=KTD)
                    P.dma(kiT_d[:, t0:t0 + 512].rearrange("p (b t) -> p b t", b=4), trv[:, :, 2, :], [trs.b], [KID], semb=KID)
                    P.dma(V_d[t0:t0 + 512, :].rearrange("(b p) c -> p b c", p=128), kvt[:, :, 256:512], [kvt.b], [VD], semb=VD)
                dump("Sst", Sst, [128, 1024])

            stop("stop1")

            psmod[0] = 3
            NIT = 22
            P.set_barrier()
            with contextlib.ExitStack() as es2:
                def sb2(name, shape, dt=F32):
                    t = T.__new__(T)
                    t.t = es2.enter_context(nc.sbuf_tensor(name, shape, dt))
                    t.b = P.mkbuf(name)
                    return t
                KT = sb2("KT", [128, 2 * S], BF16)
                kiT = sb2("kiT", [128, S], BF16)
                Vaug = sb2("Vaug", [128, NB * 2 * 132], BF16)
                scoreL = [sb2("score%d" % k, [128, S]) for k in range(2)]
                nbiasL = [sb2("nbias%d" % k, [128, S], BF16) for k in range(2)]
                rbuf = [sb2("rbuf%d" % i, [128, 512], BF16) for i in range(2)]
                pbuf = [sb2("pbuf%d" % i, [128, 512], BF16) for i in range(2)]
                aqTiL = [sb2("aqTi%d" % k, [128, 1024], BF16) for k in range(2)]
                iqTi = sb2("iqTi", [128, 512], BF16)
                sgni = sb2("sgni", [128, 8])
                Dh = sb2("Dh", [128, 1024], BF16)
                CBf = sb2("CBf", [128, 512], BF16)
                PBt = sb2("PBt", [128, 512], BF16)
                BIGI4 = sb2("BIGI4", [128, 512], BF16)
                GA = sb2("GA", [128, 1024])
                nv = sb2("nv", [128, NO])
                smL = [sb2("sm%d" % k, [128, 16]) for k in range(2)]
                cjL = [sb2("cj%d" % k, [128, 8], BF16) for k in range(2)]
                sm2 = sb2("sm2", [128, 8]); sm3 = sb2("sm3", [128, 8]); sm4 = sb2("sm4", [128, 8])
                junk2 = sb2("junk2", [128, 128], BF16)
                attb = sb2("attb", [128, 1024], BF16)
                attT = sb2("attT", [128, 1024], BF16)
                P.dma(KT[:, 0:S], KT_d[0], [KTD], [KT.b])
                P.dma(KT[:, S:2 * S], KT_d[1], [KTD], [KT.b])
                P.dma(kiT[:], kiT_d[:, :], [KID], [kiT.b])
                P.emit("pool", lambda: nc.gpsimd.memset(Vaug[:], 1.0), [], [Vaug.b])
                Vv = Vaug[:].rearrange("p (b k c) -> p b k c", k=2, c=132)
                V_dv = V_d.rearrange("(b p) c -> p b c", p=128)
                for b0 in range(0, NB, 16):
                    b1 = min(NB, b0 + 16)
                    for kv in range(2):
                        P.dma(Vv[:, b0:b1, kv, 0:128], V_dv[:, b0:b1, kv * 128:(kv + 1) * 128], [VD], [Vaug.b])
                P.emit("pool", lambda: nc.gpsimd.memset(CBf[:], 0.0), [], [CBf.b])
                P.copy("pool", CBf[:, 384:512], CB, [cstf.b], [CBf.b])
                P.dma(scoreL[0][:, 0:512], padb[0:1, :].partition_broadcast(128), [], [scoreL[0].b])
                P.copy("pool", PBt[:], scoreL[0][:, 0:512], [scoreL[0].b], [PBt.b])
                for r4 in range(4):
                    P.ts("dve", BIGI4[:, r4 * 128:(r4 + 1) * 128], ident_f, 29952.0, None, ALU.mult, None, [cstf.b], [BIGI4.b])
                P.dma(GA[:], g_att[0:1, :].partition_broadcast(128), [], [GA.b])
                P.dma(nv[:], nvalT[:, :], [], [nv.b])
                acc = [PS[3], PS[4], PS[5]]
                def stageA(i):
                    score = scoreL[i % 2]; nbias = nbiasL[i % 2]; cj = cjL[i % 2]; aqTi = aqTiL[i % 2]; sm = smL[i % 2]
                    LO, HI, TH, CNT, GE, DD, NGE, SEL, AA, BB, THF = [sm[:, k:k + 1] for k in range(11)]
                    SMB = [sm.b]
                    nkt = i + 1
                    n = nkt * 512
                    nk = 4 * (i + 1)
                    P.dma(aqTi[:].rearrange("p (h t) -> p h t", h=8), aqT_d[:, :, i * 128:(i + 1) * 128].rearrange("h p t -> p h t"), [AQD], [aqTi.b])
                    P.dma(iqTi[:].rearrange("p (h t) -> p h t", h=4), iqT_d[:, :, i * 128:(i + 1) * 128].rearrange("h p t -> p h t"), [IQD], [iqTi.b])
                    P.dma(sgni[:], sgn_d[i * 128:(i + 1) * 128, :], [SGD], [sgni.b])
                    for h in range(8):
                        P.ts("dve", Dh[:, h * 128:(h + 1) * 128], ident_f, sgni[:, h:h + 1], None, ALU.mult, None, [cstf.b, sgni.b], [Dh.b])
                    for kt in range(nkt):
                        psc = PS[6 + kt % 2]
                        for h in range(8):
                            ps = psbank()
                            pb = (h % 2) * 64
                            P.mm(ps[:, :], iqTi[pb:pb + 64, (h // 2) * 128:(h // 2 + 1) * 128], kiT[pb:pb + 64, kt * 512:(kt + 1) * 512],
                                 True, True, [iqTi.b, kiT.b], [ps.b])
                            rb = rbuf[h % 2]
                            P.act(rb[:], ps[:, :], AF.Relu, [ps.b], [rb.b])
                            P.mm(psc[:, :], Dh[:, h * 128:(h + 1) * 128], rb[:], h == 0, h == 7, [Dh.b, rb.b], [psc.b])
                        dst = score[:, kt * 512:(kt + 1) * 512]
                        if kt == nkt - 1:
                            P.tt("dve", dst, psc[:, :], CBf[:], ALU.add, [psc.b, CBf.b], [score.b])
                            if kt == 0:
                                P.tt("dve", dst, dst, PBt[:], ALU.add, [score.b, PBt.b], [score.b])
                        elif kt == 0:
                            P.tt("dve", dst, psc[:, :], PBt[:], ALU.add, [psc.b, PBt.b], [score.b])
                        else:
                            P.copy("act", dst, psc[:, :], [psc.b], [score.b])
                    sc = score[:, 0:n]
                    P.emit("dve", lambda sc=sc: nc.vector.reduce_max(out=HI, in_=sc, axis=AX.X), [score.b], SMB)
                    P.ts("dve", LO, HI, -64.0, None, ALU.add, None, SMB, SMB)
                    for it in range(NIT):
                        cw = 64.0 / (2.0 ** (it + 1))
                        P.ts("dve", TH, LO, cw, None, ALU.add, None, SMB, SMB)
                        P.ts("dve", cj[:, 0:1].to_broadcast([128, n]), sc, TH, 0.0, ALU.is_ge, ALU.add, [score.b] + SMB, [cj.b] + SMB, accum_out=CNT)
                        P.ts("dve", GE, CNT, float(KSEL), cw, ALU.is_ge, ALU.mult, SMB, SMB)
                        P.tt("dve", LO, LO, GE, ALU.add, SMB, SMB)
                    P.ts("dve", SEL, nv[:, i:i + 1], float(KSEL), None, ALU.is_gt, None, [nv.b], SMB)
                    P.ts("dve", AA, SEL, 1e20, -1e20, ALU.mult, ALU.add, SMB, SMB)
                    P.tt("dve", BB, LO, SEL, ALU.mult, SMB, SMB)
                    P.tt("dve", THF, AA, BB, ALU.add, SMB, SMB)
                    P.ts("dve", nbias[:, 0:n], sc, THF, 1.0, ALU.is_ge, ALU.subtract, [score.b] + SMB, [nbias.b])
                def stageB(i):
                    nbias = nbiasL[i % 2]; aqTi = aqTiL[i % 2]
                    nk = 4 * (i + 1)
                    for a in acc:
                        P.emit("dve", lambda a=a: nc.vector.memset(a[:, :], 0.0), [], [a.b])
                    for kb in range(nk):
                        for kvh in range(2):
                            psl = psbank()
                            P.mm(psl[:, :], KT[:, kvh * S + kb * 128:kvh * S + (kb + 1) * 128], aqTi[:, kvh * 512:(kvh + 1) * 512],
                                 True, False, [KT.b, aqTi.b], [psl.b])
                            P.mm(psl[:, :], nbias[:, kb * 128:(kb + 1) * 128], BIGI4[:], False, True, [nbias.b, BIGI4.b], [psl.b])
                            pb_ = pbuf[(kb * 2 + kvh) % 2]
                            P.act(pb_[:], psl[:, :], AF.Exp, [psl.b], [pb_.b], scale=128.0 ** -0.5)
                            vo = (kb * 2 + kvh) * 132
                            for g in range(4):
                                hd = kvh * 4 + g
                                a = acc[hd // 3]
                                off = (hd % 3) * 132
                                P.mm(a[:, off:off + 129], pb_[:, g * 128:(g + 1) * 128], Vaug[:, vo:vo + 129], False, False,
                                     [pb_.b, Vaug.b], [a.b], inc=(g == 3), skip_group_check=True)
                    for hd in range(8):
                        a = acc[hd // 3]
                        off = (hd % 3) * 132
                        P.emit("dve", lambda a=a, off=off, hd=hd: nc.vector.reciprocal(out=sm2[:, hd:hd + 1], in_=a[:, off + 128:off + 129]), [a.b], [sm2.b])
                        P.act(junk2[:], a[:, off:off + 128], AF.Square, [a.b, sm2.b], [junk2.b, sm3.b], scale=sm2[:, hd:hd + 1],
                              accum_out=sm3[:, hd:hd + 1])
                    rms_rstd(sm3[:], sm4[:], 8, 1.0 / 128, [sm3.b], [sm4.b])
                    P.tt("dve", sm4[:], sm4[:], sm2[:], ALU.mult, [sm4.b, sm2.b], [sm4.b])
                    for hd in range(8):
                        a = acc[hd // 3]
                        off = (hd % 3) * 132
                        P.stt(attb[:, hd * 128:(hd + 1) * 128], a[:, off:off + 128], sm4[:, hd:hd + 1], GA[:, hd * 128:(hd + 1) * 128],
                              ALU.mult, ALU.mult, [a.b, sm4.b, GA.b], [attb.b])
                    if i == 0:
                        dump("att0", attb, [128, 1024], BF16)
                    if i == 1:
                        dump("att1", attb, [128, 1024], BF16)
                    psr = psbank(); pr = bfv(psr)
                    for hd in range(8):
                        P.tr(pr[:, hd * 128:(hd + 1) * 128], attb[:, hd * 128:(hd + 1) * 128], ident, [attb.b, cstb.b], [psr.b])
                    P.copy("act", attT[:], pr[:, :], [psr.b], [attT.b])
                    P.dma(mixT_d[1024:2048, i * 128:(i + 1) * 128].rearrange("(h p) t -> p h t", p=128), attT[:].rearrange("p (h t) -> p h t", h=8),
                          [attT.b], [MIXD], semb=MIXD)
                stageA(0)
                for i in range(NO):
                    if i + 1 < NO:
                        stageA(i + 1)
                    stageB(i)
            psmod[0] = 6
            stop("stop2")

            h2T_d = dscr("h2T_d", [D, TO], BF16)
            H2D = Buf("h2T_d"); X1D = Buf("x1_d"); OUTD = Buf("out")
            P.set_barrier()
            with contextlib.ExitStack() as es34:
                def sb34(name, shape, dt=F32, st=es34):
                    t = T.__new__(T)
                    t.t = st.enter_context(nc.sbuf_tensor(name, shape, dt))
                    t.b = P.mkbuf(name)
                    return t
                comb = sb34("comb", [128, NO * 32])
                with contextlib.ExitStack() as es3:
                    sb3 = lambda name, shape, dt=F32: sb34(name, shape, dt, es3)
                    Wo = sb3("Wo", [128, KC * 2048], BF16)
                    wst3 = [sb3("wst3_%d" % i, [128, 2048]) for i in range(2)]
                    for kc in range(KC):
                        w = wst3[kc % 2]
                        P.dma(w[:], w_out[kc * 128:(kc + 1) * 128, :], [], [w.b])
                        P.copy("act" if kc % 2 == 0 else "pool", Wo[:, kc * 2048:(kc + 1) * 2048], w[:], [w.b], [Wo.b])
                    GT1b = sb3("GT1b", [128, 2048])
                    mod6 = mod_d.rearrange("(a n) -> a n", a=6)
                    P.dma(GT1b[:], mod6[2:3, :].partition_broadcast(128), [modD], [GT1b.b])
                    Wrt = sb3("Wrt", [128, KC * 36]); Wrtb = sb3("Wrtb", [128, KC * 36], BF16)
                    P.dma(Wrt[:].rearrange("p (k c) -> p k c", c=36), w_rt.rearrange("(k p) c -> p k c", p=128), [], [Wrt.b])
                    P.copy("dve", Wrtb[:], Wrt[:], [Wrt.b], [Wrtb.b])
                    brt = sb3("brt", [128, 36])
                    P.dma(brt[:], b_rt[0:1, :].partition_broadcast(128), [], [brt.b])
                    xo = [sb3("xo%d" % i, [128, D]) for i in range(2)]
                    x1t = [sb3("x1t%d" % i, [128, D]) for i in range(2)]
                    xn2 = sb3("xn2", [128, D], BF16)
                    mixTi = sb3("mixTi", [128, KC * 128], BF16)
                    h2Tb = sb3("h2Tb", [128, KC * 128], BF16)
                    st3 = sb3("st3", [128, 8])
                    lg = sb3("lg", [128, 36])
                    rs = sb3("rs", [128, 64])
                    for i in range(NO):
                        x_ = xo[i % 2]; x1 = x1t[i % 2]
                        p = 4 * i + 3
                        P.dma(x_[:], x_sh[p * 128:(p + 1) * 128, :], [], [x_.b])
                        P.dma(mixTi[:].rearrange("p (k t) -> p k t", t=128), mixT_d[:, i * 128:(i + 1) * 128].rearrange("(k p) t -> p k t", p=128),
                              [MIXD], [mixTi.b])
                        for dt in range(4):
                            ps = psbank()
                            for kc in range(KC):
                                P.mm(ps[:, :], mixTi[:, kc * 128:(kc + 1) * 128], Wo[:, kc * 2048 + dt * 512:kc * 2048 + (dt + 1) * 512],
                                     kc == 0, kc == KC - 1, [mixTi.b, Wo.b], [ps.b])
                            P.tt("dve", x1[:, dt * 512:(dt + 1) * 512], ps[:, :], GT1b[:, dt * 512:(dt + 1) * 512], ALU.mult, [ps.b, GT1b.b], [x1.b])
                        P.tt("dve", x1[:], x1[:], x_[:], ALU.add, [x1.b, x_.b], [x1.b])
                        if i == 0:
                            dump("x1", x1, [128, D])
                        P.dma(x1_d[i * 128:(i + 1) * 128, :], x1[:], [x1.b], [X1D], semb=X1D)
                        P.act(xn2[:], x1[:], AF.Square, [x1.b], [xn2.b, st3.b], accum_out=st3[:, 0:1])
                        rms_rstd(st3[:, 0:1], st3[:, 1:2], 1, 1.0 / D, [st3.b], [st3.b])
                        P.ts("dve", xn2[:], x1[:], st3[:, 1:2], None, ALU.mult, None, [x1.b, st3.b], [xn2.b])
                        for half in range(2):
                            ps = psbank()
                            pv = bfv(ps)
                            for k8 in range(8):
                                kc = half * 8 + k8
                                P.tr(pv[:, k8 * 128:(k8 + 1) * 128], xn2[:, kc * 128:(kc + 1) * 128], ident, [xn2.b, cstb.b], [ps.b])
                            for k8 in range(8):
                                kc = half * 8 + k8
                                o = h2Tb[:, kc * 128:(kc + 1) * 128]
                                i_ = pv[:, k8 * 128:(k8 + 1) * 128]
                                if k8 % 2 == 0:
                                    P.act(o, i_, AF.Identity, [ps.b, G2s.b, modT.b], [h2Tb.b], scale=G2s[:, kc:kc + 1], bias=SH2s[:, kc:kc + 1])
                                else:
                                    P.ts("dve", o, i_, G2s[:, kc:kc + 1], SH2s[:, kc:kc + 1], ALU.mult, ALU.add, [ps.b, G2s.b, modT.b], [h2Tb.b])
                        P.dma(h2T_d[:, i * 128:(i + 1) * 128].rearrange("(k p) t -> p k t", p=128), h2Tb[:].rearrange("p (k t) -> p k t", t=128),
                              [h2Tb.b], [H2D], semb=H2D)
                        psr = psbank()
                        for kc in range(KC):
                            P.mm(psr[:, 0:36], h2Tb[:, kc * 128:(kc + 1) * 128], Wrtb[:, kc * 36:(kc + 1) * 36], kc == 0, kc == KC - 1,
                                 [h2Tb.b, Wrtb.b], [psr.b])
                        P.tt("dve", lg[:], psr[:, 0:36], brt[:], ALU.add, [psr.b, brt.b], [lg.b])
                        RB = [rs.b]
                        GMAX, NGM, SE, PG, M1, M2, DLT, EX, DEN, W1, W2 = [rs[:, k:k + 1] for k in range(11)]
                        OHG = rs[:, 12:16]; ESEL = rs[:, 16:24]; MK1 = rs[:, 24:32]; E2 = rs[:, 32:40]; MK2 = rs[:, 40:48]; CIG = rs[:, 48:56]; EG = rs[:, 56:60]
                        P.emit("dve", lambda: nc.vector.reduce_max(out=GMAX, in_=lg[:, 0:4], axis=AX.X), [lg.b], RB)
                        P.ts("dve", NGM, GMAX, -1.0, None, ALU.mult, None, RB, RB)
                        P.act(EG, lg[:, 0:4], AF.Exp, [lg.b] + RB, RB, bias=NGM, accum_out=SE)
                        P.emit("dve", lambda: nc.vector.reciprocal(out=PG, in_=SE), RB, RB)
                        P.ts("dve", OHG, lg[:, 0:4], GMAX, None, ALU.is_ge, None, [lg.b] + RB, RB)
                        P.ts("dve", ESEL, lg[:, 4:12], rs[:, 12:13], None, ALU.mult, None, [lg.b] + RB, RB)
                        for g in range(1, 4):
                            P.stt(ESEL, lg[:, 4 + 8 * g:12 + 8 * g], rs[:, 12 + g:13 + g], ESEL, ALU.mult, ALU.add, [lg.b] + RB, RB)
                        P.emit("dve", lambda: nc.vector.reduce_max(out=M1, in_=ESEL, axis=AX.X), RB, RB)
                        P.ts("dve", MK1, ESEL, M1, None, ALU.is_ge, None, RB, RB)
                        P.stt(E2, MK1, -1e30, ESEL, ALU.mult, ALU.add, RB, RB)
                        P.emit("dve", lambda: nc.vector.reduce_max(out=M2, in_=E2, axis=AX.X), RB, RB)
                        P.ts("dve", MK2, E2, M2, None, ALU.is_ge, None, RB, RB)
                        P.tt("dve", DLT, M2, M1, ALU.subtract, RB, RB)
                        P.act(EX, DLT, AF.Exp, RB, RB)
                        P.ts("dve", DEN, EX, 1.0, None, ALU.add, None, RB, RB)
                        P.emit("dve", lambda: nc.vector.reciprocal(out=DEN, in_=DEN), RB, RB)
                        P.tt("dve", W1, DEN, PG, ALU.mult, RB, RB)
                        P.tt("dve", W2, W1, EX, ALU.mult, RB, RB)
                        P.ts("dve", CIG, MK1, W1, None, ALU.mult, None, RB, RB)
                        P.stt(CIG, MK2, W2, CIG, ALU.mult, ALU.add, RB, RB)
                        for g in range(4):
                            P.ts("dve", comb[:, i * 32 + g * 8:i * 32 + (g + 1) * 8], CIG, rs[:, 12 + g:13 + g], None, ALU.mult, None, RB, [comb.b])
                stop("stop3")
                P.set_barrier()
                with contextlib.ExitStack() as es4:
                    sb4 = lambda name, shape, dt=F32: sb34(name, shape, dt, es4)
                    CH = min(TO, 1024)
                    NCH = TO // CH
                    NT = CH // 128
                    SUB = min(512, CH)
                    h2c = sb4("h2c", [128, KC * CH], BF16)
                    yacc = sb4("yacc", [128, NT * 2048])
                    WGb = sb4("WGb", [128, KC * 512], BF16); WUb = sb4("WUb", [128, KC * 512], BF16); WDb = sb4("WDb", [128, 4 * 2048], BF16)
                    stg = [sb4("stg%d" % k, [128, 2048]) for k in range(4)]
                    sgt = [sb4("sgt%d" % k, [128, 512]) for k in range(2)]
                    heT = [sb4("heT%d" % k, [128, 4 * 512], BF16) for k in range(2)]
                    xf = sb4("xf", [128, 2048])
                    st4 = sb4("st4", [128, 8])
                    srot = [0]

                    def load_cast(dst_ap, dstb, src_ap, three=None):
                        w = stg[srot[0] % 4]
                        eng = "act" if srot[0] % 4 != 3 else "dve"
                        srot[0] += 1
                        if three is None:
                            P.dma(w[:], src_ap, [], [w.b])
                        else:
                            P.dma(w[:].rearrange("p (k c) -> p k c", c=512), src_ap, [], [w.b])
                        P.copy(eng, dst_ap, w[:], [w.b], [dstb])

                    for ch in range(NCH):
                        P.dma(h2c[:].rearrange("p (k t) -> p k t", t=CH), h2T_d[:, ch * CH:(ch + 1) * CH].rearrange("(k p) t -> p k t", p=128),
                              [H2D], [h2c.b])
                        P.emit("pool", lambda: nc.gpsimd.memset(yacc[:], 0.0), [], [yacc.b])
                        for e in range(NE):
                            for q in range(4):
                                load_cast(WGb[:, q * 2048:(q + 1) * 2048], WGb.b, w_eg[e % ne_decl, q * 512:(q + 1) * 512, :].rearrange("(k p) c -> p k c", p=128), 1)
                            for q in range(4):
                                load_cast(WUb[:, q * 2048:(q + 1) * 2048], WUb.b, w_eu[e % ne_decl, q * 512:(q + 1) * 512, :].rearrange("(k p) c -> p k c", p=128), 1)
                            for c in range(4):
                                load_cast(WDb[:, c * 2048:(c + 1) * 2048], WDb.b, w_ed[e % ne_decl, c * 128:(c + 1) * 128, :])
                            for st_ in range(CH // SUB):
                                tok0 = st_ * SUB
                                he = heT[st_ % 2]
                                for c in range(4):
                                    psg = psbank(); psu = psbank()
                                    for kc in range(KC):
                                        P.mm(psg[:, 0:SUB], WGb[:, kc * 512 + c * 128:kc * 512 + (c + 1) * 128], h2c[:, kc * CH + tok0:kc * CH + tok0 + SUB],
                                             kc == 0, kc == KC - 1, [WGb.b, h2c.b], [psg.b])
                                    for kc in range(KC):
                                        P.mm(psu[:, 0:SUB], WUb[:, kc * 512 + c * 128:kc * 512 + (c + 1) * 128], h2c[:, kc * CH + tok0:kc * CH + tok0 + SUB],
                                             kc == 0, kc == KC - 1, [WUb.b, h2c.b], [psu.b])
                                    sg = sgt[c % 2]
                                    P.act(sg[:, 0:SUB], psg[:, 0:SUB], AF.Silu, [psg.b], [sg.b])
                                    P.tt("dve", he[:, c * 512:c * 512 + SUB], sg[:, 0:SUB], psu[:, 0:SUB], ALU.mult, [sg.b, psu.b], [he.b])
                                for t_ in range(SUB // 128):
                                    tile = st_ * (SUB // 128) + t_
                                    gt = ch * NT + tile
                                    for dt in range(4):
                                        psd = psbank()
                                        for c in range(4):
                                            P.mm(psd[:, :], he[:, c * 512 + t_ * 128:c * 512 + (t_ + 1) * 128], WDb[:, c * 2048 + dt * 512:c * 2048 + (dt + 1) * 512],
                                                 c == 0, c == 3, [he.b, WDb.b], [psd.b])
                                        ya = yacc[:, tile * 2048 + dt * 512:tile * 2048 + (dt + 1) * 512]
                                        P.stt(ya, psd[:, :], comb[:, gt * 32 + e:gt * 32 + e + 1], ya, ALU.mult, ALU.add, [psd.b, comb.b, yacc.b], [yacc.b])
                        GT2b = stg[0]; GFb = stg[1]
                        P.dma(GT2b[:], mod6[5:6, :].partition_broadcast(128), [modD], [GT2b.b])
                        P.dma(GFb[:], g_fin[0:1, :].partition_broadcast(128), [], [GFb.b])
                        for tile in range(NT):
                            gt = ch * NT + tile
                            P.dma(xf[:], x1_d[gt * 128:(gt + 1) * 128, :], [X1D], [xf.b])
                            ya = yacc[:, tile * 2048:(tile + 1) * 2048]
                            P.tt("dve", ya, ya, GT2b[:], ALU.mult, [yacc.b, GT2b.b], [yacc.b])
                            P.tt("dve", xf[:], xf[:], ya, ALU.add, [xf.b, yacc.b], [xf.b])
                            P.act(ya, xf[:], AF.Square, [xf.b], [yacc.b, st4.b], accum_out=st4[:, 0:1])
                            rms_rstd(st4[:, 0:1], st4[:, 1:2], 1, 1.0 / D, [st4.b], [st4.b])
                            P.stt(xf[:], xf[:], st4[:, 1:2], GFb[:], ALU.mult, ALU.mult, [xf.b, st4.b, GFb.b], [xf.b])
                            P.dma(out_d[gt * 128:(gt + 1) * 128, :], xf[:], [xf.b], [OUTD], semb=OUTD)


        try:
            body()
        except _Stop:
            pass
        for b_ in P.dmabufs:
            P.final.append((b_.sem, b_.cnt))
        P.replay()
    return nc, dbg_out


def host_consts():
    c = np.zeros((128, 1024), np.float32)
    i = np.arange(128)
    c[:, 0:128] = np.eye(128)
    same = (i[:, None] // 64) == (i[None, :] // 64)
    c[:, 128:256] = (same & (i[:, None] <= i[None, :])).astype(np.float32)
    c[:, 256:384] = (same & (i[:, None] > i[None, :])).astype(np.float32)
    c[:, 384] = (i < 64)
    c[:, 385] = (i >= 64)
    c[:, 512:640] = np.where(i[None, :] <= i[:, None], 0.0, -1e30)
    c[:, 640:768] = np.eye(128)
    return c


def make_in_maps(inputs, S, ne=NE):
    f = lambda a: np.ascontiguousarray(np.asarray(a, dtype=np.float32))
    x = f(inputs["x"]); c = f(inputs["c"])
    NB = S // 128; NO = NB // 4
    pT = lambda v: np.ascontiguousarray(v.reshape(-1, 128).T)
    shared = {
        "w_ada": f(inputs["w_ada"][0]), "badaT": pT(f(inputs["b_ada"][0])),
        "gnmT": pT(f(inputs["g_norm_mix"][0])), "gnfT": pT(f(inputs["g_norm_ffn"][0])),
        "w_in": f(inputs["w_in"][0]), "lbl": f(inputs["lb_logits"]),
        "g_rec": f(inputs["g_rec_out"]), "g_att": f(inputs["g_att_out"]),
        "w_out": f(inputs["w_out"][0]),
        "w_rt": np.ascontiguousarray(np.concatenate([f(inputs["w_router_group"][0]), f(inputs["w_router_expert"][0])], axis=1)),
        "b_rt": np.ascontiguousarray(np.concatenate([f(inputs["b_router_group"][0]), f(inputs["b_router_expert"][0])])[None, :]),
        "w_eg": f(inputs["w_expert_gate"][0][:ne]), "w_eu": f(inputs["w_expert_up"][0][:ne]), "w_ed": f(inputs["w_expert_down"][0][:ne]),
        "g_fin": f(inputs["g_final"])[None, :], "cst": host_consts(),
    }
    maps = []
    for core in range(8):
        b, j = core // 4, core % 4
        npad = (3 - j) * 128
        xs = np.zeros((S, D), np.float32)
        xs[npad:] = x[b, :S - npad]
        valid = np.ones(S, np.float32); valid[:npad] = 0
        padb = np.zeros((1, 512), np.float32); padb[0, :npad] = -1e30
        nval = np.zeros((128, NO), np.float32)
        for i in range(NO):
            nval[:, i] = (4 * i + 3) * 128 + np.arange(128) - npad + 1
        m = dict(shared)
        m.update({"x_sh": xs, "cT": pT(c[b]), "validT": pT(valid), "padb": padb, "nvalT": nval})
        maps.append(m)
    return maps


def assemble(results, S, B=2):
    NB = S // 128; NO = NB // 4
    out = np.zeros((B, S, D), np.float32)
    for core in range(8):
        b, j = core // 4, core % 4
        o = np.asarray(results[core]["out"]).reshape(NO, 128, D)
        for i in range(NO):
            g = 4 * i + j
            out[b, g * 128:(g + 1) * 128] = o[i]
    return out


_CACHE = {}


def kernel(**inputs):
    S = int(np.asarray(inputs["x"]).shape[1])
    if S not in _CACHE:
        _CACHE[S] = build(S)[0]
    nc = _CACHE[S]
    maps = make_in_maps(inputs, S)
    res = run_bass_kernel_spmd(nc, maps, core_ids=list(range(8)))
    return assemble(res.results, S)
```

```python
import contextlib
import numpy as np
import ml_dtypes
import concourse.bass as bass
import concourse.mybir as mybir
from concourse.bass_utils import run_bass_kernel_spmd

F32 = mybir.dt.float32
BF16 = mybir.dt.bfloat16
AF = mybir.ActivationFunctionType
ALU = mybir.AluOpType
AX = mybir.AxisListType

D = 2048
KC = 16
IN_COLS = 6216
NE = 32
DE = 512
EPS = 1e-6
NEG = -30000.0
SAME_ENGINE_SYNC = True
C_RQ, C_RF, C_RI, C_RG, C_AQ, C_AK, C_AV, C_IQ, C_IK, C_IW = 0, 1024, 2048, 3072, 4096, 5120, 5376, 5632, 6144, 6208


class Buf:
    __slots__ = ("name", "w", "r", "sem", "cnt")

    def __init__(self, name):
        self.name = name
        self.w = None
        self.r = []
        self.sem = None
        self.cnt = 0


class Prog:
    ENGS = ("pe", "act", "dve", "pool", "sp")

    def __init__(self, nc, es):
        self.nc = nc
        self.es = es
        self.q = {e: [] for e in self.ENGS}
        self.cnt = {e: 0 for e in self.ENGS}
        self.esem = {e: es.enter_context(nc.semaphore("sem_" + e)) for e in ("pe", "act", "dve", "pool")}
        self.seen = {e: {} for e in self.ENGS}
        self.nsem = 0
        self.final = []
        self.dmabufs = []
        self.stopped = False
        self.barrier = []

    def set_barrier(self):
        toks = [(e, self.esem[e], self.cnt[e]) for e in ("pe", "act", "dve", "pool") if self.cnt[e] > 0]
        for b in self.dmabufs:
            toks.append(("d%d" % id(b), b.sem, b.cnt))
        self.barrier = toks

    def mkbuf(self, name):
        b = Buf(name)
        b.r = list(self.barrier)
        return b

    def newsem(self, name):
        self.nsem += 1
        return self.es.enter_context(self.nc.semaphore("d_%s_%d" % (name, self.nsem)))

    def _waits(self, eng, R, W):
        toks = []
        for b in R:
            if b.w is not None:
                toks.append(b.w)
        for b in W:
            if b.w is not None:
                toks.append(b.w)
            toks.extend(b.r)
        out = {}
        for (key, sem, v) in toks:
            if key == eng and (eng == "pe" or not SAME_ENGINE_SYNC):
                continue
            if self.seen[eng].get(key, 0) >= v:
                continue
            if out.get(key, (None, 0))[1] < v:
                out[key] = (sem, v)
        for key, (sem, v) in out.items():
            self.seen[eng][key] = v
        return list(out.values())

    def emit(self, eng, fn, R=(), W=(), inc=True):
        if self.stopped:
            return
        waits = self._waits(eng, R, W)
        sem = self.esem[eng]
        if inc:
            self.cnt[eng] += 1
            c = self.cnt[eng]
            self.q[eng].append((waits, fn, sem, 1))
        else:
            c = self.cnt[eng] + 1
            self.q[eng].append((waits, fn, sem, 0))
        tok = (eng, sem, c)
        for b in W:
            b.w = tok
            b.r = []
        for b in R:
            if b not in W:
                b.r.append(tok)

    def dma(self, out, in_, R, W, semb=None, q="sp"):
        semb = semb or W[0]
        if self.stopped:
            return (None, None, 0)
        if semb.sem is None:
            semb.sem = self.newsem(semb.name)
            self.dmabufs.append(semb)
        waits = self._waits(q, R, W)
        semb.cnt += 16
        nc = self.nc
        eng = {"sp": nc.sync, "act": nc.scalar, "pool": nc.gpsimd}[q]
        self.q[q].append((waits, lambda: eng.dma_start(out=out, in_=in_), semb.sem, 16))
        tok = ("d%d" % id(semb), semb.sem, semb.cnt)
        for b in W:
            b.w = tok
            b.r = []
        for b in R:
            b.r.append(tok)
        return tok

    def act(self, out, in_, func, R, W, **kw):
        nc = self.nc
        self.emit("act", lambda: nc.scalar.activation(out=out, in_=in_, func=func, **kw), R, W)

    def ts(self, eng, out, in0, s1, s2, op0, op1, R, W, **kw):
        e = self.nc.vector if eng == "dve" else self.nc.gpsimd
        if op1 is None:
            self.emit(eng, lambda: e.tensor_scalar(out=out, in0=in0, scalar1=s1, scalar2=None, op0=op0, **kw), R, W)
        else:
            self.emit(eng, lambda: e.tensor_scalar(out=out, in0=in0, scalar1=s1, scalar2=s2, op0=op0, op1=op1, **kw), R, W)

    def tt(self, eng, out, in0, in1, op, R, W):
        e = self.nc.vector if eng == "dve" else self.nc.gpsimd
        self.emit(eng, lambda: e.tensor_tensor(out=out, in0=in0, in1=in1, op=op), R, W)

    def stt(self, out, in0, scalar, in1, op0, op1, R, W):
        nc = self.nc
        self.emit("dve", lambda: nc.vector.scalar_tensor_tensor(out=out, in0=in0, scalar=scalar, in1=in1, op0=op0, op1=op1), R, W)

    def copy(self, eng, out, in_, R, W):
        nc = self.nc
        if eng == "act":
            self.emit("act", lambda: nc.scalar.copy(out=out, in_=in_), R, W)
        elif eng == "dve":
            self.emit("dve", lambda: nc.vector.tensor_copy(out=out, in_=in_), R, W)
        else:
            self.emit("pool", lambda: nc.gpsimd.tensor_copy(out=out, in_=in_), R, W)

    def mm(self, out, lhsT, rhs, start, stop, R, W, inc=None, **kw):
        nc = self.nc
        if inc is None:
            inc = bool(stop)
        self.emit("pe", lambda: nc.tensor.matmul(out, lhsT, rhs, start=start, stop=stop, **kw), R, W, inc=inc)

    def tr(self, out, in_, ident, R, W, inc=True):
        nc = self.nc
        self.emit("pe", lambda: nc.tensor.transpose(out, in_, ident), R, W, inc=inc)

    def replay(self):
        nc = self.nc
        engmap = {"pe": "tensor", "act": "scalar", "dve": "vector", "pool": "gpsimd", "sp": "sync"}
        with nc.Block() as blk:
            for e in self.ENGS:
                lst = self.q[e]
                final = self.final

                def body(eng, lst=lst, e=e):
                    for (waits, fn, sem, n) in lst:
                        for (s, v) in waits:
                            eng.wait_ge(s, v)
                        if n:
                            fn().then_inc(sem, n)
                        else:
                            fn()
                    if e == "sp":
                        for (s, v) in final:
                            eng.wait_ge(s, v)

                getattr(blk, engmap[e])(body)


class T:
    def __init__(self, P, kind, name, shape, dtype):
        nc = P.nc
        if kind == "sb":
            self.t = P.es.enter_context(nc.sbuf_tensor(name, shape, dtype))
        else:
            self.t = P.es.enter_context(nc.psum_tensor(name, shape, dtype))
        self.b = Buf(name)

    def __getitem__(self, k):
        return self.t[k]


def build(S, dbg=None):
    NB = S // 128
    NSB = NB // 4
    NO = NSB
    TO = NO * 128
    KSEL = min(256, S // 4)
    dbg = dbg or ()
    nc = bass.Bass("TRN2", target_bir_lowering=False)

    def din(name, shape, dt=F32):
        return nc.dram_tensor(name, list(shape), dt, kind="ExternalInput").ap()

    def dscr(name, shape, dt):
        return nc.dram_tensor(name, list(shape), dt, kind="Internal").ap()

    x_sh = din("x_sh", [S, D])
    cT = din("cT", [128, KC])
    w_ada = din("w_ada", [D, 6 * D])
    badaT = din("badaT", [128, 96])
    gnmT = din("gnmT", [128, KC])
    gnfT = din("gnfT", [128, KC])
    w_in = din("w_in", [D, IN_COLS])
    lbl = din("lbl", [2, 1024])
    g_rec = din("g_rec", [1, 1024])
    g_att = din("g_att", [1, 1024])
    w_out = din("w_out", [D, D])
    w_rt = din("w_rt", [D, 36])
    b_rt = din("b_rt", [1, 36])
    ne_decl = 1 if any(d.startswith("stop") for d in dbg) else NE
    w_eg = din("w_eg", [ne_decl, D, DE])
    w_eu = din("w_eu", [ne_decl, D, DE])
    w_ed = din("w_ed", [ne_decl, DE, D])
    g_fin = din("g_fin", [1, D])
    validT = din("validT", [128, NB])
    padb = din("padb", [1, 512])
    nvalT = din("nvalT", [128, NO])
    cst = din("cst", [128, 8 * 128])
    out_d = nc.dram_tensor("out", [TO, D], F32, kind="ExternalOutput").ap()

    win_bf = dscr("win_bf", [D, IN_COLS], BF16)
    mod_d = dscr("mod_d", [96 * 128], F32)
    KT_d = dscr("KT_d", [2, 128, S], BF16)
    V_d = dscr("V_d", [S, 256], BF16)
    kiT_d = dscr("kiT_d", [128, S], BF16)
    aqT_d = dscr("aqT_d", [8, 128, TO], BF16)
    iqT_d = dscr("iqT_d", [4, 128, TO], BF16)
    sgn_d = dscr("sgn_d", [TO, 8], F32)
    mixT_d = dscr("mixT_d", [D, TO], BF16)
    x1_d = dscr("x1_d", [TO, D], F32)

    dbg_out = {}

    class _Stop(Exception):
        pass

    es = contextlib.ExitStack()
    with es:
        P = Prog(nc, es)
        allbufs = []

        def stop(tag):
            if tag in dbg:
                P.stopped = True

        def body():

            def sb(name, shape, dt=F32):
                return T(P, "sb", name, shape, dt)

            PS = [T(P, "ps", "ps%d" % i, [128, 512], F32) for i in range(8)]
            psrot = [0]
            psmod = [6]

            def psbank():
                t = PS[psrot[0] % psmod[0]]
                psrot[0] += 1
                return t

            def bfv(ps):
                return ps.t[:, :].bitcast(BF16)

            def dump(name, tile, shape, dt=F32):
                if name not in dbg:
                    return
                o = nc.dram_tensor("dbg_" + name, list(shape), dt, kind="ExternalOutput").ap()
                b = Buf("dbg_" + name)
                tok = P.dma(o, tile.t[:] if isinstance(tile, T) else tile[0], [tile.b if isinstance(tile, T) else tile[1]], [b])
                P.final.append((tok[1], tok[2]))
                dbg_out[name] = (shape, dt)

            cstf = sb("cstf", [128, 8 * 128])
            P.dma(cstf[:], cst[:, :], [], [cstf.b])
            ident_f = cstf[:, 0:128]
            TL = cstf[:, 128:256]
            TU = cstf[:, 256:384]
            CI = cstf[:, 384:386]
            CB = cstf[:, 512:640]
            cstb = sb("cstb", [128, 8 * 128], BF16)
            P.copy("dve", cstb[:], cstf[:], [cstf.b], [cstb.b])
            ident = cstb[:, 0:128]
            MBD = cstf[:, 128:256]
            I4 = cstb[:, 640:640 + 128]

            cTt = sb("cTt", [128, KC])
            P.dma(cTt[:], cT[:, :], [], [cTt.b])
            cact = sb("cact", [128, KC])
            P.act(cact[:], cTt[:], AF.Silu, [cTt.b], [cact.b])
            modps = psbank()
            modT = sb("modT", [128, 96])
            bT = sb("bT", [128, 96])
            modTT = sb("modTT", [96, 128])
            g1t = sb("g1t", [128, KC]); g2t = sb("g2t", [128, KC])
            G1s = sb("G1s", [128, KC]); G2s = sb("G2s", [128, KC])
            with contextlib.ExitStack() as es0:
                wad = [T.__new__(T) for _ in range(2)]
                for i, w in enumerate(wad):
                    w.t = es0.enter_context(nc.sbuf_tensor("wad%d" % i, [128, KC, 512], F32))
                    w.b = Buf("wad%d" % i)
                w_ada_v = w_ada.rearrange("(kc p) c -> p kc c", p=128)
                for n in range(24):
                    w = wad[n % 2]
                    P.dma(w[:], w_ada_v[:, :, n * 512:(n + 1) * 512], [], [w.b])
                    for mm_ in range(4):
                        m = n * 4 + mm_
                        for kc in range(KC):
                            P.mm(modps[:, m:m + 1], w[:, kc, mm_ * 128:(mm_ + 1) * 128], cact[:, kc:kc + 1],
                                 kc == 0, kc == KC - 1, [w.b, cact.b], [modps.b])
                P.dma(bT[:], badaT[:, :], [], [bT.b])
                P.tt("dve", modT[:], modps[:, 0:96], bT[:], ALU.add, [modps.b, bT.b], [modT.b])
                dump("modT", modT, [128, 96])
                P.dma(g1t[:], gnmT[:, :], [], [g1t.b])
                P.dma(g2t[:], gnfT[:, :], [], [g2t.b])
                P.stt(G1s[:], modT[:, 16:32], 1.0, g1t[:], ALU.add, ALU.mult, [modT.b, g1t.b], [G1s.b])
                P.stt(G2s[:], modT[:, 64:80], 1.0, g2t[:], ALU.add, ALU.mult, [modT.b, g2t.b], [G2s.b])
                SH1s = modT[:, 0:16]
                SH2s = modT[:, 48:64]
                modD = Buf("mod_d")
                pmt = psbank()
                P.tr(pmt[0:96, 0:128], modT[:], ident_f, [modT.b, cstf.b], [pmt.b])
                P.copy("dve", modTT[:], pmt[0:96, 0:128], [pmt.b], [modTT.b])
                P.dma(mod_d.rearrange("(m p) -> m p", p=128), modTT[:], [modTT.b], [modD])

                winD = Buf("win_bf")
                wst = []
                wsb = []
                for i in range(2):
                    a = T.__new__(T); a.t = es0.enter_context(nc.sbuf_tensor("wst%d" % i, [128, IN_COLS], F32)); a.b = Buf("wst%d" % i)
                    c_ = T.__new__(T); c_.t = es0.enter_context(nc.sbuf_tensor("wsb%d" % i, [128, IN_COLS], BF16)); c_.b = Buf("wsb%d" % i)
                    wst.append(a); wsb.append(c_)
                for kc in range(KC):
                    a = wst[kc % 2]; c_ = wsb[kc % 2]
                    P.dma(a[:], w_in[kc * 128:(kc + 1) * 128, :], [], [a.b])
                    h = IN_COLS // 2
                    P.copy("act", c_[:, 0:h], a[:, 0:h], [a.b], [c_.b])
                    P.copy("pool", c_[:, h:], a[:, h:], [a.b], [c_.b])
                    P.dma(win_bf[kc * 128:(kc + 1) * 128, :], c_[:], [c_.b], [winD], semb=winD)

            stop("stop0")
            P.set_barrier()
            with contextlib.ExitStack() as es1:
                def sb1(name, shape, dt=F32):
                    t = T.__new__(T)
                    t.t = es1.enter_context(nc.sbuf_tensor(name, shape, dt))
                    t.b = P.mkbuf(name)
                    return t

                LB = sb1("LB", [128, 1024]); OMLB = sb1("OMLB", [128, 1024]); GS = sb1("GS", [128, 1024]); l1 = GS
                P.dma(LB[:], lbl[0:1, :].partition_broadcast(128), [], [LB.b])
                P.dma(l1[:], lbl[1:2, :].partition_broadcast(128), [], [l1.b])
                P.tt("dve", LB[:], LB[:], l1[:], ALU.subtract, [LB.b, l1.b], [LB.b])
                P.act(LB[:], LB[:], AF.Sigmoid, [LB.b], [LB.b])
                P.ts("dve", OMLB[:], LB[:], -1.0, 1.0, ALU.mult, ALU.add, [LB.b], [OMLB.b])
                GR = sb1("GR", [128, 1024])
                P.dma(GR[:], g_rec[0:1, :].partition_broadcast(128), [], [GR.b])
                vT = sb1("vT", [128, NB])
                P.dma(vT[:], validT[:, :], [], [vT.b])

                xt = [sb1("xt%d" % i, [128, D]) for i in range(2)]
                junk = sb1("junk", [128, 128], BF16)
                xn = [sb1("xn%d" % i, [128, D], BF16) for i in range(2)]
                st = sb1("st", [128, 8])
                hT = sb1("hT", [128, KC, 512], BF16)
                wt = [sb1("wt%d" % i, [128, KC, 512], BF16) for i in range(2)]
                wrot = [0]
                sgf = sb1("sgf", [128, 4, 1024])
                lgf = sb1("lgf", [128, 4, 1024])
                vbf = sb1("vbf", [128, 4, 1024], BF16)
                kvt = sb1("kvt", [128, 4, 512], BF16)
                ikt = sb1("ikt", [128, 4, 128], BF16)
                qs = sb1("qs", [128, 1024]); gs = sb1("gs", [128, 1024])
                aqt = sb1("aqt", [128, 1024], BF16)
                iqt = sb1("iqt", [128, 512]); iwt = sb1("iwt", [128, 8])
                eR = sb1("eR", [128, 1024]); eB = sb1("eB", [128, 1024])
                ktb = sb1("ktb", [128, 1024], BF16)
                qtb = sb1("qtb", [128, 1024], BF16); qtb1 = sb1("qtb1", [128, 1024], BF16); khb = sb1("khb", [128, 1024], BF16)
                dec = sb1("dec", [128, 16])
                Sst = sb1("Sst", [128, 1024])
                Sbf = [sb1("Sbf%d" % i, [128, 1024], BF16) for i in range(2)]
                qT0 = sb1("qT0", [128, 1024], BF16); qT1 = sb1("qT1", [128, 1024], BF16)
                khT = sb1("khT", [128, 1024], BF16)
                pT = [sb1("pT%d" % i, [128, 128], BF16) for i in range(2)]
                ssq = sb1("ssq", [128, 8]); rsq = sb1("rsq", [128, 8])
                recb = sb1("recb", [128, 1024], BF16)
                trs = sb1("trs", [128, 1536], BF16)
                recT = sb1("recT", [128, 1024], BF16)
                aqT = sb1("aqT", [128, 1024], BF16)
                iqs = sb1("iqs", [128, 512], BF16)
                iqT = sb1("iqT", [128, 512], BF16)
                aw = sb1("aw", [128, 8]); sgn = sb1("sgn", [128, 8])
                P.emit("pool", lambda: nc.gpsimd.memset(Sst[:], 0.0), [], [Sst.b])
                P.emit("pool", lambda: nc.gpsimd.memset(qtb[:], 0.0), [], [qtb.b])
                P.emit("pool", lambda: nc.gpsimd.memset(qtb1[:], 0.0), [], [qtb1.b])

                KTD = Buf("KT_d"); VD = Buf("V_d"); KID = Buf("kiT_d"); AQD = Buf("aqT_d"); IQD = Buf("iqT_d")
                SGD = Buf("sgn_d"); MIXD = Buf("mixT_d")
                win_v = win_bf.rearrange("(kc p) c -> p kc c", p=128)

                def rms_rstd(ssap, outap, n, scale, R, W):
                    P.ts("dve", outap, ssap, scale, EPS, ALU.mult, ALU.add, R, W)
                    P.act(outap, outap, AF.Sqrt, W, W)
                    P.emit("dve", lambda: nc.vector.reciprocal(out=outap, in_=outap), W, W)

                def proj_tile(c0, ncols, blks, evac):
                    w = wt[wrot[0] % 2]; wrot[0] += 1
                    P.dma(w[:, :, 0:ncols], win_v[:, :, c0:c0 + ncols], [winD], [w.b])
                    for blk in blks:
                        ps = psbank()
                        for kc in range(KC):
                            P.mm(ps[:, 0:ncols], hT[:, kc, blk * 128:(blk + 1) * 128], w[:, kc, 0:ncols],
                                 kc == 0, kc == KC - 1, [hT.b, w.b], [ps.b])
                        evac(ps, blk)

                for sbi in range(NSB):
                    for blk in range(4):
                        p = sbi * 4 + blk
                        x_ = xt[p % 2]; xn_ = xn[p % 2]
                        P.dma(x_[:], x_sh[p * 128:(p + 1) * 128, :], [], [x_.b])
                        P.act(xn_[:], x_[:], AF.Square, [x_.b], [xn_.b, st.b], accum_out=st[:, 0:1])
                        rms_rstd(st[:, 0:1], st[:, 1:2], 1, 1.0 / D, [st.b], [st.b])
                        P.ts("dve", xn_[:], x_[:], st[:, 1:2], None, ALU.mult, None, [x_.b, st.b], [xn_.b])
                        for half in range(2):
                            ps = psbank()
                            pv = bfv(ps)
                            for k8 in range(8):
                                kc = half * 8 + k8
                                P.tr(pv[:, k8 * 128:(k8 + 1) * 128], xn_[:, kc * 128:(kc + 1) * 128], ident, [xn_.b, cstb.b], [ps.b])
                            for k8 in range(8):
                                kc = half * 8 + k8
                                o = hT[:, kc, blk * 128:(blk + 1) * 128]
                                i_ = pv[:, k8 * 128:(k8 + 1) * 128]
                                if k8 % 2 == 0:
                                    P.act(o, i_, AF.Identity, [ps.b, G1s.b, modT.b], [hT.b], scale=G1s[:, kc:kc + 1], bias=SH1s[:, kc:kc + 1])
                                else:
                                    P.ts("dve", o, i_, G1s[:, kc:kc + 1], SH1s[:, kc:kc + 1], ALU.mult, ALU.add, [ps.b, G1s.b, modT.b], [hT.b])
                    if sbi == 0:
                        dump("hT", hT, [128, KC, 512], BF16)
                    stop("stopA")
                    for ti in range(2):
                        proj_tile(C_RF + ti * 512, 512, range(4),
                                  lambda ps, blk, ti=ti: P.act(sgf[:, blk, ti * 512:(ti + 1) * 512], ps[:, :], AF.Sigmoid, [ps.b], [sgf.b]))
                    for blk in range(4):
                        P.tt("dve", sgf[:, blk, :], sgf[:, blk, :], OMLB[:], ALU.mult, [sgf.b, OMLB.b], [sgf.b])
                        P.tt("dve", sgf[:, blk, :], sgf[:, blk, :], LB[:], ALU.add, [sgf.b, LB.b], [sgf.b])
                        P.act(lgf[:, blk, :], sgf[:, blk, :], AF.Ln, [sgf.b], [lgf.b])
                        P.ts("pool", sgf[:, blk, :], sgf[:, blk, :], -1.0, 1.0, ALU.mult, ALU.add, [sgf.b, lgf.b], [sgf.b])
                    for ti in range(2):
                        proj_tile(C_RI + ti * 512, 512, range(4),
                                  lambda ps, blk, ti=ti: P.copy("act", vbf[:, blk, ti * 512:(ti + 1) * 512], ps[:, :], [ps.b], [vbf.b]))
                    proj_tile(C_AK, 512, range(4), lambda ps, blk: P.copy("dve", kvt[:, blk, :], ps[:, :], [ps.b], [kvt.b]))

                    def ev_ik(ps, blk):
                        P.copy("act", ikt[:, blk, 0:64], ps[:, 0:64], [ps.b], [ikt.b])
                        P.copy("act", ikt[:, blk, 64:128], ps[:, 0:64], [ps.b], [ikt.b])
                    proj_tile(C_IK, 64, range(4), ev_ik)
                    for ti in range(2):
                        proj_tile(C_RQ + ti * 512, 512, [3],
                                  lambda ps, blk, ti=ti: P.act(qs[:, ti * 512:(ti + 1) * 512], ps[:, :], AF.Silu, [ps.b], [qs.b]))
                    for ti in range(2):
                        proj_tile(C_RG + ti * 512, 512, [3],
                                  lambda ps, blk, ti=ti: P.act(gs[:, ti * 512:(ti + 1) * 512], ps[:, :], AF.Silu, [ps.b], [gs.b]))
                    for ti in range(2):
                        proj_tile(C_AQ + ti * 512, 512, [3],
                                  lambda ps, blk, ti=ti: P.copy("dve", aqt[:, ti * 512:(ti + 1) * 512], ps[:, :], [ps.b], [aqt.b]))
                    proj_tile(C_IQ, 512, [3], lambda ps, blk: P.copy("dve", iqt[:], ps[:, :], [ps.b], [iqt.b]))
                    proj_tile(C_IW, 8, [3], lambda ps, blk: P.copy("dve", iwt[:], ps[:, 0:8], [ps.b], [iwt.b]))

                    stop("stopB")
                    for blk in range(4):
                        p = sbi * 4 + blk
                        own = (blk == 3)
                        for ti in range(2):
                            ps = psbank()
                            P.mm(ps[:, :], TU, lgf[:, blk, ti * 512:(ti + 1) * 512], True, True, [cstf.b, lgf.b], [ps.b])
                            P.act(eR[:, ti * 512:(ti + 1) * 512], ps[:, :], AF.Exp, [ps.b], [eR.b])
                        P.stt(ktb[:], sgf[:, blk, :], vT[:, p:p + 1], eR[:], ALU.mult, ALU.mult, [sgf.b, vT.b, eR.b], [ktb.b])
                        psd = psbank()
                        for hd in range(8):
                            P.mm(psd[:, 2 * hd:2 * hd + 2], lgf[:, blk, hd * 128:(hd + 1) * 128], CI, True, True, [lgf.b, cstf.b], [psd.b])
                        P.act(dec[:], psd[:, 0:16], AF.Exp, [psd.b], [dec.b])
                        stop("stopD1")
                        if own:
                            for ti in range(2):
                                ps = psbank()
                                P.mm(ps[:, :], TL, lgf[:, blk, ti * 512:(ti + 1) * 512], True, True, [cstf.b, lgf.b], [ps.b])
                                P.act(eB[:, ti * 512:(ti + 1) * 512], ps[:, :], AF.Exp, [ps.b], [eB.b])
                                P.act(eR[:, ti * 512:(ti + 1) * 512], ps[:, :], AF.Exp, [ps.b, ktb.b], [eR.b], scale=-1.0)
                            P.stt(qtb[0:64, :], qs[0:64, :], 128.0 ** -0.5, eB[0:64, :], ALU.mult, ALU.mult, [qs.b, eB.b], [qtb.b])
                            P.stt(qtb1[64:128, :], qs[64:128, :], 128.0 ** -0.5, eB[64:128, :], ALU.mult, ALU.mult, [qs.b, eB.b], [qtb1.b])
                            P.tt("dve", khb[:], sgf[:, blk, :], eR[:], ALU.mult, [sgf.b, eR.b], [khb.b])
                            stop("stopD3a")
                            psq = psbank(); psk = psbank()
                            pq = bfv(psq); pk = bfv(psk)
                            for hd in range(8):
                                P.tr(pq[:, hd * 128:(hd + 1) * 128], qtb[:, hd * 128:(hd + 1) * 128], ident, [qtb.b, cstb.b], [psq.b])
                            for hd in range(8):
                                P.tr(pk[:, hd * 128:(hd + 1) * 128], khb[:, hd * 128:(hd + 1) * 128], ident, [khb.b, cstb.b], [psk.b])
                            psq1 = psbank(); pq1 = bfv(psq1)
                            for hd in range(8):
                                P.tr(pq1[:, hd * 128:(hd + 1) * 128], qtb1[:, hd * 128:(hd + 1) * 128], ident, [qtb1.b, cstb.b], [psq1.b])
                            stop("stopD3b")
                            P.copy("act", qT0[:], pq[:, :], [psq.b], [qT0.b])
                            P.copy("dve", qT1[:], pq1[:, :], [psq1.b], [qT1.b])
                            P.copy("act", khT[:], pk[:, :], [psk.b], [khT.b])
                            stop("stopD3")
                        for c in range(2):
                            if own:
                                P.copy("act" if c == 0 else "pool", Sbf[c][:], Sst[:], [Sst.b], [Sbf[c].b])
                            for hh in range(2):
                                ps = psbank()
                                for h4 in range(4):
                                    hd = hh * 4 + h4
                                    P.mm(ps[:, h4 * 128:(h4 + 1) * 128], ktb[c * 64:(c + 1) * 64, hd * 128:(hd + 1) * 128],
                                         vbf[c * 64:(c + 1) * 64, blk, hd * 128:(hd + 1) * 128], True, True, [ktb.b, vbf.b], [ps.b])
                                for h4 in range(4):
                                    hd = hh * 4 + h4
                                    P.stt(Sst[:, hd * 128:(hd + 1) * 128], Sst[:, hd * 128:(hd + 1) * 128], dec[:, 2 * hd + c:2 * hd + c + 1], ps[:, h4 * 128:(h4 + 1) * 128],
                                          ALU.mult, ALU.add, [Sst.b, dec.b, ps.b], [Sst.b])
                        stop("stopD2")
                        if own:
                            ob = sbi
                            pso = [PS[6], PS[7]]
                            for hd in range(8):
                                pss = psbank()
                                P.mm(pss[:, 0:128], khT[:, hd * 128:(hd + 1) * 128], qT0[:, hd * 128:(hd + 1) * 128], True, False, [khT.b, qT0.b], [pss.b])
                                P.mm(pss[:, 0:128], khT[:, hd * 128:(hd + 1) * 128], qT1[:, hd * 128:(hd + 1) * 128], False, True, [khT.b, qT1.b], [pss.b])
                                pt = pT[hd % 2]
                                P.tt("dve", pt[:], pss[:, 0:128], MBD, ALU.mult, [pss.b, cstf.b], [pt.b])
                                po = pso[hd // 4]
                                oo = po[:, (hd % 4) * 128:(hd % 4 + 1) * 128]
                                P.mm(oo, pt[:], vbf[:, blk, hd * 128:(hd + 1) * 128], True, False, [pt.b, vbf.b], [po.b])
                                P.mm(oo, qT0[:, hd * 128:(hd + 1) * 128], Sbf[0][:, hd * 128:(hd + 1) * 128], False, False, [qT0.b, Sbf[0].b], [po.b])
                                P.mm(oo, qT1[:, hd * 128:(hd + 1) * 128], Sbf[1][:, hd * 128:(hd + 1) * 128], False, True, [qT1.b, Sbf[1].b], [po.b])
                            stop("stopD3c")
                            for hd in range(8):
                                po = pso[hd // 4]
                                P.act(junk[:, 0:128], po[:, (hd % 4) * 128:(hd % 4 + 1) * 128], AF.Square, [po.b], [junk.b, ssq.b],
                                      accum_out=ssq[:, hd:hd + 1])
                            stop("stopD3d")
                            rms_rstd(ssq[:], rsq[:], 8, 1.0 / 128, [ssq.b], [rsq.b])
                            P.tt("dve", GS[:], gs[:], GR[:], ALU.mult, [gs.b, GR.b], [GS.b])
                            for hd in range(8):
                                po = pso[hd // 4]
                                P.stt(recb[:, hd * 128:(hd + 1) * 128], po[:, (hd % 4) * 128:(hd % 4 + 1) * 128], rsq[:, hd:hd + 1],
                                      GS[:, hd * 128:(hd + 1) * 128], ALU.mult, ALU.mult, [po.b, rsq.b, GS.b], [recb.b])
                            if sbi == 0:
                                dump("rec0", recb, [128, 1024], BF16)
                            stop("stopD4")
                            psr = psbank(); pr = bfv(psr)
                            for hd in range(8):
                                P.tr(pr[:, hd * 128:(hd + 1) * 128], recb[:, hd * 128:(hd + 1) * 128], ident, [recb.b, cstb.b], [psr.b])
                            P.copy("act", recT[:], pr[:, :], [psr.b], [recT.b])
                            P.dma(mixT_d[0:1024, ob * 128:(ob + 1) * 128].rearrange("(h p) t -> p h t", p=128), recT[:].rearrange("p (h t) -> p h t", h=8), [recT.b], [MIXD], semb=MIXD)
                            psa = psbank(); pa = bfv(psa)
                            for hd in range(8):
                                P.tr(pa[:, hd * 128:(hd + 1) * 128], aqt[:, hd * 128:(hd + 1) * 128], ident, [aqt.b, cstb.b], [psa.b])
                            P.copy("act", aqT[:], pa[:, :], [psa.b], [aqT.b])
                            P.dma(aqT_d[:, :, ob * 128:(ob + 1) * 128].rearrange("h p t -> p h t"), aqT[:].rearrange("p (h t) -> p h t", h=8), [aqT.b], [AQD], semb=AQD)
                            P.act(aw[:], iwt[:], AF.Abs, [iwt.b], [aw.b], scale=64.0 ** -0.5 * 8.0 ** -0.5)
                            P.ts("dve", sgn[:], iwt[:], 0.0, 2.0, ALU.is_ge, ALU.mult, [iwt.b], [sgn.b])
                            P.ts("dve", sgn[:], sgn[:], -1.0, None, ALU.add, None, [sgn.b], [sgn.b])
                            for h in range(8):
                                P.ts("pool", iqs[:, h * 64:(h + 1) * 64], iqt[:, h * 64:(h + 1) * 64], aw[:, h:h + 1], None, ALU.mult, None,
                                     [iqt.b, aw.b], [iqs.b])
                            psi = psbank(); pi = bfv(psi)
                            for c4 in range(4):
                                P.tr(pi[:, c4 * 128:(c4 + 1) * 128], iqs[:, c4 * 128:(c4 + 1) * 128], ident, [iqs.b, cstb.b], [psi.b])
                            P.copy("act", iqT[:], pi[:, 0:512], [psi.b], [iqT.b])
                            P.dma(iqT_d[:, :, ob * 128:(ob + 1) * 128].rearrange("h p t -> p h t"), iqT[:].rearrange("p (h t) -> p h t", h=4), [iqT.b], [IQD], semb=IQD)
                            P.dma(sgn_d[ob * 128:(ob + 1) * 128, :], sgn[:], [sgn.b], [SGD], semb=SGD)
                    stop("stopD")
                    pst = [psbank(), psbank()]
                    for blk in range(4):
                        for w3 in range(3):
                            idx = blk * 3 + w3
                            pv = bfv(pst[idx // 8])
                            src = kvt[:, blk, w3 * 128:(w3 + 1) * 128] if w3 < 2 else ikt[:, blk, :]
                            P.tr(pv[:, (idx % 8) * 128:(idx % 8 + 1) * 128], src, ident, [kvt.b, ikt.b, cstb.b], [pst[idx // 8].b])
                    P.copy("act", trs[:, 0:1024], bfv(pst[0])[:, :], [pst[0].b], [trs.b])
                    P.copy("dve", trs[:, 1024:1536], bfv(pst[1])[:, 0:512], [pst[1].b], [trs.b])
                    trv = trs[:].rearrange("p (b w t) -> p b w t", w=3, t=128)
                    t0 = sbi * 512
                    for kv in range(2):
                        P.dma(KT_d[kv, :, t0:t0 + 512].rearrange("p (b t) -> p b t", b=4), trv[:, :, kv, :], [trs.b], [KTD], semb=KTD)
                    P.dma(kiT_d[:, t0:t0 + 512].rearrange("p (b t) -> p b t", b=4), trv[:, :, 2, :], [trs.b], [KID], semb=KID)
                    P.dma(V_d[t0:t0 + 512, :].rearrange("(b p) c -> p b c", p=128), kvt[:, :, 256:512], [kvt.b], [VD], semb=VD)
                dump("Sst", Sst, [128, 1024])

            stop("stop1")

            psmod[0] = 3
            NIT = 22
            P.set_barrier()
            with contextlib.ExitStack() as es2:
                def sb2(name, shape, dt=F32):
                    t = T.__new__(T)
                    t.t = es2.enter_context(nc.sbuf_tensor(name, shape, dt))
                    t.b = P.mkbuf(name)
                    return t
                KT = sb2("KT", [128, 2 * S], BF16)
                kiT = sb2("kiT", [128, S], BF16)
                Vaug = sb2("Vaug", [128, NB * 2 * 132], BF16)
                scoreL = [sb2("score%d" % k, [128, S]) for k in range(2)]
                nbiasL = [sb2("nbias%d" % k, [128, S], BF16) for k in range(2)]
                rbuf = [sb2("rbuf%d" % i, [128, 512], BF16) for i in range(2)]
                pbuf = [sb2("pbuf%d" % i, [128, 512], BF16) for i in range(2)]
                aqTiL = [sb2("aqTi%d" % k, [128, 1024], BF16) for k in range(2)]
                iqTi = sb2("iqTi", [128, 512], BF16)
                sgni = sb2("sgni", [128, 8])
                Dh = sb2("Dh", [128, 1024], BF16)
                CBf = sb2("CBf", [128, 512], BF16)
                PBt = sb2("PBt", [128, 512], BF16)
                BIGI4 = sb2("BIGI4", [128, 512], BF16)
                GA = sb2("GA", [128, 1024])
                nv = sb2("nv", [128, NO])
                smL = [sb2("sm%d" % k, [128, 16]) for k in range(2)]
                cjL = [sb2("cj%d" % k, [128, 8], BF16) for k in range(2)]
                sm2 = sb2("sm2", [128, 8]); sm3 = sb2("sm3", [128, 8]); sm4 = sb2("sm4", [128, 8])
                junk2 = sb2("junk2", [128, 128], BF16)
                attb = sb2("attb", [128, 1024], BF16)
                attT = sb2("attT", [128, 1024], BF16)
                P.dma(KT[:, 0:S], KT_d[0], [KTD], [KT.b])
                P.dma(KT[:, S:2 * S], KT_d[1], [KTD], [KT.b])
                P.dma(kiT[:], kiT_d[:, :], [KID], [kiT.b])
                P.emit("pool", lambda: nc.gpsimd.memset(Vaug[:], 1.0), [], [Vaug.b])
                Vv = Vaug[:].rearrange("p (b k c) -> p b k c", k=2, c=132)
                V_dv = V_d.rearrange("(b p) c -> p b c", p=128)
                for b0 in range(0, NB, 16):
                    b1 = min(NB, b0 + 16)
                    for kv in range(2):
                        P.dma(Vv[:, b0:b1, kv, 0:128], V_dv[:, b0:b1, kv * 128:(kv + 1) * 128], [VD], [Vaug.b])
                P.emit("pool", lambda: nc.gpsimd.memset(CBf[:], 0.0), [], [CBf.b])
                P.copy("pool", CBf[:, 384:512], CB, [cstf.b], [CBf.b])
                P.dma(scoreL[0][:, 0:512], padb[0:1, :].partition_broadcast(128), [], [scoreL[0].b])
                P.copy("pool", PBt[:], scoreL[0][:, 0:512], [scoreL[0].b], [PBt.b])
                for r4 in range(4):
                    P.ts("dve", BIGI4[:, r4 * 128:(r4 + 1) * 128], ident_f, 29952.0, None, ALU.mult, None, [cstf.b], [BIGI4.b])
                P.dma(GA[:], g_att[0:1, :].partition_broadcast(128), [], [GA.b])
                P.dma(nv[:], nvalT[:, :], [], [nv.b])
                acc = [PS[3], PS[4], PS[5]]
                def stageA(i):
                    score = scoreL[i % 2]; nbias = nbiasL[i % 2]; cj = cjL[i % 2]; aqTi = aqTiL[i % 2]; sm = smL[i % 2]
                    LO, HI, TH, CNT, GE, DD, NGE, SEL, AA, BB, THF = [sm[:, k:k + 1] for k in range(11)]
                    SMB = [sm.b]
                    nkt = i + 1
                    n = nkt * 512
                    nk = 4 * (i + 1)
                    P.dma(aqTi[:].rearrange("p (h t) -> p h t", h=8), aqT_d[:, :, i * 128:(i + 1) * 128].rearrange("h p t -> p h t"), [AQD], [aqTi.b])
                    P.dma(iqTi[:].rearrange("p (h t) -> p h t", h=4), iqT_d[:, :, i * 128:(i + 1) * 128].rearrange("h p t -> p h t"), [IQD], [iqTi.b])
                    P.dma(sgni[:], sgn_d[i * 128:(i + 1) * 128, :], [SGD], [sgni.b])
                    for h in range(8):
                        P.ts("dve", Dh[:, h * 128:(h + 1) * 128], ident_f, sgni[:, h:h + 1], None, ALU.mult, None, [cstf.b, sgni.b], [Dh.b])
                    for kt in range(nkt):
                        psc = PS[6 + kt % 2]
                        for h in range(8):
                            ps = psbank()
                            pb = (h % 2) * 64
                            P.mm(ps[:, :], iqTi[pb:pb + 64, (h // 2) * 128:(h // 2 + 1) * 128], kiT[pb:pb + 64, kt * 512:(kt + 1) * 512],
                                 True, True, [iqTi.b, kiT.b], [ps.b])
                            rb = rbuf[h % 2]
                            P.act(rb[:], ps[:, :], AF.Relu, [ps.b], [rb.b])
                            P.mm(psc[:, :], Dh[:, h * 128:(h + 1) * 128], rb[:], h == 0, h == 7, [Dh.b, rb.b], [psc.b])
                        dst = score[:, kt * 512:(kt + 1) * 512]
                        if kt == nkt - 1:
                            P.tt("dve", dst, psc[:, :], CBf[:], ALU.add, [psc.b, CBf.b], [score.b])
                            if kt == 0:
                                P.tt("dve", dst, dst, PBt[:], ALU.add, [score.b, PBt.b], [score.b])
                        elif kt == 0:
                            P.tt("dve", dst, psc[:, :], PBt[:], ALU.add, [psc.b, PBt.b], [score.b])
                        else:
                            P.copy("act", dst, psc[:, :], [psc.b], [score.b])
                    sc = score[:, 0:n]
                    P.emit("dve", lambda sc=sc: nc.vector.reduce_max(out=HI, in_=sc, axis=AX.X), [score.b], SMB)
                    P.ts("dve", LO, HI, -64.0, None, ALU.add, None, SMB, SMB)
                    for it in range(NIT):
                        cw = 64.0 / (2.0 ** (it + 1))
                        P.ts("dve", TH, LO, cw, None, ALU.add, None, SMB, SMB)
                        P.ts("dve", cj[:, 0:1].to_broadcast([128, n]), sc, TH, 0.0, ALU.is_ge, ALU.add, [score.b] + SMB, [cj.b] + SMB, accum_out=CNT)
                        P.ts("dve", GE, CNT, float(KSEL), cw, ALU.is_ge, ALU.mult, SMB, SMB)
                        P.tt("dve", LO, LO, GE, ALU.add, SMB, SMB)
                    P.ts("dve", SEL, nv[:, i:i + 1], float(KSEL), None, ALU.is_gt, None, [nv.b], SMB)
                    P.ts("dve", AA, SEL, 1e20, -1e20, ALU.mult, ALU.add, SMB, SMB)
                    P.tt("dve", BB, LO, SEL, ALU.mult, SMB, SMB)
                    P.tt("dve", THF, AA, BB, ALU.add, SMB, SMB)
                    P.ts("dve", nbias[:, 0:n], sc, THF, 1.0, ALU.is_ge, ALU.subtract, [score.b] + SMB, [nbias.b])
                def stageB(i):
                    nbias = nbiasL[i % 2]; aqTi = aqTiL[i % 2]
                    nk = 4 * (i + 1)
                    its = [(kb, kvh) for kb in range(nk) for kvh in range(2)]

                    def Lmm(k):
                        kb, kvh = its[k]
                        psl = psbank()
                        P.mm(psl[:, :], KT[:, kvh * S + kb * 128:kvh * S + (kb + 1) * 128], aqTi[:, kvh * 512:(kvh + 1) * 512],
                             True, False, [KT.b, aqTi.b], [psl.b])
                        P.mm(psl[:, :], nbias[:, kb * 128:(kb + 1) * 128], BIGI4[:], False, True, [nbias.b, BIGI4.b], [psl.b])
                        return psl
                    psl_next = Lmm(0)
                    for k, (kb, kvh) in enumerate(its):
                        psl = psl_next
                        if k + 1 < len(its):
                            psl_next = Lmm(k + 1)
                        pb_ = pbuf[k % 2]
                        P.act(pb_[:], psl[:, :], AF.Exp, [psl.b], [pb_.b], scale=128.0 ** -0.5)
                        vo = (kb * 2 + kvh) * 132
                        for g in range(4):
                            hd = kvh * 4 + g
                            a = acc[hd // 3]
                            off = (hd % 3) * 132
                            P.mm(a[:, off:off + 129], pb_[:, g * 128:(g + 1) * 128], Vaug[:, vo:vo + 129], (kb == 0 and hd % 3 == 0), False,
                                 [pb_.b, Vaug.b], [a.b], inc=(g == 3), skip_group_check=True)
                    for hd in range(8):
                        a = acc[hd // 3]
                        off = (hd % 3) * 132
                        P.emit("dve", lambda a=a, off=off, hd=hd: nc.vector.reciprocal(out=sm2[:, hd:hd + 1], in_=a[:, off + 128:off + 129]), [a.b], [sm2.b])
                        P.act(junk2[:], a[:, off:off + 128], AF.Square, [a.b, sm2.b], [junk2.b, sm3.b], scale=sm2[:, hd:hd + 1],
                              accum_out=sm3[:, hd:hd + 1])
                    rms_rstd(sm3[:], sm4[:], 8, 1.0 / 128, [sm3.b], [sm4.b])
                    P.tt("dve", sm4[:], sm4[:], sm2[:], ALU.mult, [sm4.b, sm2.b], [sm4.b])
                    for hd in range(8):
                        a = acc[hd // 3]
                        off = (hd % 3) * 132
                        P.stt(attb[:, hd * 128:(hd + 1) * 128], a[:, off:off + 128], sm4[:, hd:hd + 1], GA[:, hd * 128:(hd + 1) * 128],
                              ALU.mult, ALU.mult, [a.b, sm4.b, GA.b], [attb.b])
                    if i == 0:
                        dump("att0", attb, [128, 1024], BF16)
                    if i == 1:
                        dump("att1", attb, [128, 1024], BF16)
                    psr = psbank(); pr = bfv(psr)
                    for hd in range(8):
                        P.tr(pr[:, hd * 128:(hd + 1) * 128], attb[:, hd * 128:(hd + 1) * 128], ident, [attb.b, cstb.b], [psr.b])
                    P.copy("act", attT[:], pr[:, :], [psr.b], [attT.b])
                    P.dma(mixT_d[1024:2048, i * 128:(i + 1) * 128].rearrange("(h p) t -> p h t", p=128), attT[:].rearrange("p (h t) -> p h t", h=8),
                          [attT.b], [MIXD], semb=MIXD)
                stageA(0)
                for i in range(NO):
                    if i + 1 < NO:
                        stageA(i + 1)
                    stageB(i)
            psmod[0] = 6
            stop("stop2")

            h2T_d = dscr("h2T_d", [D, TO], BF16)
            H2D = Buf("h2T_d"); X1D = Buf("x1_d"); OUTD = Buf("out")
            P.set_barrier()
            with contextlib.ExitStack() as es34:
                def sb34(name, shape, dt=F32, st=es34):
                    t = T.__new__(T)
                    t.t = st.enter_context(nc.sbuf_tensor(name, shape, dt))
                    t.b = P.mkbuf(name)
                    return t
                comb = sb34("comb", [128, NO * 32])
                with contextlib.ExitStack() as es3:
                    sb3 = lambda name, shape, dt=F32: sb34(name, shape, dt, es3)
                    Wo = sb3("Wo", [128, KC * 2048], BF16)
                    wst3 = [sb3("wst3_%d" % i, [128, 2048]) for i in range(2)]
                    for kc in range(KC):
                        w = wst3[kc % 2]
                        P.dma(w[:], w_out[kc * 128:(kc + 1) * 128, :], [], [w.b])
                        P.copy("act" if kc % 2 == 0 else "pool", Wo[:, kc * 2048:(kc + 1) * 2048], w[:], [w.b], [Wo.b])
                    GT1b = sb3("GT1b", [128, 2048])
                    mod6 = mod_d.rearrange("(a n) -> a n", a=6)
                    P.dma(GT1b[:], mod6[2:3, :].partition_broadcast(128), [modD], [GT1b.b])
                    Wrt = sb3("Wrt", [128, KC * 36]); Wrtb = sb3("Wrtb", [128, KC * 36], BF16)
                    P.dma(Wrt[:].rearrange("p (k c) -> p k c", c=36), w_rt.rearrange("(k p) c -> p k c", p=128), [], [Wrt.b])
                    P.copy("dve", Wrtb[:], Wrt[:], [Wrt.b], [Wrtb.b])
                    brt = sb3("brt", [128, 36])
                    P.dma(brt[:], b_rt[0:1, :].partition_broadcast(128), [], [brt.b])
                    xo = [sb3("xo%d" % i, [128, D]) for i in range(2)]
                    x1t = [sb3("x1t%d" % i, [128, D]) for i in range(2)]
                    xn2 = sb3("xn2", [128, D], BF16)
                    mixTi = sb3("mixTi", [128, KC * 128], BF16)
                    h2Tb = sb3("h2Tb", [128, KC * 128], BF16)
                    st3 = sb3("st3", [128, 8])
                    lg = sb3("lg", [128, 36])
                    rs = sb3("rs", [128, 64])
                    for i in range(NO):
                        x_ = xo[i % 2]; x1 = x1t[i % 2]
                        p = 4 * i + 3
                        P.dma(x_[:], x_sh[p * 128:(p + 1) * 128, :], [], [x_.b])
                        P.dma(mixTi[:].rearrange("p (k t) -> p k t", t=128), mixT_d[:, i * 128:(i + 1) * 128].rearrange("(k p) t -> p k t", p=128),
                              [MIXD], [mixTi.b])
                        for dt in range(4):
                            ps = psbank()
                            for kc in range(KC):
                                P.mm(ps[:, :], mixTi[:, kc * 128:(kc + 1) * 128], Wo[:, kc * 2048 + dt * 512:kc * 2048 + (dt + 1) * 512],
                                     kc == 0, kc == KC - 1, [mixTi.b, Wo.b], [ps.b])
                            P.tt("dve", x1[:, dt * 512:(dt + 1) * 512], ps[:, :], GT1b[:, dt * 512:(dt + 1) * 512], ALU.mult, [ps.b, GT1b.b], [x1.b])
                        P.tt("dve", x1[:], x1[:], x_[:], ALU.add, [x1.b, x_.b], [x1.b])
                        if i == 0:
                            dump("x1", x1, [128, D])
                        P.dma(x1_d[i * 128:(i + 1) * 128, :], x1[:], [x1.b], [X1D], semb=X1D)
                        P.act(xn2[:], x1[:], AF.Square, [x1.b], [xn2.b, st3.b], accum_out=st3[:, 0:1])
                        rms_rstd(st3[:, 0:1], st3[:, 1:2], 1, 1.0 / D, [st3.b], [st3.b])
                        P.ts("dve", xn2[:], x1[:], st3[:, 1:2], None, ALU.mult, None, [x1.b, st3.b], [xn2.b])
                        for half in range(2):
                            ps = psbank()
                            pv = bfv(ps)
                            for k8 in range(8):
                                kc = half * 8 + k8
                                P.tr(pv[:, k8 * 128:(k8 + 1) * 128], xn2[:, kc * 128:(kc + 1) * 128], ident, [xn2.b, cstb.b], [ps.b])
                            for k8 in range(8):
                                kc = half * 8 + k8
                                o = h2Tb[:, kc * 128:(kc + 1) * 128]
                                i_ = pv[:, k8 * 128:(k8 + 1) * 128]
                                if k8 % 2 == 0:
                                    P.act(o, i_, AF.Identity, [ps.b, G2s.b, modT.b], [h2Tb.b], scale=G2s[:, kc:kc + 1], bias=SH2s[:, kc:kc + 1])
                                else:
                                    P.ts("dve", o, i_, G2s[:, kc:kc + 1], SH2s[:, kc:kc + 1], ALU.mult, ALU.add, [ps.b, G2s.b, modT.b], [h2Tb.b])
                        P.dma(h2T_d[:, i * 128:(i + 1) * 128].rearrange("(k p) t -> p k t", p=128), h2Tb[:].rearrange("p (k t) -> p k t", t=128),
                              [h2Tb.b], [H2D], semb=H2D)
                        psr = psbank()
                        for kc in range(KC):
                            P.mm(psr[:, 0:36], h2Tb[:, kc * 128:(kc + 1) * 128], Wrtb[:, kc * 36:(kc + 1) * 36], kc == 0, kc == KC - 1,
                                 [h2Tb.b, Wrtb.b], [psr.b])
                        P.tt("dve", lg[:], psr[:, 0:36], brt[:], ALU.add, [psr.b, brt.b], [lg.b])
                        RB = [rs.b]
                        GMAX, NGM, SE, PG, M1, M2, DLT, EX, DEN, W1, W2 = [rs[:, k:k + 1] for k in range(11)]
                        OHG = rs[:, 12:16]; ESEL = rs[:, 16:24]; MK1 = rs[:, 24:32]; E2 = rs[:, 32:40]; MK2 = rs[:, 40:48]; CIG = rs[:, 48:56]; EG = rs[:, 56:60]
                        P.emit("dve", lambda: nc.vector.reduce_max(out=GMAX, in_=lg[:, 0:4], axis=AX.X), [lg.b], RB)
                        P.ts("dve", NGM, GMAX, -1.0, None, ALU.mult, None, RB, RB)
                        P.act(EG, lg[:, 0:4], AF.Exp, [lg.b] + RB, RB, bias=NGM, accum_out=SE)
                        P.emit("dve", lambda: nc.vector.reciprocal(out=PG, in_=SE), RB, RB)
                        P.ts("dve", OHG, lg[:, 0:4], GMAX, None, ALU.is_ge, None, [lg.b] + RB, RB)
                        P.ts("dve", ESEL, lg[:, 4:12], rs[:, 12:13], None, ALU.mult, None, [lg.b] + RB, RB)
                        for g in range(1, 4):
                            P.stt(ESEL, lg[:, 4 + 8 * g:12 + 8 * g], rs[:, 12 + g:13 + g], ESEL, ALU.mult, ALU.add, [lg.b] + RB, RB)
                        P.emit("dve", lambda: nc.vector.reduce_max(out=M1, in_=ESEL, axis=AX.X), RB, RB)
                        P.ts("dve", MK1, ESEL, M1, None, ALU.is_ge, None, RB, RB)
                        P.stt(E2, MK1, -1e30, ESEL, ALU.mult, ALU.add, RB, RB)
                        P.emit("dve", lambda: nc.vector.reduce_max(out=M2, in_=E2, axis=AX.X), RB, RB)
                        P.ts("dve", MK2, E2, M2, None, ALU.is_ge, None, RB, RB)
                        P.tt("dve", DLT, M2, M1, ALU.subtract, RB, RB)
                        P.act(EX, DLT, AF.Exp, RB, RB)
                        P.ts("dve", DEN, EX, 1.0, None, ALU.add, None, RB, RB)
                        P.emit("dve", lambda: nc.vector.reciprocal(out=DEN, in_=DEN), RB, RB)
                        P.tt("dve", W1, DEN, PG, ALU.mult, RB, RB)
                        P.tt("dve", W2, W1, EX, ALU.mult, RB, RB)
                        P.ts("dve", CIG, MK1, W1, None, ALU.mult, None, RB, RB)
                        P.stt(CIG, MK2, W2, CIG, ALU.mult, ALU.add, RB, RB)
                        for g in range(4):
                            P.ts("dve", comb[:, i * 32 + g * 8:i * 32 + (g + 1) * 8], CIG, rs[:, 12 + g:13 + g], None, ALU.mult, None, RB, [comb.b])
                stop("stop3")
                P.set_barrier()
                with contextlib.ExitStack() as es4:
                    sb4 = lambda name, shape, dt=F32: sb34(name, shape, dt, es4)
                    CH = min(TO, 1024)
                    NCH = TO // CH
                    NT = CH // 128
                    SUB = min(512, CH)
                    h2c = sb4("h2c", [128, KC * CH], BF16)
                    yacc = sb4("yacc", [128, NT * 2048])
                    WGb = sb4("WGb", [128, KC * 512], BF16); WUb = sb4("WUb", [128, KC * 512], BF16); WDb = sb4("WDb", [128, 4 * 2048], BF16)
                    stg = [sb4("stg%d" % k, [128, 2048]) for k in range(4)]
                    sgt = [sb4("sgt%d" % k, [128, 512]) for k in range(2)]
                    heT = [sb4("heT%d" % k, [128, 4 * 512], BF16) for k in range(2)]
                    xf = sb4("xf", [128, 2048])
                    st4 = sb4("st4", [128, 8])
                    srot = [0]

                    def load_cast(dst_ap, dstb, src_ap, three=None):
                        w = stg[srot[0] % 4]
                        eng = "act" if srot[0] % 4 != 3 else "dve"
                        srot[0] += 1
                        if three is None:
                            P.dma(w[:], src_ap, [], [w.b])
                        else:
                            P.dma(w[:].rearrange("p (k c) -> p k c", c=512), src_ap, [], [w.b])
                        P.copy(eng, dst_ap, w[:], [w.b], [dstb])

                    for ch in range(NCH):
                        P.dma(h2c[:].rearrange("p (k t) -> p k t", t=CH), h2T_d[:, ch * CH:(ch + 1) * CH].rearrange("(k p) t -> p k t", p=128),
                              [H2D], [h2c.b])
                        P.emit("pool", lambda: nc.gpsimd.memset(yacc[:], 0.0), [], [yacc.b])
                        for e in range(NE):
                            for q in range(4):
                                load_cast(WGb[:, q * 2048:(q + 1) * 2048], WGb.b, w_eg[e % ne_decl, q * 512:(q + 1) * 512, :].rearrange("(k p) c -> p k c", p=128), 1)
                            for q in range(4):
                                load_cast(WUb[:, q * 2048:(q + 1) * 2048], WUb.b, w_eu[e % ne_decl, q * 512:(q + 1) * 512, :].rearrange("(k p) c -> p k c", p=128), 1)
                            for c in range(4):
                                load_cast(WDb[:, c * 2048:(c + 1) * 2048], WDb.b, w_ed[e % ne_decl, c * 128:(c + 1) * 128, :])
                            for st_ in range(CH // SUB):
                                tok0 = st_ * SUB
                                he = heT[st_ % 2]
                                for c in range(4):
                                    psg = psbank(); psu = psbank()
                                    for kc in range(KC):
                                        P.mm(psg[:, 0:SUB], WGb[:, kc * 512 + c * 128:kc * 512 + (c + 1) * 128], h2c[:, kc * CH + tok0:kc * CH + tok0 + SUB],
                                             kc == 0, kc == KC - 1, [WGb.b, h2c.b], [psg.b])
                                    for kc in range(KC):
                                        P.mm(psu[:, 0:SUB], WUb[:, kc * 512 + c * 128:kc * 512 + (c + 1) * 128], h2c[:, kc * CH + tok0:kc * CH + tok0 + SUB],
                                             kc == 0, kc == KC - 1, [WUb.b, h2c.b], [psu.b])
                                    sg = sgt[c % 2]
                                    P.act(sg[:, 0:SUB], psg[:, 0:SUB], AF.Silu, [psg.b], [sg.b])
                                    P.tt("dve", he[:, c * 512:c * 512 + SUB], sg[:, 0:SUB], psu[:, 0:SUB], ALU.mult, [sg.b, psu.b], [he.b])
                                for t_ in range(SUB // 128):
                                    tile = st_ * (SUB // 128) + t_
                                    gt = ch * NT + tile
                                    for dt in range(4):
                                        psd = psbank()
                                        for c in range(4):
                                            P.mm(psd[:, :], he[:, c * 512 + t_ * 128:c * 512 + (t_ + 1) * 128], WDb[:, c * 2048 + dt * 512:c * 2048 + (dt + 1) * 512],
                                                 c == 0, c == 3, [he.b, WDb.b], [psd.b])
                                        ya = yacc[:, tile * 2048 + dt * 512:tile * 2048 + (dt + 1) * 512]
                                        P.stt(ya, psd[:, :], comb[:, gt * 32 + e:gt * 32 + e + 1], ya, ALU.mult, ALU.add, [psd.b, comb.b, yacc.b], [yacc.b])
                        GT2b = stg[0]; GFb = stg[1]
                        P.dma(GT2b[:], mod6[5:6, :].partition_broadcast(128), [modD], [GT2b.b])
                        P.dma(GFb[:], g_fin[0:1, :].partition_broadcast(128), [], [GFb.b])
                        for tile in range(NT):
                            gt = ch * NT + tile
                            P.dma(xf[:], x1_d[gt * 128:(gt + 1) * 128, :], [X1D], [xf.b])
                            ya = yacc[:, tile * 2048:(tile + 1) * 2048]
                            P.tt("dve", ya, ya, GT2b[:], ALU.mult, [yacc.b, GT2b.b], [yacc.b])
                            P.tt("dve", xf[:], xf[:], ya, ALU.add, [xf.b, yacc.b], [xf.b])
                            P.act(ya, xf[:], AF.Square, [xf.b], [yacc.b, st4.b], accum_out=st4[:, 0:1])
                            rms_rstd(st4[:, 0:1], st4[:, 1:2], 1, 1.0 / D, [st4.b], [st4.b])
                            P.stt(xf[:], xf[:], st4[:, 1:2], GFb[:], ALU.mult, ALU.mult, [xf.b, st4.b, GFb.b], [xf.b])
                            P.dma(out_d[gt * 128:(gt + 1) * 128, :], xf[:], [xf.b], [OUTD], semb=OUTD)


        try:
            body()
        except _Stop:
            pass
        for b_ in P.dmabufs:
            P.final.append((b_.sem, b_.cnt))
        P.replay()
    return nc, dbg_out


def host_consts():
    c = np.zeros((128, 1024), np.float32)
    i = np.arange(128)
    c[:, 0:128] = np.eye(128)
    same = (i[:, None] // 64) == (i[None, :] // 64)
    c[:, 128:256] = (same & (i[:, None] <= i[None, :])).astype(np.float32)
    c[:, 256:384] = (same & (i[:, None] > i[None, :])).astype(np.float32)
    c[:, 384] = (i < 64)
    c[:, 385] = (i >= 64)
    c[:, 512:640] = np.where(i[None, :] <= i[:, None], 0.0, -1e30)
    c[:, 640:768] = np.eye(128)
    return c


def make_in_maps(inputs, S, ne=NE):
    f = lambda a: np.ascontiguousarray(np.asarray(a, dtype=np.float32))
    x = f(inputs["x"]); c = f(inputs["c"])
    NB = S // 128; NO = NB // 4
    pT = lambda v: np.ascontiguousarray(v.reshape(-1, 128).T)
    shared = {
        "w_ada": f(inputs["w_ada"][0]), "badaT": pT(f(inputs["b_ada"][0])),
        "gnmT": pT(f(inputs["g_norm_mix"][0])), "gnfT": pT(f(inputs["g_norm_ffn"][0])),
        "w_in": f(inputs["w_in"][0]), "lbl": f(inputs["lb_logits"]),
        "g_rec": f(inputs["g_rec_out"]), "g_att": f(inputs["g_att_out"]),
        "w_out": f(inputs["w_out"][0]),
        "w_rt": np.ascontiguousarray(np.concatenate([f(inputs["w_router_group"][0]), f(inputs["w_router_expert"][0])], axis=1)),
        "b_rt": np.ascontiguousarray(np.concatenate([f(inputs["b_router_group"][0]), f(inputs["b_router_expert"][0])])[None, :]),
        "w_eg": f(inputs["w_expert_gate"][0][:ne]), "w_eu": f(inputs["w_expert_up"][0][:ne]), "w_ed": f(inputs["w_expert_down"][0][:ne]),
        "g_fin": f(inputs["g_final"])[None, :], "cst": host_consts(),
    }
    maps = []
    for core in range(8):
        b, j = core // 4, core % 4
        npad = (3 - j) * 128
        xs = np.zeros((S, D), np.float32)
        xs[npad:] = x[b, :S - npad]
        valid = np.ones(S, np.float32); valid[:npad] = 0
        padb = np.zeros((1, 512), np.float32); padb[0, :npad] = -1e30
        nval = np.zeros((128, NO), np.float32)
        for i in range(NO):
            nval[:, i] = (4 * i + 3) * 128 + np.arange(128) - npad + 1
        m = dict(shared)
        m.update({"x_sh": xs, "cT": pT(c[b]), "validT": pT(valid), "padb": padb, "nvalT": nval})
        maps.append(m)
    return maps


def assemble(results, S, B=2):
    NB = S // 128; NO = NB // 4
    out = np.zeros((B, S, D), np.float32)
    for core in range(8):
        b, j = core // 4, core % 4
        o = np.asarray(results[core]["out"]).reshape(NO, 128, D)
        for i in range(NO):
            g = 4 * i + j
            out[b, g * 128:(g + 1) * 128] = o[i]
    return out


_CACHE = {}


def kernel(**inputs):
    S = int(np.asarray(inputs["x"]).shape[1])
    if S not in _CACHE:
        _CACHE[S] = build(S)[0]
    nc = _CACHE[S]
    maps = make_in_maps(inputs, S)
    res = run_bass_kernel_spmd(nc, maps, core_ids=list(range(8)))
    return assemble(res.results, S)
```

```python
import contextlib
import numpy as np
import ml_dtypes
import concourse.bass as bass
import concourse.mybir as mybir
from concourse.bass_utils import run_bass_kernel_spmd

F32 = mybir.dt.float32
BF16 = mybir.dt.bfloat16
AF = mybir.ActivationFunctionType
ALU = mybir.AluOpType
AX = mybir.AxisListType

D = 2048
KC = 16
IN_COLS = 6216
NE = 32
DE = 512
EPS = 1e-6
NEG = -30000.0
SAME_ENGINE_SYNC = True
C_RQ, C_RF, C_RI, C_RG, C_AQ, C_AK, C_AV, C_IQ, C_IK, C_IW = 0, 1024, 2048, 3072, 4096, 5120, 5376, 5632, 6144, 6208


class Buf:
    __slots__ = ("name", "w", "r", "sem", "cnt")

    def __init__(self, name):
        self.name = name
        self.w = None
        self.r = []
        self.sem = None
        self.cnt = 0


class Prog:
    ENGS = ("pe", "act", "dve", "pool", "sp")

    def __init__(self, nc, es):
        self.nc = nc
        self.es = es
        self.q = {e: [] for e in self.ENGS}
        self.cnt = {e: 0 for e in self.ENGS}
        self.esem = {e: es.enter_context(nc.semaphore("sem_" + e)) for e in ("pe", "act", "dve", "pool")}
        self.seen = {e: {} for e in self.ENGS}
        self.nsem = 0
        self.final = []
        self.dmabufs = []
        self.stopped = False
        self.barrier = []

    def set_barrier(self):
        toks = [(e, self.esem[e], self.cnt[e]) for e in ("pe", "act", "dve", "pool") if self.cnt[e] > 0]
        for b in self.dmabufs:
            toks.append(("d%d" % id(b), b.sem, b.cnt))
        self.barrier = toks

    def mkbuf(self, name):
        b = Buf(name)
        b.r = list(self.barrier)
        return b

    def newsem(self, name):
        self.nsem += 1
        return self.es.enter_context(self.nc.semaphore("d_%s_%d" % (name, self.nsem)))

    def _waits(self, eng, R, W):
        toks = []
        for b in R:
            if b.w is not None:
                toks.append(b.w)
        for b in W:
            if b.w is not None:
                toks.append(b.w)
            toks.extend(b.r)
        out = {}
        for (key, sem, v) in toks:
            if key == eng and (eng == "pe" or not SAME_ENGINE_SYNC):
                continue
            if self.seen[eng].get(key, 0) >= v:
                continue
            if out.get(key, (None, 0))[1] < v:
                out[key] = (sem, v)
        for key, (sem, v) in out.items():
            self.seen[eng][key] = v
        return list(out.values())

    def emit(self, eng, fn, R=(), W=(), inc=True):
        if self.stopped:
            return
        waits = self._waits(eng, R, W)
        sem = self.esem[eng]
        if inc:
            self.cnt[eng] += 1
            c = self.cnt[eng]
            self.q[eng].append((waits, fn, sem, 1))
        else:
            c = self.cnt[eng] + 1
            self.q[eng].append((waits, fn, sem, 0))
        tok = (eng, sem, c)
        for b in W:
            b.w = tok
            b.r = []
        for b in R:
            if b not in W:
                b.r.append(tok)

    def dma(self, out, in_, R, W, semb=None, q="sp"):
        semb = semb or W[0]
        if self.stopped:
            return (None, None, 0)
        if semb.sem is None:
            semb.sem = self.newsem(semb.name)
            self.dmabufs.append(semb)
        waits = self._waits(q, R, W)
        semb.cnt += 16
        nc = self.nc
        eng = {"sp": nc.sync, "act": nc.scalar, "pool": nc.gpsimd}[q]
        self.q[q].append((waits, lambda: eng.dma_start(out=out, in_=in_), semb.sem, 16))
        tok = ("d%d" % id(semb), semb.sem, semb.cnt)
        for b in W:
            b.w = tok
            b.r = []
        for b in R:
            b.r.append(tok)
        return tok

    def act(self, out, in_, func, R, W, **kw):
        nc = self.nc
        self.emit("act", lambda: nc.scalar.activation(out=out, in_=in_, func=func, **kw), R, W)

    def ts(self, eng, out, in0, s1, s2, op0, op1, R, W, **kw):
        e = self.nc.vector if eng == "dve" else self.nc.gpsimd
        if op1 is None:
            self.emit(eng, lambda: e.tensor_scalar(out=out, in0=in0, scalar1=s1, scalar2=None, op0=op0, **kw), R, W)
        else:
            self.emit(eng, lambda: e.tensor_scalar(out=out, in0=in0, scalar1=s1, scalar2=s2, op0=op0, op1=op1, **kw), R, W)

    def tt(self, eng, out, in0, in1, op, R, W):
        e = self.nc.vector if eng == "dve" else self.nc.gpsimd
        self.emit(eng, lambda: e.tensor_tensor(out=out, in0=in0, in1=in1, op=op), R, W)

    def stt(self, out, in0, scalar, in1, op0, op1, R, W):
        nc = self.nc
        self.emit("dve", lambda: nc.vector.scalar_tensor_tensor(out=out, in0=in0, scalar=scalar, in1=in1, op0=op0, op1=op1), R, W)

    def copy(self, eng, out, in_, R, W):
        nc = self.nc
        if eng == "act":
            self.emit("act", lambda: nc.scalar.copy(out=out, in_=in_), R, W)
        elif eng == "dve":
            self.emit("dve", lambda: nc.vector.tensor_copy(out=out, in_=in_), R, W)
        else:
            self.emit("pool", lambda: nc.gpsimd.tensor_copy(out=out, in_=in_), R, W)

    def mm(self, out, lhsT, rhs, start, stop, R, W, inc=None, **kw):
        nc = self.nc
        if inc is None:
            inc = bool(stop)
        self.emit("pe", lambda: nc.tensor.matmul(out, lhsT, rhs, start=start, stop=stop, **kw), R, W, inc=inc)

    def tr(self, out, in_, ident, R, W, inc=True):
        nc = self.nc
        self.emit("pe", lambda: nc.tensor.transpose(out, in_, ident), R, W, inc=inc)

    def replay(self):
        nc = self.nc
        engmap = {"pe": "tensor", "act": "scalar", "dve": "vector", "pool": "gpsimd", "sp": "sync"}
        with nc.Block() as blk:
            for e in self.ENGS:
                lst = self.q[e]
                final = self.final

                def body(eng, lst=lst, e=e):
                    for (waits, fn, sem, n) in lst:
                        for (s, v) in waits:
                            eng.wait_ge(s, v)
                        if n:
                            fn().then_inc(sem, n)
                        else:
                            fn()
                    if e == "sp":
                        for (s, v) in final:
                            eng.wait_ge(s, v)

                getattr(blk, engmap[e])(body)


class T:
    def __init__(self, P, kind, name, shape, dtype):
        nc = P.nc
        if kind == "sb":
            self.t = P.es.enter_context(nc.sbuf_tensor(name, shape, dtype))
        else:
            self.t = P.es.enter_context(nc.psum_tensor(name, shape, dtype))
        self.b = Buf(name)

    def __getitem__(self, k):
        return self.t[k]


def build(S, dbg=None):
    NB = S // 128
    NSB = NB // 4
    NO = NSB
    TO = NO * 128
    KSEL = min(256, S // 4)
    dbg = dbg or ()
    nc = bass.Bass("TRN2", target_bir_lowering=False)

    def din(name, shape, dt=F32):
        return nc.dram_tensor(name, list(shape), dt, kind="ExternalInput").ap()

    def dscr(name, shape, dt):
        return nc.dram_tensor(name, list(shape), dt, kind="Internal").ap()

    x_sh = din("x_sh", [S, D])
    cT = din("cT", [128, KC])
    w_ada = din("w_ada", [D, 6 * D])
    badaT = din("badaT", [128, 96])
    gnmT = din("gnmT", [128, KC])
    gnfT = din("gnfT", [128, KC])
    w_in = din("w_in", [D, IN_COLS])
    lbl = din("lbl", [2, 1024])
    g_rec = din("g_rec", [1, 1024])
    g_att = din("g_att", [1, 1024])
    w_out = din("w_out", [D, D])
    w_rt = din("w_rt", [D, 36])
    b_rt = din("b_rt", [1, 36])
    ne_decl = 1 if any(d.startswith("stop") for d in dbg) else NE
    w_eg = din("w_eg", [ne_decl, D, DE])
    w_eu = din("w_eu", [ne_decl, D, DE])
    w_ed = din("w_ed", [ne_decl, DE, D])
    g_fin = din("g_fin", [1, D])
    validT = din("validT", [128, NB])
    padb = din("padb", [1, 512])
    nvalT = din("nvalT", [128, NO])
    cst = din("cst", [128, 8 * 128])
    out_d = nc.dram_tensor("out", [TO, D], F32, kind="ExternalOutput").ap()

    win_bf = dscr("win_bf", [D, IN_COLS], BF16)
    mod_d = dscr("mod_d", [96 * 128], F32)
    KT_d = dscr("KT_d", [2, 128, S], BF16)
    V_d = dscr("V_d", [S, 256], BF16)
    kiT_d = dscr("kiT_d", [128, S], BF16)
    aqT_d = dscr("aqT_d", [8, 128, TO], BF16)
    iqT_d = dscr("iqT_d", [4, 128, TO], BF16)
    sgn_d = dscr("sgn_d", [TO, 8], F32)
    mixT_d = dscr("mixT_d", [D, TO], BF16)
    x1_d = dscr("x1_d", [TO, D], F32)

    dbg_out = {}

    class _Stop(Exception):
        pass

    es = contextlib.ExitStack()
    with es:
        P = Prog(nc, es)
        allbufs = []

        def stop(tag):
            if tag in dbg:
                P.stopped = True

        def body():

            def sb(name, shape, dt=F32):
                return T(P, "sb", name, shape, dt)

            PS = [T(P, "ps", "ps%d" % i, [128, 512], F32) for i in range(8)]
            psrot = [0]
            psmod = [6]

            def psbank():
                t = PS[psrot[0] % psmod[0]]
                psrot[0] += 1
                return t

            def bfv(ps):
                return ps.t[:, :].bitcast(BF16)

            def dump(name, tile, shape, dt=F32):
                if name not in dbg:
                    return
                o = nc.dram_tensor("dbg_" + name, list(shape), dt, kind="ExternalOutput").ap()
                b = Buf("dbg_" + name)
                tok = P.dma(o, tile.t[:] if isinstance(tile, T) else tile[0], [tile.b if isinstance(tile, T) else tile[1]], [b])
                P.final.append((tok[1], tok[2]))
                dbg_out[name] = (shape, dt)

            cstf = sb("cstf", [128, 8 * 128])
            P.dma(cstf[:], cst[:, :], [], [cstf.b])
            ident_f = cstf[:, 0:128]
            TL = cstf[:, 128:256]
            TU = cstf[:, 256:384]
            CI = cstf[:, 384:386]
            CB = cstf[:, 512:640]
            cstb = sb("cstb", [128, 8 * 128], BF16)
            P.copy("dve", cstb[:], cstf[:], [cstf.b], [cstb.b])
            ident = cstb[:, 0:128]
            MBD = cstf[:, 128:256]
            I4 = cstb[:, 640:640 + 128]

            cTt = sb("cTt", [128, KC])
            P.dma(cTt[:], cT[:, :], [], [cTt.b])
            cact = sb("cact", [128, KC])
            P.act(cact[:], cTt[:], AF.Silu, [cTt.b], [cact.b])
            modps = psbank()
            modT = sb("modT", [128, 96])
            bT = sb("bT", [128, 96])
            modTT = sb("modTT", [96, 128])
            g1t = sb("g1t", [128, KC]); g2t = sb("g2t", [128, KC])
            G1s = sb("G1s", [128, KC]); G2s = sb("G2s", [128, KC])
            with contextlib.ExitStack() as es0:
                wad = [T.__new__(T) for _ in range(2)]
                for i, w in enumerate(wad):
                    w.t = es0.enter_context(nc.sbuf_tensor("wad%d" % i, [128, KC, 512], F32))
                    w.b = Buf("wad%d" % i)
                w_ada_v = w_ada.rearrange("(kc p) c -> p kc c", p=128)
                modrow = T.__new__(T)
                modrow.t = es0.enter_context(nc.sbuf_tensor("modrow", [1, 6 * D], F32))
                modrow.b = Buf("modrow")
                for n in range(24):
                    w = wad[n % 2]
                    P.dma(w[:], w_ada_v[:, :, n * 512:(n + 1) * 512], [], [w.b])
                    psr_ = psbank()
                    for kc in range(KC):
                        P.mm(psr_[0:1, :], cact[:, kc:kc + 1], w[:, kc, :], kc == 0, kc == KC - 1, [w.b, cact.b], [psr_.b])
                    P.copy("act" if n % 2 == 0 else "dve", modrow[0:1, n * 512:(n + 1) * 512], psr_[0:1, :], [psr_.b], [modrow.b])
                for m in range(96):
                    P.mm(modps[:, m:m + 1], modrow[0:1, m * 128:(m + 1) * 128], ident_f[0:1, 0:1], True, True, [modrow.b, cstf.b], [modps.b],
                         inc=(m == 95))
                P.dma(bT[:], badaT[:, :], [], [bT.b])
                P.tt("dve", modT[:], modps[:, 0:96], bT[:], ALU.add, [modps.b, bT.b], [modT.b])
                dump("modT", modT, [128, 96])
                P.dma(g1t[:], gnmT[:, :], [], [g1t.b])
                P.dma(g2t[:], gnfT[:, :], [], [g2t.b])
                P.stt(G1s[:], modT[:, 16:32], 1.0, g1t[:], ALU.add, ALU.mult, [modT.b, g1t.b], [G1s.b])
                P.stt(G2s[:], modT[:, 64:80], 1.0, g2t[:], ALU.add, ALU.mult, [modT.b, g2t.b], [G2s.b])
                SH1s = modT[:, 0:16]
                SH2s = modT[:, 48:64]
                modD = Buf("mod_d")
                pmt = psbank()
                P.tr(pmt[0:96, 0:128], modT[:], ident_f, [modT.b, cstf.b], [pmt.b])
                P.copy("dve", modTT[:], pmt[0:96, 0:128], [pmt.b], [modTT.b])
                P.dma(mod_d.rearrange("(m p) -> m p", p=128), modTT[:], [modTT.b], [modD])

                winD = Buf("win_bf")
                wst = []
                wsb = []
                for i in range(2):
                    a = T.__new__(T); a.t = es0.enter_context(nc.sbuf_tensor("wst%d" % i, [128, IN_COLS], F32)); a.b = Buf("wst%d" % i)
                    c_ = T.__new__(T); c_.t = es0.enter_context(nc.sbuf_tensor("wsb%d" % i, [128, IN_COLS], BF16)); c_.b = Buf("wsb%d" % i)
                    wst.append(a); wsb.append(c_)
                for kc in range(KC):
                    a = wst[kc % 2]; c_ = wsb[kc % 2]
                    P.dma(a[:], w_in[kc * 128:(kc + 1) * 128, :], [], [a.b])
                    h = IN_COLS // 2
                    P.copy("act", c_[:, 0:h], a[:, 0:h], [a.b], [c_.b])
                    P.copy("pool", c_[:, h:], a[:, h:], [a.b], [c_.b])
                    P.dma(win_bf[kc * 128:(kc + 1) * 128, :], c_[:], [c_.b], [winD], semb=winD)

            stop("stop0")
            P.set_barrier()
            with contextlib.ExitStack() as es1:
                def sb1(name, shape, dt=F32):
                    t = T.__new__(T)
                    t.t = es1.enter_context(nc.sbuf_tensor(name, shape, dt))
                    t.b = P.mkbuf(name)
                    return t

                LB = sb1("LB", [128, 1024]); OMLB = sb1("OMLB", [128, 1024]); GS = sb1("GS", [128, 1024]); l1 = GS
                P.dma(LB[:], lbl[0:1, :].partition_broadcast(128), [], [LB.b])
                P.dma(l1[:], lbl[1:2, :].partition_broadcast(128), [], [l1.b])
                P.tt("dve", LB[:], LB[:], l1[:], ALU.subtract, [LB.b, l1.b], [LB.b])
                P.act(LB[:], LB[:], AF.Sigmoid, [LB.b], [LB.b])
                P.ts("dve", OMLB[:], LB[:], -1.0, 1.0, ALU.mult, ALU.add, [LB.b], [OMLB.b])
                GR = sb1("GR", [128, 1024])
                P.dma(GR[:], g_rec[0:1, :].partition_broadcast(128), [], [GR.b])
                vT = sb1("vT", [128, NB])
                P.dma(vT[:], validT[:, :], [], [vT.b])

                xt = [sb1("xt%d" % i, [128, D]) for i in range(2)]
                junk = sb1("junk", [128, 128], BF16)
                xn = [sb1("xn%d" % i, [128, D], BF16) for i in range(2)]
                st = sb1("st", [128, 8])
                hT = sb1("hT", [128, KC, 512], BF16)
                wt = [sb1("wt%d" % i, [128, KC, 512], BF16) for i in range(2)]
                wrot = [0]
                sgf = sb1("sgf", [128, 4, 1024])
                lgf = sb1("lgf", [128, 4, 1024])
                vbf = sb1("vbf", [128, 4, 1024], BF16)
                kvt = sb1("kvt", [128, 4, 512], BF16)
                ikt = sb1("ikt", [128, 4, 128], BF16)
                qs = sb1("qs", [128, 1024]); gs = sb1("gs", [128, 1024])
                aqt = sb1("aqt", [128, 1024], BF16)
                iqt = sb1("iqt", [128, 512]); iwt = sb1("iwt", [128, 8])
                eR = sb1("eR", [128, 1024]); eB = sb1("eB", [128, 1024])
                ktb = sb1("ktb", [128, 1024], BF16)
                qtb = sb1("qtb", [128, 1024], BF16); qtb1 = sb1("qtb1", [128, 1024], BF16); khb = sb1("khb", [128, 1024], BF16)
                dec = sb1("dec", [128, 16])
                Sst = sb1("Sst", [128, 1024])
                Sbf = [sb1("Sbf%d" % i, [128, 1024], BF16) for i in range(2)]
                qT0 = sb1("qT0", [128, 1024], BF16); qT1 = sb1("qT1", [128, 1024], BF16)
                khT = sb1("khT", [128, 1024], BF16)
                pT = [sb1("pT%d" % i, [128, 128], BF16) for i in range(2)]
                ssq = sb1("ssq", [128, 8]); rsq = sb1("rsq", [128, 8])
                recb = sb1("recb", [128, 1024], BF16)
                trs = sb1("trs", [128, 1536], BF16)
                recT = sb1("recT", [128, 1024], BF16)
                aqT = sb1("aqT", [128, 1024], BF16)
                iqs = sb1("iqs", [128, 512], BF16)
                iqT = sb1("iqT", [128, 512], BF16)
                aw = sb1("aw", [128, 8]); sgn = sb1("sgn", [128, 8])
                P.emit("pool", lambda: nc.gpsimd.memset(Sst[:], 0.0), [], [Sst.b])
                P.emit("pool", lambda: nc.gpsimd.memset(qtb[:], 0.0), [], [qtb.b])
                P.emit("pool", lambda: nc.gpsimd.memset(qtb1[:], 0.0), [], [qtb1.b])

                KTD = Buf("KT_d"); VD = Buf("V_d"); KID = Buf("kiT_d"); AQD = Buf("aqT_d"); IQD = Buf("iqT_d")
                SGD = Buf("sgn_d"); MIXD = Buf("mixT_d")
                win_v = win_bf.rearrange("(kc p) c -> p kc c", p=128)

                def rms_rstd(ssap, outap, n, scale, R, W):
                    P.ts("dve", outap, ssap, scale, EPS, ALU.mult, ALU.add, R, W)
                    P.act(outap, outap, AF.Sqrt, W, W)
                    P.emit("dve", lambda: nc.vector.reciprocal(out=outap, in_=outap), W, W)

                def proj_tile(c0, ncols, blks, evac):
                    w = wt[wrot[0] % 2]; wrot[0] += 1
                    P.dma(w[:, :, 0:ncols], win_v[:, :, c0:c0 + ncols], [winD], [w.b])
                    for blk in blks:
                        ps = psbank()
                        for kc in range(KC):
                            P.mm(ps[:, 0:ncols], hT[:, kc, blk * 128:(blk + 1) * 128], w[:, kc, 0:ncols],
                                 kc == 0, kc == KC - 1, [hT.b, w.b], [ps.b])
                        evac(ps, blk)

                def secA(sbi):
                    for blk in range(4):
                        p = sbi * 4 + blk
                        x_ = xt[p % 2]; xn_ = xn[p % 2]
                        P.dma(x_[:], x_sh[p * 128:(p + 1) * 128, :], [], [x_.b])
                        P.act(xn_[:], x_[:], AF.Square, [x_.b], [xn_.b, st.b], accum_out=st[:, 0:1])
                        rms_rstd(st[:, 0:1], st[:, 1:2], 1, 1.0 / D, [st.b], [st.b])
                        P.ts("dve", xn_[:], x_[:], st[:, 1:2], None, ALU.mult, None, [x_.b, st.b], [xn_.b])
                        for half in range(2):
                            ps = psbank()
                            pv = bfv(ps)
                            for k8 in range(8):
                                kc = half * 8 + k8
                                P.tr(pv[:, k8 * 128:(k8 + 1) * 128], xn_[:, kc * 128:(kc + 1) * 128], ident, [xn_.b, cstb.b], [ps.b])
                            for k8 in range(8):
                                kc = half * 8 + k8
                                o = hT[:, kc, blk * 128:(blk + 1) * 128]
                                i_ = pv[:, k8 * 128:(k8 + 1) * 128]
                                if k8 % 2 == 0:
                                    P.act(o, i_, AF.Identity, [ps.b, G1s.b, modT.b], [hT.b], scale=G1s[:, kc:kc + 1], bias=SH1s[:, kc:kc + 1])
                                else:
                                    P.ts("dve", o, i_, G1s[:, kc:kc + 1], SH1s[:, kc:kc + 1], ALU.mult, ALU.add, [ps.b, G1s.b, modT.b], [hT.b])
                    if sbi == 0:
                        dump("hT", hT, [128, KC, 512], BF16)
                def secB(sbi):
                    for ti in range(2):
                        proj_tile(C_RF + ti * 512, 512, range(4),
                                  lambda ps, blk, ti=ti: P.act(sgf[:, blk, ti * 512:(ti + 1) * 512], ps[:, :], AF.Sigmoid, [ps.b], [sgf.b]))
                    for blk in range(4):
                        P.tt("dve", sgf[:, blk, :], sgf[:, blk, :], OMLB[:], ALU.mult, [sgf.b, OMLB.b], [sgf.b])
                        P.tt("dve", sgf[:, blk, :], sgf[:, blk, :], LB[:], ALU.add, [sgf.b, LB.b], [sgf.b])
                        P.act(lgf[:, blk, :], sgf[:, blk, :], AF.Ln, [sgf.b], [lgf.b])
                        P.ts("pool", sgf[:, blk, :], sgf[:, blk, :], -1.0, 1.0, ALU.mult, ALU.add, [sgf.b, lgf.b], [sgf.b])
                    for ti in range(2):
                        proj_tile(C_RI + ti * 512, 512, range(4),
                                  lambda ps, blk, ti=ti: P.copy("act", vbf[:, blk, ti * 512:(ti + 1) * 512], ps[:, :], [ps.b], [vbf.b]))
                    proj_tile(C_AK, 512, range(4), lambda ps, blk: P.copy("dve", kvt[:, blk, :], ps[:, :], [ps.b], [kvt.b]))

                    def ev_ik(ps, blk):
                        P.copy("act", ikt[:, blk, 0:64], ps[:, 0:64], [ps.b], [ikt.b])
                        P.copy("act", ikt[:, blk, 64:128], ps[:, 0:64], [ps.b], [ikt.b])
                    proj_tile(C_IK, 64, range(4), ev_ik)
                    for ti in range(2):
                        proj_tile(C_RQ + ti * 512, 512, [3],
                                  lambda ps, blk, ti=ti: P.act(qs[:, ti * 512:(ti + 1) * 512], ps[:, :], AF.Silu, [ps.b], [qs.b]))
                    for ti in range(2):
                        proj_tile(C_RG + ti * 512, 512, [3],
                                  lambda ps, blk, ti=ti: P.act(gs[:, ti * 512:(ti + 1) * 512], ps[:, :], AF.Silu, [ps.b], [gs.b]))
                    for ti in range(2):
                        proj_tile(C_AQ + ti * 512, 512, [3],
                                  lambda ps, blk, ti=ti: P.copy("dve", aqt[:, ti * 512:(ti + 1) * 512], ps[:, :], [ps.b], [aqt.b]))
                    proj_tile(C_IQ, 512, [3], lambda ps, blk: P.copy("dve", iqt[:], ps[:, :], [ps.b], [iqt.b]))
                    proj_tile(C_IW, 8, [3], lambda ps, blk: P.copy("dve", iwt[:], ps[:, 0:8], [ps.b], [iwt.b]))

                def secD(sbi):
                    for blk in range(4):
                        p = sbi * 4 + blk
                        own = (blk == 3)
                        for ti in range(2):
                            ps = psbank()
                            P.mm(ps[:, :], TU, lgf[:, blk, ti * 512:(ti + 1) * 512], True, True, [cstf.b, lgf.b], [ps.b])
                            P.act(eR[:, ti * 512:(ti + 1) * 512], ps[:, :], AF.Exp, [ps.b], [eR.b])
                        P.stt(ktb[:], sgf[:, blk, :], vT[:, p:p + 1], eR[:], ALU.mult, ALU.mult, [sgf.b, vT.b, eR.b], [ktb.b])
                        psd = psbank()
                        for hd in range(8):
                            P.mm(psd[:, 2 * hd:2 * hd + 2], lgf[:, blk, hd * 128:(hd + 1) * 128], CI, True, True, [lgf.b, cstf.b], [psd.b])
                        P.act(dec[:], psd[:, 0:16], AF.Exp, [psd.b], [dec.b])
                        stop("stopD1")
                        if own:
                            for ti in range(2):
                                ps = psbank()
                                P.mm(ps[:, :], TL, lgf[:, blk, ti * 512:(ti + 1) * 512], True, True, [cstf.b, lgf.b], [ps.b])
                                P.act(eB[:, ti * 512:(ti + 1) * 512], ps[:, :], AF.Exp, [ps.b], [eB.b])
                                P.act(eR[:, ti * 512:(ti + 1) * 512], ps[:, :], AF.Exp, [ps.b, ktb.b], [eR.b], scale=-1.0)
                            P.stt(qtb[0:64, :], qs[0:64, :], 128.0 ** -0.5, eB[0:64, :], ALU.mult, ALU.mult, [qs.b, eB.b], [qtb.b])
                            P.stt(qtb1[64:128, :], qs[64:128, :], 128.0 ** -0.5, eB[64:128, :], ALU.mult, ALU.mult, [qs.b, eB.b], [qtb1.b])
                            P.tt("dve", khb[:], sgf[:, blk, :], eR[:], ALU.mult, [sgf.b, eR.b], [khb.b])
                            stop("stopD3a")
                            psq = psbank(); psk = psbank()
                            pq = bfv(psq); pk = bfv(psk)
                            for hd in range(8):
                                P.tr(pq[:, hd * 128:(hd + 1) * 128], qtb[:, hd * 128:(hd + 1) * 128], ident, [qtb.b, cstb.b], [psq.b])
                            for hd in range(8):
                                P.tr(pk[:, hd * 128:(hd + 1) * 128], khb[:, hd * 128:(hd + 1) * 128], ident, [khb.b, cstb.b], [psk.b])
                            psq1 = psbank(); pq1 = bfv(psq1)
                            for hd in range(8):
                                P.tr(pq1[:, hd * 128:(hd + 1) * 128], qtb1[:, hd * 128:(hd + 1) * 128], ident, [qtb1.b, cstb.b], [psq1.b])
                            stop("stopD3b")
                            P.copy("act", qT0[:], pq[:, :], [psq.b], [qT0.b])
                            P.copy("dve", qT1[:], pq1[:, :], [psq1.b], [qT1.b])
                            P.copy("act", khT[:], pk[:, :], [psk.b], [khT.b])
                            stop("stopD3")
                        for c in range(2):
                            if own:
                                P.copy("act" if c == 0 else "pool", Sbf[c][:], Sst[:], [Sst.b], [Sbf[c].b])
                            for hh in range(2):
                                ps = psbank()
                                for h4 in range(4):
                                    hd = hh * 4 + h4
                                    P.mm(ps[:, h4 * 128:(h4 + 1) * 128], ktb[c * 64:(c + 1) * 64, hd * 128:(hd + 1) * 128],
                                         vbf[c * 64:(c + 1) * 64, blk, hd * 128:(hd + 1) * 128], True, True, [ktb.b, vbf.b], [ps.b])
                                for h4 in range(4):
                                    hd = hh * 4 + h4
                                    P.stt(Sst[:, hd * 128:(hd + 1) * 128], Sst[:, hd * 128:(hd + 1) * 128], dec[:, 2 * hd + c:2 * hd + c + 1], ps[:, h4 * 128:(h4 + 1) * 128],
                                          ALU.mult, ALU.add, [Sst.b, dec.b, ps.b], [Sst.b])
                        stop("stopD2")
                        if own:
                            ob = sbi
                            pso = [PS[6], PS[7]]
                            for hd in range(8):
                                pss = psbank()
                                P.mm(pss[:, 0:128], khT[:, hd * 128:(hd + 1) * 128], qT0[:, hd * 128:(hd + 1) * 128], True, False, [khT.b, qT0.b], [pss.b])
                                P.mm(pss[:, 0:128], khT[:, hd * 128:(hd + 1) * 128], qT1[:, hd * 128:(hd + 1) * 128], False, True, [khT.b, qT1.b], [pss.b])
                                pt = pT[hd % 2]
                                P.tt("dve", pt[:], pss[:, 0:128], MBD, ALU.mult, [pss.b, cstf.b], [pt.b])
                                po = pso[hd // 4]
                                oo = po[:, (hd % 4) * 128:(hd % 4 + 1) * 128]
                                P.mm(oo, pt[:], vbf[:, blk, hd * 128:(hd + 1) * 128], True, False, [pt.b, vbf.b], [po.b])
                                P.mm(oo, qT0[:, hd * 128:(hd + 1) * 128], Sbf[0][:, hd * 128:(hd + 1) * 128], False, False, [qT0.b, Sbf[0].b], [po.b])
                                P.mm(oo, qT1[:, hd * 128:(hd + 1) * 128], Sbf[1][:, hd * 128:(hd + 1) * 128], False, True, [qT1.b, Sbf[1].b], [po.b])
                            stop("stopD3c")
                            for hd in range(8):
                                po = pso[hd // 4]
                                P.act(junk[:, 0:128], po[:, (hd % 4) * 128:(hd % 4 + 1) * 128], AF.Square, [po.b], [junk.b, ssq.b],
                                      accum_out=ssq[:, hd:hd + 1])
                            stop("stopD3d")
                            rms_rstd(ssq[:], rsq[:], 8, 1.0 / 128, [ssq.b], [rsq.b])
                            P.tt("dve", GS[:], gs[:], GR[:], ALU.mult, [gs.b, GR.b], [GS.b])
                            for hd in range(8):
                                po = pso[hd // 4]
                                P.stt(recb[:, hd * 128:(hd + 1) * 128], po[:, (hd % 4) * 128:(hd % 4 + 1) * 128], rsq[:, hd:hd + 1],
                                      GS[:, hd * 128:(hd + 1) * 128], ALU.mult, ALU.mult, [po.b, rsq.b, GS.b], [recb.b])
                            if sbi == 0:
                                dump("rec0", recb, [128, 1024], BF16)
                            stop("stopD4")
                            psr = psbank(); pr = bfv(psr)
                            for hd in range(8):
                                P.tr(pr[:, hd * 128:(hd + 1) * 128], recb[:, hd * 128:(hd + 1) * 128], ident, [recb.b, cstb.b], [psr.b])
                            P.copy("act", recT[:], pr[:, :], [psr.b], [recT.b])
                            P.dma(mixT_d[0:1024, ob * 128:(ob + 1) * 128].rearrange("(h p) t -> p h t", p=128), recT[:].rearrange("p (h t) -> p h t", h=8), [recT.b], [MIXD], semb=MIXD)
                            psa = psbank(); pa = bfv(psa)
                            for hd in range(8):
                                P.tr(pa[:, hd * 128:(hd + 1) * 128], aqt[:, hd * 128:(hd + 1) * 128], ident, [aqt.b, cstb.b], [psa.b])
                            P.copy("act", aqT[:], pa[:, :], [psa.b], [aqT.b])
                            P.dma(aqT_d[:, :, ob * 128:(ob + 1) * 128].rearrange("h p t -> p h t"), aqT[:].rearrange("p (h t) -> p h t", h=8), [aqT.b], [AQD], semb=AQD)
                            P.act(aw[:], iwt[:], AF.Abs, [iwt.b], [aw.b], scale=64.0 ** -0.5 * 8.0 ** -0.5)
                            P.ts("dve", sgn[:], iwt[:], 0.0, 2.0, ALU.is_ge, ALU.mult, [iwt.b], [sgn.b])
                            P.ts("dve", sgn[:], sgn[:], -1.0, None, ALU.add, None, [sgn.b], [sgn.b])
                            for h in range(8):
                                P.ts("pool", iqs[:, h * 64:(h + 1) * 64], iqt[:, h * 64:(h + 1) * 64], aw[:, h:h + 1], None, ALU.mult, None,
                                     [iqt.b, aw.b], [iqs.b])
                            psi = psbank(); pi = bfv(psi)
                            for c4 in range(4):
                                P.tr(pi[:, c4 * 128:(c4 + 1) * 128], iqs[:, c4 * 128:(c4 + 1) * 128], ident, [iqs.b, cstb.b], [psi.b])
                            P.copy("act", iqT[:], pi[:, 0:512], [psi.b], [iqT.b])
                            P.dma(iqT_d[:, :, ob * 128:(ob + 1) * 128].rearrange("h p t -> p h t"), iqT[:].rearrange("p (h t) -> p h t", h=4), [iqT.b], [IQD], semb=IQD)
                            P.dma(sgn_d[ob * 128:(ob + 1) * 128, :], sgn[:], [sgn.b], [SGD], semb=SGD)
                def secE(sbi):
                    pst = [psbank(), psbank()]
                    for blk in range(4):
                        for w3 in range(3):
                            idx = blk * 3 + w3
                            pv = bfv(pst[idx // 8])
                            src = kvt[:, blk, w3 * 128:(w3 + 1) * 128] if w3 < 2 else ikt[:, blk, :]
                            P.tr(pv[:, (idx % 8) * 128:(idx % 8 + 1) * 128], src, ident, [kvt.b, ikt.b, cstb.b], [pst[idx // 8].b])
                    P.copy("act", trs[:, 0:1024], bfv(pst[0])[:, :], [pst[0].b], [trs.b])
                    P.copy("dve", trs[:, 1024:1536], bfv(pst[1])[:, 0:512], [pst[1].b], [trs.b])
                    trv = trs[:].rearrange("p (b w t) -> p b w t", w=3, t=128)
                    t0 = sbi * 512
                    for kv in range(2):
                        P.dma(KT_d[kv, :, t0:t0 + 512].rearrange("p (b t) -> p b t", b=4), trv[:, :, kv, :], [trs.b], [KTD], semb=KTD)
                    P.dma(kiT_d[:, t0:t0 + 512].rearrange("p (b t) -> p b t", b=4), trv[:, :, 2, :], [trs.b], [KID], semb=KID)
                    P.dma(V_d[t0:t0 + 512, :].rearrange("(b p) c -> p b c", p=128), kvt[:, :, 256:512], [kvt.b], [VD], semb=VD)
                secA(0)
                for sbi in range(NSB):
                    secB(sbi)
                    if sbi + 1 < NSB:
                        secA(sbi + 1)
                    secD(sbi)
                    secE(sbi)
                dump("Sst", Sst, [128, 1024])

            stop("stop1")

            psmod[0] = 3
            NIT = 22
            P.set_barrier()
            with contextlib.ExitStack() as es2:
                def sb2(name, shape, dt=F32):
                    t = T.__new__(T)
                    t.t = es2.enter_context(nc.sbuf_tensor(name, shape, dt))
                    t.b = P.mkbuf(name)
                    return t
                KT = sb2("KT", [128, 2 * S], BF16)
                kiT = sb2("kiT", [128, S], BF16)
                Vaug = sb2("Vaug", [128, NB * 2 * 132], BF16)
                scoreL = [sb2("score%d" % k, [128, S]) for k in range(2)]
                nbiasL = [sb2("nbias%d" % k, [128, S], BF16) for k in range(2)]
                rbuf = [sb2("rbuf%d" % i, [128, 512], BF16) for i in range(2)]
                pbuf = [sb2("pbuf%d" % i, [128, 512], BF16) for i in range(2)]
                aqTiL = [sb2("aqTi%d" % k, [128, 1024], BF16) for k in range(2)]
                iqTi = sb2("iqTi", [128, 512], BF16)
                sgni = sb2("sgni", [128, 8])
                Dh = sb2("Dh", [128, 1024], BF16)
                CBf = sb2("CBf", [128, 512], BF16)
                PBt = sb2("PBt", [128, 512], BF16)
                BIGI4 = sb2("BIGI4", [128, 512], BF16)
                GA = sb2("GA", [128, 1024])
                nv = sb2("nv", [128, NO])
                smL = [sb2("sm%d" % k, [128, 16]) for k in range(2)]
                cjL = [sb2("cj%d" % k, [128, 8], BF16) for k in range(2)]
                sm2 = sb2("sm2", [128, 8]); sm3 = sb2("sm3", [128, 8]); sm4 = sb2("sm4", [128, 8])
                junk2 = sb2("junk2", [128, 128], BF16)
                attb = sb2("attb", [128, 1024], BF16)
                attT = sb2("attT", [128, 1024], BF16)
                P.dma(KT[:, 0:S], KT_d[0], [KTD], [KT.b])
                P.dma(KT[:, S:2 * S], KT_d[1], [KTD], [KT.b])
                P.dma(kiT[:], kiT_d[:, :], [KID], [kiT.b])
                P.emit("pool", lambda: nc.gpsimd.memset(Vaug[:], 1.0), [], [Vaug.b])
                Vv = Vaug[:].rearrange("p (b k c) -> p b k c", k=2, c=132)
                V_dv = V_d.rearrange("(b p) c -> p b c", p=128)
                for b0 in range(0, NB, 16):
                    b1 = min(NB, b0 + 16)
                    for kv in range(2):
                        P.dma(Vv[:, b0:b1, kv, 0:128], V_dv[:, b0:b1, kv * 128:(kv + 1) * 128], [VD], [Vaug.b])
                P.emit("pool", lambda: nc.gpsimd.memset(CBf[:], 0.0), [], [CBf.b])
                P.copy("pool", CBf[:, 384:512], CB, [cstf.b], [CBf.b])
                P.dma(scoreL[0][:, 0:512], padb[0:1, :].partition_broadcast(128), [], [scoreL[0].b])
                P.copy("pool", PBt[:], scoreL[0][:, 0:512], [scoreL[0].b], [PBt.b])
                for r4 in range(4):
                    P.ts("dve", BIGI4[:, r4 * 128:(r4 + 1) * 128], ident_f, 29952.0, None, ALU.mult, None, [cstf.b], [BIGI4.b])
                P.dma(GA[:], g_att[0:1, :].partition_broadcast(128), [], [GA.b])
                P.dma(nv[:], nvalT[:, :], [], [nv.b])
                acc = [PS[3], PS[4], PS[5]]
                def stageA(i):
                    score = scoreL[i % 2]; nbias = nbiasL[i % 2]; cj = cjL[i % 2]; aqTi = aqTiL[i % 2]; sm = smL[i % 2]
                    LO, HI, TH, CNT, GE, DD, NGE, SEL, AA, BB, THF = [sm[:, k:k + 1] for k in range(11)]
                    SMB = [sm.b]
                    nkt = i + 1
                    n = nkt * 512
                    nk = 4 * (i + 1)
                    P.dma(aqTi[:].rearrange("p (h t) -> p h t", h=8), aqT_d[:, :, i * 128:(i + 1) * 128].rearrange("h p t -> p h t"), [AQD], [aqTi.b])
                    P.dma(iqTi[:].rearrange("p (h t) -> p h t", h=4), iqT_d[:, :, i * 128:(i + 1) * 128].rearrange("h p t -> p h t"), [IQD], [iqTi.b])
                    P.dma(sgni[:], sgn_d[i * 128:(i + 1) * 128, :], [SGD], [sgni.b])
                    for h in range(8):
                        P.ts("dve", Dh[:, h * 128:(h + 1) * 128], ident_f, sgni[:, h:h + 1], None, ALU.mult, None, [cstf.b, sgni.b], [Dh.b])
                    for kt in range(nkt):
                        psc = PS[6 + kt % 2]
                        for h in range(8):
                            ps = psbank()
                            pb = (h % 2) * 64
                            P.mm(ps[:, :], iqTi[pb:pb + 64, (h // 2) * 128:(h // 2 + 1) * 128], kiT[pb:pb + 64, kt * 512:(kt + 1) * 512],
                                 True, True, [iqTi.b, kiT.b], [ps.b])
                            rb = rbuf[h % 2]
                            P.act(rb[:], ps[:, :], AF.Relu, [ps.b], [rb.b])
                            P.mm(psc[:, :], Dh[:, h * 128:(h + 1) * 128], rb[:], h == 0, h == 7, [Dh.b, rb.b], [psc.b])
                        dst = score[:, kt * 512:(kt + 1) * 512]
                        if kt == nkt - 1:
                            P.tt("dve", dst, psc[:, :], CBf[:], ALU.add, [psc.b, CBf.b], [score.b])
                            if kt == 0:
                                P.tt("dve", dst, dst, PBt[:], ALU.add, [score.b, PBt.b], [score.b])
                        elif kt == 0:
                            P.tt("dve", dst, psc[:, :], PBt[:], ALU.add, [psc.b, PBt.b], [score.b])
                        else:
                            P.copy("act", dst, psc[:, :], [psc.b], [score.b])
                    sc = score[:, 0:n]
                    P.emit("dve", lambda sc=sc: nc.vector.reduce_max(out=HI, in_=sc, axis=AX.X), [score.b], SMB)
                    P.ts("dve", LO, HI, -64.0, None, ALU.add, None, SMB, SMB)
                    for it in range(NIT):
                        cw = 64.0 / (2.0 ** (it + 1))
                        P.ts("dve", TH, LO, cw, None, ALU.add, None, SMB, SMB)
                        P.ts("dve", cj[:, 0:1].to_broadcast([128, n]), sc, TH, 0.0, ALU.is_ge, ALU.add, [score.b] + SMB, [cj.b] + SMB, accum_out=CNT)
                        P.ts("dve", GE, CNT, float(KSEL), cw, ALU.is_ge, ALU.mult, SMB, SMB)
                        P.tt("dve", LO, LO, GE, ALU.add, SMB, SMB)
                    P.ts("dve", SEL, nv[:, i:i + 1], float(KSEL), None, ALU.is_gt, None, [nv.b], SMB)
                    P.ts("dve", AA, SEL, 1e20, -1e20, ALU.mult, ALU.add, SMB, SMB)
                    P.tt("dve", BB, LO, SEL, ALU.mult, SMB, SMB)
                    P.tt("dve", THF, AA, BB, ALU.add, SMB, SMB)
                    P.ts("dve", nbias[:, 0:n], sc, THF, 1.0, ALU.is_ge, ALU.subtract, [score.b] + SMB, [nbias.b])
                def stageB(i):
                    nbias = nbiasL[i % 2]; aqTi = aqTiL[i % 2]
                    nk = 4 * (i + 1)
                    its = [(kb, kvh) for kb in range(nk) for kvh in range(2)]

                    def Lmm(k):
                        kb, kvh = its[k]
                        psl = psbank()
                        P.mm(psl[:, :], KT[:, kvh * S + kb * 128:kvh * S + (kb + 1) * 128], aqTi[:, kvh * 512:(kvh + 1) * 512],
                             True, False, [KT.b, aqTi.b], [psl.b])
                        P.mm(psl[:, :], nbias[:, kb * 128:(kb + 1) * 128], BIGI4[:], False, True, [nbias.b, BIGI4.b], [psl.b])
                        return psl
                    psl_next = Lmm(0)
                    for k, (kb, kvh) in enumerate(its):
                        psl = psl_next
                        if k + 1 < len(its):
                            psl_next = Lmm(k + 1)
                        pb_ = pbuf[k % 2]
                        P.act(pb_[:], psl[:, :], AF.Exp, [psl.b], [pb_.b], scale=128.0 ** -0.5)
                        vo = (kb * 2 + kvh) * 132
                        for g in range(4):
                            hd = kvh * 4 + g
                            a = acc[hd // 3]
                            off = (hd % 3) * 132
                            P.mm(a[:, off:off + 129], pb_[:, g * 128:(g + 1) * 128], Vaug[:, vo:vo + 129], (kb == 0 and hd % 3 == 0), False,
                                 [pb_.b, Vaug.b], [a.b], inc=(g == 3), skip_group_check=True)
                    for hd in range(8):
                        a = acc[hd // 3]
                        off = (hd % 3) * 132
                        P.emit("dve", lambda a=a, off=off, hd=hd: nc.vector.reciprocal(out=sm2[:, hd:hd + 1], in_=a[:, off + 128:off + 129]), [a.b], [sm2.b])
                        P.act(junk2[:], a[:, off:off + 128], AF.Square, [a.b, sm2.b], [junk2.b, sm3.b], scale=sm2[:, hd:hd + 1],
                              accum_out=sm3[:, hd:hd + 1])
                    rms_rstd(sm3[:], sm4[:], 8, 1.0 / 128, [sm3.b], [sm4.b])
                    P.tt("dve", sm4[:], sm4[:], sm2[:], ALU.mult, [sm4.b, sm2.b], [sm4.b])
                    for hd in range(8):
                        a = acc[hd // 3]
                        off = (hd % 3) * 132
                        P.stt(attb[:, hd * 128:(hd + 1) * 128], a[:, off:off + 128], sm4[:, hd:hd + 1], GA[:, hd * 128:(hd + 1) * 128],
                              ALU.mult, ALU.mult, [a.b, sm4.b, GA.b], [attb.b])
                    if i == 0:
                        dump("att0", attb, [128, 1024], BF16)
                    if i == 1:
                        dump("att1", attb, [128, 1024], BF16)
                    psr = psbank(); pr = bfv(psr)
                    for hd in range(8):
                        P.tr(pr[:, hd * 128:(hd + 1) * 128], attb[:, hd * 128:(hd + 1) * 128], ident, [attb.b, cstb.b], [psr.b])
                    P.copy("act", attT[:], pr[:, :], [psr.b], [attT.b])
                    P.dma(mixT_d[1024:2048, i * 128:(i + 1) * 128].rearrange("(h p) t -> p h t", p=128), attT[:].rearrange("p (h t) -> p h t", h=8),
                          [attT.b], [MIXD], semb=MIXD)
                stageA(0)
                for i in range(NO):
                    if i + 1 < NO:
                        stageA(i + 1)
                    stageB(i)
            psmod[0] = 6
            stop("stop2")

            h2T_d = dscr("h2T_d", [D, TO], BF16)
            H2D = Buf("h2T_d"); X1D = Buf("x1_d"); OUTD = Buf("out")
            P.set_barrier()
            with contextlib.ExitStack() as es34:
                def sb34(name, shape, dt=F32, st=es34):
                    t = T.__new__(T)
                    t.t = st.enter_context(nc.sbuf_tensor(name, shape, dt))
                    t.b = P.mkbuf(name)
                    return t
                comb = sb34("comb", [128, NO * 32])
                with contextlib.ExitStack() as es3:
                    sb3 = lambda name, shape, dt=F32: sb34(name, shape, dt, es3)
                    Wo = sb3("Wo", [128, KC * 2048], BF16)
                    wst3 = [sb3("wst3_%d" % i, [128, 2048]) for i in range(2)]
                    for kc in range(KC):
                        w = wst3[kc % 2]
                        P.dma(w[:], w_out[kc * 128:(kc + 1) * 128, :], [], [w.b])
                        P.copy("act" if kc % 2 == 0 else "pool", Wo[:, kc * 2048:(kc + 1) * 2048], w[:], [w.b], [Wo.b])
                    GT1b = sb3("GT1b", [128, 2048])
                    mod6 = mod_d.rearrange("(a n) -> a n", a=6)
                    P.dma(GT1b[:], mod6[2:3, :].partition_broadcast(128), [modD], [GT1b.b])
                    Wrt = sb3("Wrt", [128, KC * 36]); Wrtb = sb3("Wrtb", [128, KC * 36], BF16)
                    P.dma(Wrt[:].rearrange("p (k c) -> p k c", c=36), w_rt.rearrange("(k p) c -> p k c", p=128), [], [Wrt.b])
                    P.copy("dve", Wrtb[:], Wrt[:], [Wrt.b], [Wrtb.b])
                    brt = sb3("brt", [128, 36])
                    P.dma(brt[:], b_rt[0:1, :].partition_broadcast(128), [], [brt.b])
                    xo = [sb3("xo%d" % i, [128, D]) for i in range(2)]
                    x1t = [sb3("x1t%d" % i, [128, D]) for i in range(2)]
                    xn2 = sb3("xn2", [128, D], BF16)
                    mixTi = sb3("mixTi", [128, KC * 128], BF16)
                    h2Tb = sb3("h2Tb", [128, KC * 128], BF16)
                    st3 = sb3("st3", [128, 8])
                    lg = sb3("lg", [128, 36])
                    rs = sb3("rs", [128, 64])
                    for i in range(NO):
                        x_ = xo[i % 2]; x1 = x1t[i % 2]
                        p = 4 * i + 3
                        P.dma(x_[:], x_sh[p * 128:(p + 1) * 128, :], [], [x_.b])
                        P.dma(mixTi[:].rearrange("p (k t) -> p k t", t=128), mixT_d[:, i * 128:(i + 1) * 128].rearrange("(k p) t -> p k t", p=128),
                              [MIXD], [mixTi.b])
                        for dt in range(4):
                            ps = psbank()
                            for kc in range(KC):
                                P.mm(ps[:, :], mixTi[:, kc * 128:(kc + 1) * 128], Wo[:, kc * 2048 + dt * 512:kc * 2048 + (dt + 1) * 512],
                                     kc == 0, kc == KC - 1, [mixTi.b, Wo.b], [ps.b])
                            P.tt("dve", x1[:, dt * 512:(dt + 1) * 512], ps[:, :], GT1b[:, dt * 512:(dt + 1) * 512], ALU.mult, [ps.b, GT1b.b], [x1.b])
                        P.tt("dve", x1[:], x1[:], x_[:], ALU.add, [x1.b, x_.b], [x1.b])
                        if i == 0:
                            dump("x1", x1, [128, D])
                        P.dma(x1_d[i * 128:(i + 1) * 128, :], x1[:], [x1.b], [X1D], semb=X1D)
                        P.act(xn2[:], x1[:], AF.Square, [x1.b], [xn2.b, st3.b], accum_out=st3[:, 0:1])
                        rms_rstd(st3[:, 0:1], st3[:, 1:2], 1, 1.0 / D, [st3.b], [st3.b])
                        P.ts("dve", xn2[:], x1[:], st3[:, 1:2], None, ALU.mult, None, [x1.b, st3.b], [xn2.b])
                        for half in range(2):
                            ps = psbank()
                            pv = bfv(ps)
                            for k8 in range(8):
                                kc = half * 8 + k8
                                P.tr(pv[:, k8 * 128:(k8 + 1) * 128], xn2[:, kc * 128:(kc + 1) * 128], ident, [xn2.b, cstb.b], [ps.b])
                            for k8 in range(8):
                                kc = half * 8 + k8
                                o = h2Tb[:, kc * 128:(kc + 1) * 128]
                                i_ = pv[:, k8 * 128:(k8 + 1) * 128]
                                if k8 % 2 == 0:
                                    P.act(o, i_, AF.Identity, [ps.b, G2s.b, modT.b], [h2Tb.b], scale=G2s[:, kc:kc + 1], bias=SH2s[:, kc:kc + 1])
                                else:
                                    P.ts("dve", o, i_, G2s[:, kc:kc + 1], SH2s[:, kc:kc + 1], ALU.mult, ALU.add, [ps.b, G2s.b, modT.b], [h2Tb.b])
                        P.dma(h2T_d[:, i * 128:(i + 1) * 128].rearrange("(k p) t -> p k t", p=128), h2Tb[:].rearrange("p (k t) -> p k t", t=128),
                              [h2Tb.b], [H2D], semb=H2D)
                        psr = psbank()
                        for kc in range(KC):
                            P.mm(psr[:, 0:36], h2Tb[:, kc * 128:(kc + 1) * 128], Wrtb[:, kc * 36:(kc + 1) * 36], kc == 0, kc == KC - 1,
                                 [h2Tb.b, Wrtb.b], [psr.b])
                        P.tt("dve", lg[:], psr[:, 0:36], brt[:], ALU.add, [psr.b, brt.b], [lg.b])
                        RB = [rs.b]
                        GMAX, NGM, SE, PG, M1, M2, DLT, EX, DEN, W1, W2 = [rs[:, k:k + 1] for k in range(11)]
                        OHG = rs[:, 12:16]; ESEL = rs[:, 16:24]; MK1 = rs[:, 24:32]; E2 = rs[:, 32:40]; MK2 = rs[:, 40:48]; CIG = rs[:, 48:56]; EG = rs[:, 56:60]
                        P.emit("dve", lambda: nc.vector.reduce_max(out=GMAX, in_=lg[:, 0:4], axis=AX.X), [lg.b], RB)
                        P.ts("dve", NGM, GMAX, -1.0, None, ALU.mult, None, RB, RB)
                        P.act(EG, lg[:, 0:4], AF.Exp, [lg.b] + RB, RB, bias=NGM, accum_out=SE)
                        P.emit("dve", lambda: nc.vector.reciprocal(out=PG, in_=SE), RB, RB)
                        P.ts("dve", OHG, lg[:, 0:4], GMAX, None, ALU.is_ge, None, [lg.b] + RB, RB)
                        P.ts("dve", ESEL, lg[:, 4:12], rs[:, 12:13], None, ALU.mult, None, [lg.b] + RB, RB)
                        for g in range(1, 4):
                            P.stt(ESEL, lg[:, 4 + 8 * g:12 + 8 * g], rs[:, 12 + g:13 + g], ESEL, ALU.mult, ALU.add, [lg.b] + RB, RB)
                        P.emit("dve", lambda: nc.vector.reduce_max(out=M1, in_=ESEL, axis=AX.X), RB, RB)
                        P.ts("dve", MK1, ESEL, M1, None, ALU.is_ge, None, RB, RB)
                        P.stt(E2, MK1, -1e30, ESEL, ALU.mult, ALU.add, RB, RB)
                        P.emit("dve", lambda: nc.vector.reduce_max(out=M2, in_=E2, axis=AX.X), RB, RB)
                        P.ts("dve", MK2, E2, M2, None, ALU.is_ge, None, RB, RB)
                        P.tt("dve", DLT, M2, M1, ALU.subtract, RB, RB)
                        P.act(EX, DLT, AF.Exp, RB, RB)
                        P.ts("dve", DEN, EX, 1.0, None, ALU.add, None, RB, RB)
                        P.emit("dve", lambda: nc.vector.reciprocal(out=DEN, in_=DEN), RB, RB)
                        P.tt("dve", W1, DEN, PG, ALU.mult, RB, RB)
                        P.tt("dve", W2, W1, EX, ALU.mult, RB, RB)
                        P.ts("dve", CIG, MK1, W1, None, ALU.mult, None, RB, RB)
                        P.stt(CIG, MK2, W2, CIG, ALU.mult, ALU.add, RB, RB)
                        for g in range(4):
                            P.ts("dve", comb[:, i * 32 + g * 8:i * 32 + (g + 1) * 8], CIG, rs[:, 12 + g:13 + g], None, ALU.mult, None, RB, [comb.b])
                stop("stop3")
                P.set_barrier()
                with contextlib.ExitStack() as es4:
                    sb4 = lambda name, shape, dt=F32: sb34(name, shape, dt, es4)
                    CH = min(TO, 1024)
                    NCH = TO // CH
                    NT = CH // 128
                    SUB = min(512, CH)
                    h2c = sb4("h2c", [128, KC * CH], BF16)
                    yacc = sb4("yacc", [128, NT * 2048])
                    WGb = sb4("WGb", [128, KC * 512], BF16); WUb = sb4("WUb", [128, KC * 512], BF16); WDb = sb4("WDb", [128, 4 * 2048], BF16)
                    stg = [sb4("stg%d" % k, [128, 2048]) for k in range(4)]
                    sgt = [sb4("sgt%d" % k, [128, 512]) for k in range(2)]
                    heT = [sb4("heT%d" % k, [128, 4 * 512], BF16) for k in range(2)]
                    xf = sb4("xf", [128, 2048])
                    st4 = sb4("st4", [128, 8])
                    srot = [0]

                    def load_cast(dst_ap, dstb, src_ap, three=None):
                        w = stg[srot[0] % 4]
                        eng = "act" if srot[0] % 4 != 3 else "dve"
                        srot[0] += 1
                        if three is None:
                            P.dma(w[:], src_ap, [], [w.b])
                        else:
                            P.dma(w[:].rearrange("p (k c) -> p k c", c=512), src_ap, [], [w.b])
                        P.copy(eng, dst_ap, w[:], [w.b], [dstb])

                    for ch in range(NCH):
                        P.dma(h2c[:].rearrange("p (k t) -> p k t", t=CH), h2T_d[:, ch * CH:(ch + 1) * CH].rearrange("(k p) t -> p k t", p=128),
                              [H2D], [h2c.b])
                        P.emit("pool", lambda: nc.gpsimd.memset(yacc[:], 0.0), [], [yacc.b])
                        for e in range(NE):
                            for q in range(4):
                                load_cast(WGb[:, q * 2048:(q + 1) * 2048], WGb.b, w_eg[e % ne_decl, q * 512:(q + 1) * 512, :].rearrange("(k p) c -> p k c", p=128), 1)
                            for q in range(4):
                                load_cast(WUb[:, q * 2048:(q + 1) * 2048], WUb.b, w_eu[e % ne_decl, q * 512:(q + 1) * 512, :].rearrange("(k p) c -> p k c", p=128), 1)
                            for c in range(4):
                                load_cast(WDb[:, c * 2048:(c + 1) * 2048], WDb.b, w_ed[e % ne_decl, c * 128:(c + 1) * 128, :])
                            for st_ in range(CH // SUB):
                                tok0 = st_ * SUB
                                he = heT[st_ % 2]
                                for c in range(4):
                                    psg = psbank(); psu = psbank()
                                    for kc in range(KC):
                                        P.mm(psg[:, 0:SUB], WGb[:, kc * 512 + c * 128:kc * 512 + (c + 1) * 128], h2c[:, kc * CH + tok0:kc * CH + tok0 + SUB],
                                             kc == 0, kc == KC - 1, [WGb.b, h2c.b], [psg.b])
                                    for kc in range(KC):
                                        P.mm(psu[:, 0:SUB], WUb[:, kc * 512 + c * 128:kc * 512 + (c + 1) * 128], h2c[:, kc * CH + tok0:kc * CH + tok0 + SUB],
                                             kc == 0, kc == KC - 1, [WUb.b, h2c.b], [psu.b])
                                    sg = sgt[c % 2]
                                    P.act(sg[:, 0:SUB], psg[:, 0:SUB], AF.Silu, [psg.b], [sg.b])
                                    P.tt("dve", he[:, c * 512:c * 512 + SUB], sg[:, 0:SUB], psu[:, 0:SUB], ALU.mult, [sg.b, psu.b], [he.b])
                                for t_ in range(SUB // 128):
                                    tile = st_ * (SUB // 128) + t_
                                    gt = ch * NT + tile
                                    for dt in range(4):
                                        psd = psbank()
                                        for c in range(4):
                                            P.mm(psd[:, :], he[:, c * 512 + t_ * 128:c * 512 + (t_ + 1) * 128], WDb[:, c * 2048 + dt * 512:c * 2048 + (dt + 1) * 512],
                                                 c == 0, c == 3, [he.b, WDb.b], [psd.b])
                                        ya = yacc[:, tile * 2048 + dt * 512:tile * 2048 + (dt + 1) * 512]
                                        P.stt(ya, psd[:, :], comb[:, gt * 32 + e:gt * 32 + e + 1], ya, ALU.mult, ALU.add, [psd.b, comb.b, yacc.b], [yacc.b])
                        GT2b = stg[0]; GFb = stg[1]
                        P.dma(GT2b[:], mod6[5:6, :].partition_broadcast(128), [modD], [GT2b.b])
                        P.dma(GFb[:], g_fin[0:1, :].partition_broadcast(128), [], [GFb.b])
                        for tile in range(NT):
                            gt = ch * NT + tile
                            P.dma(xf[:], x1_d[gt * 128:(gt + 1) * 128, :], [X1D], [xf.b])
                            ya = yacc[:, tile * 2048:(tile + 1) * 2048]
                            P.tt("dve", ya, ya, GT2b[:], ALU.mult, [yacc.b, GT2b.b], [yacc.b])
                            P.tt("dve", xf[:], xf[:], ya, ALU.add, [xf.b, yacc.b], [xf.b])
                            P.act(ya, xf[:], AF.Square, [xf.b], [yacc.b, st4.b], accum_out=st4[:, 0:1])
                            rms_rstd(st4[:, 0:1], st4[:, 1:2], 1, 1.0 / D, [st4.b], [st4.b])
                            P.stt(xf[:], xf[:], st4[:, 1:2], GFb[:], ALU.mult, ALU.mult, [xf.b, st4.b, GFb.b], [xf.b])
                            P.dma(out_d[gt * 128:(gt + 1) * 128, :], xf[:], [xf.b], [OUTD], semb=OUTD)


        try:
            body()
        except _Stop:
            pass
        for b_ in P.dmabufs:
            P.final.append((b_.sem, b_.cnt))
        P.replay()
    return nc, dbg_out


def host_consts():
    c = np.zeros((128, 1024), np.float32)
    i = np.arange(128)
    c[:, 0:128] = np.eye(128)
    same = (i[:, None] // 64) == (i[None, :] // 64)
    c[:, 128:256] = (same & (i[:, None] <= i[None, :])).astype(np.float32)
    c[:, 256:384] = (same & (i[:, None] > i[None, :])).astype(np.float32)
    c[:, 384] = (i < 64)
    c[:, 385] = (i >= 64)
    c[:, 512:640] = np.where(i[None, :] <= i[:, None], 0.0, -1e30)
    c[:, 640:768] = np.eye(128)
    return c


def make_in_maps(inputs, S, ne=NE):
    f = lambda a: np.ascontiguousarray(np.asarray(a, dtype=np.float32))
    x = f(inputs["x"]); c = f(inputs["c"])
    NB = S // 128; NO = NB // 4
    pT = lambda v: np.ascontiguousarray(v.reshape(-1, 128).T)
    shared = {
        "w_ada": f(inputs["w_ada"][0]), "badaT": pT(f(inputs["b_ada"][0])),
        "gnmT": pT(f(inputs["g_norm_mix"][0])), "gnfT": pT(f(inputs["g_norm_ffn"][0])),
        "w_in": f(inputs["w_in"][0]), "lbl": f(inputs["lb_logits"]),
        "g_rec": f(inputs["g_rec_out"]), "g_att": f(inputs["g_att_out"]),
        "w_out": f(inputs["w_out"][0]),
        "w_rt": np.ascontiguousarray(np.concatenate([f(inputs["w_router_group"][0]), f(inputs["w_router_expert"][0])], axis=1)),
        "b_rt": np.ascontiguousarray(np.concatenate([f(inputs["b_router_group"][0]), f(inputs["b_router_expert"][0])])[None, :]),
        "w_eg": f(inputs["w_expert_gate"][0][:ne]), "w_eu": f(inputs["w_expert_up"][0][:ne]), "w_ed": f(inputs["w_expert_down"][0][:ne]),
        "g_fin": f(inputs["g_final"])[None, :], "cst": host_consts(),
    }
    maps = []
    for core in range(8):
        b, j = core // 4, core % 4
        npad = (3 - j) * 128
        xs = np.zeros((S, D), np.float32)
        xs[npad:] = x[b, :S - npad]
        valid = np.ones(S, np.float32); valid[:npad] = 0
        padb = np.zeros((1, 512), np.float32); padb[0, :npad] = -1e30
        nval = np.zeros((128, NO), np.float32)
        for i in range(NO):
            nval[:, i] = (4 * i + 3) * 128 + np.arange(128) - npad + 1
        m = dict(shared)
        m.update({"x_sh": xs, "cT": pT(c[b]), "validT": pT(valid), "padb": padb, "nvalT": nval})
        maps.append(m)
    return maps


def assemble(results, S, B=2):
    NB = S // 128; NO = NB // 4
    out = np.zeros((B, S, D), np.float32)
    for core in range(8):
        b, j = core // 4, core % 4
        o = np.asarray(results[core]["out"]).reshape(NO, 128, D)
        for i in range(NO):
            g = 4 * i + j
            out[b, g * 128:(g + 1) * 128] = o[i]
    return out


_CACHE = {}


def kernel(**inputs):
    S = int(np.asarray(inputs["x"]).shape[1])
    if S not in _CACHE:
        _CACHE[S] = build(S)[0]
    nc = _CACHE[S]
    maps = make_in_maps(inputs, S)
    res = run_bass_kernel_spmd(nc, maps, core_ids=list(range(8)))
    return assemble(res.results, S)
```

```python
import contextlib
import numpy as np
import ml_dtypes
import concourse.bass as bass
import concourse.mybir as mybir
from concourse.bass_utils import run_bass_kernel_spmd

F32 = mybir.dt.float32
BF16 = mybir.dt.bfloat16
AF = mybir.ActivationFunctionType
ALU = mybir.AluOpType
AX = mybir.AxisListType

D = 2048
KC = 16
IN_COLS = 6216
NE = 32
DE = 512
EPS = 1e-6
NEG = -30000.0
SAME_ENGINE_SYNC = True
C_RQ, C_RF, C_RI, C_RG, C_AQ, C_AK, C_AV, C_IQ, C_IK, C_IW = 0, 1024, 2048, 3072, 4096, 5120, 5376, 5632, 6144, 6208


class Buf:
    __slots__ = ("name", "w", "r", "sem", "cnt")

    def __init__(self, name):
        self.name = name
        self.w = None
        self.r = []
        self.sem = None
        self.cnt = 0


class Prog:
    ENGS = ("pe", "act", "dve", "pool", "sp")

    def __init__(self, nc, es):
        self.nc = nc
        self.es = es
        self.q = {e: [] for e in self.ENGS}
        self.cnt = {e: 0 for e in self.ENGS}
        self.esem = {e: es.enter_context(nc.semaphore("sem_" + e)) for e in ("pe", "act", "dve", "pool")}
        self.seen = {e: {} for e in self.ENGS}
        self.nsem = 0
        self.final = []
        self.dmabufs = []
        self.stopped = False
        self.barrier = []

    def set_barrier(self):
        toks = [(e, self.esem[e], self.cnt[e]) for e in ("pe", "act", "dve", "pool") if self.cnt[e] > 0]
        for b in self.dmabufs:
            toks.append(("d%d" % id(b), b.sem, b.cnt))
        self.barrier = toks

    def mkbuf(self, name):
        b = Buf(name)
        b.r = list(self.barrier)
        return b

    def newsem(self, name):
        self.nsem += 1
        return self.es.enter_context(self.nc.semaphore("d_%s_%d" % (name, self.nsem)))

    def _waits(self, eng, R, W):
        toks = []
        for b in R:
            if b.w is not None:
                toks.append(b.w)
        for b in W:
            if b.w is not None:
                toks.append(b.w)
            toks.extend(b.r)
        out = {}
        for (key, sem, v) in toks:
            if key == eng and (eng == "pe" or not SAME_ENGINE_SYNC):
                continue
            if self.seen[eng].get(key, 0) >= v:
                continue
            if out.get(key, (None, 0))[1] < v:
                out[key] = (sem, v)
        for key, (sem, v) in out.items():
            self.seen[eng][key] = v
        return list(out.values())

    def emit(self, eng, fn, R=(), W=(), inc=True):
        if self.stopped:
            return
        waits = self._waits(eng, R, W)
        sem = self.esem[eng]
        if inc:
            self.cnt[eng] += 1
            c = self.cnt[eng]
            self.q[eng].append((waits, fn, sem, 1))
        else:
            c = self.cnt[eng] + 1
            self.q[eng].append((waits, fn, sem, 0))
        tok = (eng, sem, c)
        for b in W:
            b.w = tok
            b.r = []
        for b in R:
            if b not in W:
                b.r.append(tok)

    def dma(self, out, in_, R, W, semb=None, q="sp"):
        semb = semb or W[0]
        if self.stopped:
            return (None, None, 0)
        if semb.sem is None:
            semb.sem = self.newsem(semb.name)
            self.dmabufs.append(semb)
        waits = self._waits(q, R, W)
        semb.cnt += 16
        nc = self.nc
        eng = {"sp": nc.sync, "act": nc.scalar, "pool": nc.gpsimd}[q]
        self.q[q].append((waits, lambda: eng.dma_start(out=out, in_=in_), semb.sem, 16))
        tok = ("d%d" % id(semb), semb.sem, semb.cnt)
        for b in W:
            b.w = tok
            b.r = []
        for b in R:
            b.r.append(tok)
        return tok

    def act(self, out, in_, func, R, W, **kw):
        nc = self.nc
        self.emit("act", lambda: nc.scalar.activation(out=out, in_=in_, func=func, **kw), R, W)

    def ts(self, eng, out, in0, s1, s2, op0, op1, R, W, **kw):
        e = self.nc.vector if eng == "dve" else self.nc.gpsimd
        if op1 is None:
            self.emit(eng, lambda: e.tensor_scalar(out=out, in0=in0, scalar1=s1, scalar2=None, op0=op0, **kw), R, W)
        else:
            self.emit(eng, lambda: e.tensor_scalar(out=out, in0=in0, scalar1=s1, scalar2=s2, op0=op0, op1=op1, **kw), R, W)

    def tt(self, eng, out, in0, in1, op, R, W):
        e = self.nc.vector if eng == "dve" else self.nc.gpsimd
        self.emit(eng, lambda: e.tensor_tensor(out=out, in0=in0, in1=in1, op=op), R, W)

    def stt(self, out, in0, scalar, in1, op0, op1, R, W):
        nc = self.nc
        self.emit("dve", lambda: nc.vector.scalar_tensor_tensor(out=out, in0=in0, scalar=scalar, in1=in1, op0=op0, op1=op1), R, W)

    def copy(self, eng, out, in_, R, W):
        nc = self.nc
        if eng == "act":
            self.emit("act", lambda: nc.scalar.copy(out=out, in_=in_), R, W)
        elif eng == "dve":
            self.emit("dve", lambda: nc.vector.tensor_copy(out=out, in_=in_), R, W)
        else:
            self.emit("pool", lambda: nc.gpsimd.tensor_copy(out=out, in_=in_), R, W)

    def mm(self, out, lhsT, rhs, start, stop, R, W, inc=None, **kw):
        nc = self.nc
        if inc is None:
            inc = bool(stop)
        self.emit("pe", lambda: nc.tensor.matmul(out, lhsT, rhs, start=start, stop=stop, **kw), R, W, inc=inc)

    def tr(self, out, in_, ident, R, W, inc=True):
        nc = self.nc
        self.emit("pe", lambda: nc.tensor.transpose(out, in_, ident), R, W, inc=inc)

    def replay(self):
        nc = self.nc
        engmap = {"pe": "tensor", "act": "scalar", "dve": "vector", "pool": "gpsimd", "sp": "sync"}
        with nc.Block() as blk:
            for e in self.ENGS:
                lst = self.q[e]
                final = self.final

                def body(eng, lst=lst, e=e):
                    for (waits, fn, sem, n) in lst:
                        for (s, v) in waits:
                            eng.wait_ge(s, v)
                        if n:
                            fn().then_inc(sem, n)
                        else:
                            fn()
                    if e == "sp":
                        for (s, v) in final:
                            eng.wait_ge(s, v)

                getattr(blk, engmap[e])(body)


class T:
    def __init__(self, P, kind, name, shape, dtype):
        nc = P.nc
        if kind == "sb":
            self.t = P.es.enter_context(nc.sbuf_tensor(name, shape, dtype))
        else:
            self.t = P.es.enter_context(nc.psum_tensor(name, shape, dtype))
        self.b = Buf(name)

    def __getitem__(self, k):
        return self.t[k]


def build(S, dbg=None):
    NB = S // 128
    NSB = NB // 4
    NO = NSB
    TO = NO * 128
    KSEL = min(256, S // 4)
    dbg = dbg or ()
    nc = bass.Bass("TRN2", target_bir_lowering=False)

    def din(name, shape, dt=F32):
        return nc.dram_tensor(name, list(shape), dt, kind="ExternalInput").ap()

    def dscr(name, shape, dt):
        return nc.dram_tensor(name, list(shape), dt, kind="Internal").ap()

    x_sh = din("x_sh", [S, D])
    cT = din("cT", [128, KC])
    w_ada = din("w_ada", [D, 6 * D])
    badaT = din("badaT", [128, 96])
    gnmT = din("gnmT", [128, KC])
    gnfT = din("gnfT", [128, KC])
    w_in = din("w_in", [D, IN_COLS])
    lbl = din("lbl", [2, 1024])
    g_rec = din("g_rec", [1, 1024])
    g_att = din("g_att", [1, 1024])
    gaT_in = din("gaT", [128, 8])
    w_out = din("w_out", [D, D])
    w_rt = din("w_rt", [D, 36])
    b_rt = din("b_rt", [1, 36])
    ne_decl = 1 if any(d.startswith("stop") for d in dbg) else NE
    w_eg = din("w_eg", [ne_decl, D, DE])
    w_eu = din("w_eu", [ne_decl, D, DE])
    w_ed = din("w_ed", [ne_decl, DE, D])
    g_fin = din("g_fin", [1, D])
    validT = din("validT", [128, NB])
    padb = din("padb", [1, 512])
    nvalT = din("nvalT", [128, NO])
    cst = din("cst", [128, 8 * 128])
    out_d = nc.dram_tensor("out", [TO, D], F32, kind="ExternalOutput").ap()

    win_bf = dscr("win_bf", [D, IN_COLS], BF16)
    mod_d = dscr("mod_d", [96 * 128], F32)
    KT_d = dscr("KT_d", [2, 128, S], BF16)
    V_d = dscr("V_d", [S, 256], BF16)
    kiT_d = dscr("kiT_d", [128, S], BF16)
    aqT_d = dscr("aqT_d", [8, 128, TO], BF16)
    iqT_d = dscr("iqT_d", [4, 128, TO], BF16)
    sgn_d = dscr("sgn_d", [TO, 8], F32)
    mixT_d = dscr("mixT_d", [D, TO], BF16)
    x1_d = dscr("x1_d", [TO, D], F32)

    dbg_out = {}

    class _Stop(Exception):
        pass

    es = contextlib.ExitStack()
    with es:
        P = Prog(nc, es)
        allbufs = []

        def stop(tag):
            if tag in dbg:
                P.stopped = True

        def body():

            def sb(name, shape, dt=F32):
                return T(P, "sb", name, shape, dt)

            PS = [T(P, "ps", "ps%d" % i, [128, 512], F32) for i in range(8)]
            psrot = [0]
            psmod = [6]

            def psbank():
                t = PS[psrot[0] % psmod[0]]
                psrot[0] += 1
                return t

            def bfv(ps):
                return ps.t[:, :].bitcast(BF16)

            def dump(name, tile, shape, dt=F32):
                if name not in dbg:
                    return
                o = nc.dram_tensor("dbg_" + name, list(shape), dt, kind="ExternalOutput").ap()
                b = Buf("dbg_" + name)
                tok = P.dma(o, tile.t[:] if isinstance(tile, T) else tile[0], [tile.b if isinstance(tile, T) else tile[1]], [b])
                P.final.append((tok[1], tok[2]))
                dbg_out[name] = (shape, dt)

            cstf = sb("cstf", [128, 8 * 128])
            P.dma(cstf[:], cst[:, :], [], [cstf.b])
            ident_f = cstf[:, 0:128]
            TL = cstf[:, 128:256]
            TU = cstf[:, 256:384]
            CI = cstf[:, 384:386]
            CB = cstf[:, 512:640]
            cstb = sb("cstb", [128, 8 * 128], BF16)
            P.copy("dve", cstb[:], cstf[:], [cstf.b], [cstb.b])
            ident = cstb[:, 0:128]
            MBD = cstf[:, 128:256]
            I4 = cstb[:, 640:640 + 128]

            cTt = sb("cTt", [128, KC])
            P.dma(cTt[:], cT[:, :], [], [cTt.b])
            cact = sb("cact", [128, KC])
            P.act(cact[:], cTt[:], AF.Silu, [cTt.b], [cact.b])
            modps = psbank()
            modT = sb("modT", [128, 96])
            bT = sb("bT", [128, 96])
            modTT = sb("modTT", [96, 128])
            g1t = sb("g1t", [128, KC]); g2t = sb("g2t", [128, KC])
            G1s = sb("G1s", [128, KC]); G2s = sb("G2s", [128, KC])
            with contextlib.ExitStack() as es0:
                wad = [T.__new__(T) for _ in range(2)]
                for i, w in enumerate(wad):
                    w.t = es0.enter_context(nc.sbuf_tensor("wad%d" % i, [128, KC, 512], F32))
                    w.b = Buf("wad%d" % i)
                w_ada_v = w_ada.rearrange("(kc p) c -> p kc c", p=128)
                modrow = T.__new__(T)
                modrow.t = es0.enter_context(nc.sbuf_tensor("modrow", [1, 6 * D], F32))
                modrow.b = Buf("modrow")
                for n in range(24):
                    w = wad[n % 2]
                    P.dma(w[:], w_ada_v[:, :, n * 512:(n + 1) * 512], [], [w.b])
                    psr_ = psbank()
                    for kc in range(KC):
                        P.mm(psr_[0:1, :], cact[:, kc:kc + 1], w[:, kc, :], kc == 0, kc == KC - 1, [w.b, cact.b], [psr_.b])
                    P.copy("act" if n % 2 == 0 else "dve", modrow[0:1, n * 512:(n + 1) * 512], psr_[0:1, :], [psr_.b], [modrow.b])
                for m in range(96):
                    P.mm(modps[:, m:m + 1], modrow[0:1, m * 128:(m + 1) * 128], ident_f[0:1, 0:1], True, True, [modrow.b, cstf.b], [modps.b],
                         inc=(m == 95))
                P.dma(bT[:], badaT[:, :], [], [bT.b])
                P.tt("dve", modT[:], modps[:, 0:96], bT[:], ALU.add, [modps.b, bT.b], [modT.b])
                dump("modT", modT, [128, 96])
                P.dma(g1t[:], gnmT[:, :], [], [g1t.b])
                P.dma(g2t[:], gnfT[:, :], [], [g2t.b])
                P.stt(G1s[:], modT[:, 16:32], 1.0, g1t[:], ALU.add, ALU.mult, [modT.b, g1t.b], [G1s.b])
                P.stt(G2s[:], modT[:, 64:80], 1.0, g2t[:], ALU.add, ALU.mult, [modT.b, g2t.b], [G2s.b])
                SH1s = modT[:, 0:16]
                SH2s = modT[:, 48:64]
                modD = Buf("mod_d")
                pmt = psbank()
                P.tr(pmt[0:96, 0:128], modT[:], ident_f, [modT.b, cstf.b], [pmt.b])
                P.copy("dve", modTT[:], pmt[0:96, 0:128], [pmt.b], [modTT.b])
                P.dma(mod_d.rearrange("(m p) -> m p", p=128), modTT[:], [modTT.b], [modD])

                winD = Buf("win_bf")
                wst = []
                wsb = []
                for i in range(2):
                    a = T.__new__(T); a.t = es0.enter_context(nc.sbuf_tensor("wst%d" % i, [128, IN_COLS], F32)); a.b = Buf("wst%d" % i)
                    c_ = T.__new__(T); c_.t = es0.enter_context(nc.sbuf_tensor("wsb%d" % i, [128, IN_COLS], BF16)); c_.b = Buf("wsb%d" % i)
                    wst.append(a); wsb.append(c_)
                for kc in range(KC):
                    a = wst[kc % 2]; c_ = wsb[kc % 2]
                    P.dma(a[:], w_in[kc * 128:(kc + 1) * 128, :], [], [a.b])
                    h = IN_COLS // 2
                    P.copy("act", c_[:, 0:h], a[:, 0:h], [a.b], [c_.b])
                    P.copy("dve", c_[:, h:], a[:, h:], [a.b], [c_.b])
                    P.dma(win_bf[kc * 128:(kc + 1) * 128, :], c_[:], [c_.b], [winD], semb=winD)

            stop("stop0")
            P.set_barrier()
            with contextlib.ExitStack() as es1:
                def sb1(name, shape, dt=F32):
                    t = T.__new__(T)
                    t.t = es1.enter_context(nc.sbuf_tensor(name, shape, dt))
                    t.b = P.mkbuf(name)
                    return t

                LB = sb1("LB", [128, 1024]); OMLB = sb1("OMLB", [128, 1024]); GS = sb1("GS", [128, 1024]); l1 = GS
                P.dma(LB[:], lbl[0:1, :].partition_broadcast(128), [], [LB.b])
                P.dma(l1[:], lbl[1:2, :].partition_broadcast(128), [], [l1.b])
                P.tt("dve", LB[:], LB[:], l1[:], ALU.subtract, [LB.b, l1.b], [LB.b])
                P.act(LB[:], LB[:], AF.Sigmoid, [LB.b], [LB.b])
                P.ts("dve", OMLB[:], LB[:], -1.0, 1.0, ALU.mult, ALU.add, [LB.b], [OMLB.b])
                GR = sb1("GR", [128, 1024])
                P.dma(GR[:], g_rec[0:1, :].partition_broadcast(128), [], [GR.b])
                vT = sb1("vT", [128, NB])
                P.dma(vT[:], validT[:, :], [], [vT.b])

                xt = [sb1("xt%d" % i, [128, D]) for i in range(2)]
                junk = sb1("junk", [128, 128], BF16)
                xn = [sb1("xn%d" % i, [128, D], BF16) for i in range(2)]
                st = sb1("st", [128, 8])
                hT = sb1("hT", [128, KC, 512], BF16)
                wt = [sb1("wt%d" % i, [128, KC, 512], BF16) for i in range(2)]
                wrot = [0]
                sgf = sb1("sgf", [128, 4, 1024])
                lgf = sb1("lgf", [128, 4, 1024])
                vbf = sb1("vbf", [128, 4, 1024], BF16)
                kvt = sb1("kvt", [128, 4, 512], BF16)
                ikt = sb1("ikt", [128, 4, 128], BF16)
                qs = sb1("qs", [128, 1024]); gs = sb1("gs", [128, 1024])
                aqt = sb1("aqt", [128, 1024], BF16)
                iqt = sb1("iqt", [128, 512]); iwt = sb1("iwt", [128, 8])
                eR = sb1("eR", [128, 1024]); eB = sb1("eB", [128, 1024])
                ktb = sb1("ktb", [128, 1024], BF16)
                qtb = sb1("qtb", [128, 1024], BF16); qtb1 = sb1("qtb1", [128, 1024], BF16); khb = sb1("khb", [128, 1024], BF16)
                dec = sb1("dec", [128, 16])
                Sst = sb1("Sst", [128, 1024])
                Sbf = [sb1("Sbf%d" % i, [128, 1024], BF16) for i in range(2)]
                qT0 = sb1("qT0", [128, 1024], BF16); qT1 = sb1("qT1", [128, 1024], BF16)
                khT = sb1("khT", [128, 1024], BF16)
                pT = [sb1("pT%d" % i, [128, 128], BF16) for i in range(2)]
                ssq = sb1("ssq", [128, 8]); rsq = sb1("rsq", [128, 8])
                recb = sb1("recb", [128, 1024], BF16)
                trs = sb1("trs", [128, 1536], BF16)
                recT = sb1("recT", [128, 1024], BF16)
                aqT = sb1("aqT", [128, 1024], BF16)
                iqs = sb1("iqs", [128, 512], BF16)
                iqT = sb1("iqT", [128, 512], BF16)
                aw = sb1("aw", [128, 8]); sgn = sb1("sgn", [128, 8])
                P.emit("pool", lambda: nc.gpsimd.memset(Sst[:], 0.0), [], [Sst.b])
                P.emit("pool", lambda: nc.gpsimd.memset(qtb[:], 0.0), [], [qtb.b])
                P.emit("pool", lambda: nc.gpsimd.memset(qtb1[:], 0.0), [], [qtb1.b])

                KTD = Buf("KT_d"); VD = Buf("V_d"); KID = Buf("kiT_d"); AQD = Buf("aqT_d"); IQD = Buf("iqT_d")
                SGD = Buf("sgn_d"); MIXD = Buf("mixT_d")
                win_v = win_bf.rearrange("(kc p) c -> p kc c", p=128)

                def rms_rstd(ssap, outap, n, scale, R, W):
                    P.ts("dve", outap, ssap, scale, EPS, ALU.mult, ALU.add, R, W)
                    P.act(outap, outap, AF.Sqrt, W, W)
                    P.emit("dve", lambda: nc.vector.reciprocal(out=outap, in_=outap), W, W)

                def proj_tile(c0, ncols, blks, evac):
                    w = wt[wrot[0] % 2]; wrot[0] += 1
                    P.dma(w[:, :, 0:ncols], win_v[:, :, c0:c0 + ncols], [winD], [w.b])
                    for blk in blks:
                        ps = psbank()
                        for kc in range(KC):
                            P.mm(ps[:, 0:ncols], hT[:, kc, blk * 128:(blk + 1) * 128], w[:, kc, 0:ncols],
                                 kc == 0, kc == KC - 1, [hT.b, w.b], [ps.b])
                        evac(ps, blk)

                def secA(sbi):
                    for blk in range(4):
                        p = sbi * 4 + blk
                        x_ = xt[p % 2]; xn_ = xn[p % 2]
                        P.dma(x_[:], x_sh[p * 128:(p + 1) * 128, :], [], [x_.b])
                        P.act(xn_[:], x_[:], AF.Square, [x_.b], [xn_.b, st.b], accum_out=st[:, 0:1])
                        rms_rstd(st[:, 0:1], st[:, 1:2], 1, 1.0 / D, [st.b], [st.b])
                        P.ts("dve", xn_[:], x_[:], st[:, 1:2], None, ALU.mult, None, [x_.b, st.b], [xn_.b])
                        for half in range(2):
                            ps = psbank()
                            pv = bfv(ps)
                            for k8 in range(8):
                                kc = half * 8 + k8
                                P.tr(pv[:, k8 * 128:(k8 + 1) * 128], xn_[:, kc * 128:(kc + 1) * 128], ident, [xn_.b, cstb.b], [ps.b])
                            for k8 in range(8):
                                kc = half * 8 + k8
                                o = hT[:, kc, blk * 128:(blk + 1) * 128]
                                i_ = pv[:, k8 * 128:(k8 + 1) * 128]
                                if k8 % 2 == 0:
                                    P.act(o, i_, AF.Identity, [ps.b, G1s.b, modT.b], [hT.b], scale=G1s[:, kc:kc + 1], bias=SH1s[:, kc:kc + 1])
                                else:
                                    P.ts("dve", o, i_, G1s[:, kc:kc + 1], SH1s[:, kc:kc + 1], ALU.mult, ALU.add, [ps.b, G1s.b, modT.b], [hT.b])
                    if sbi == 0:
                        dump("hT", hT, [128, KC, 512], BF16)
                def secB(sbi):
                    for ti in range(2):
                        proj_tile(C_RF + ti * 512, 512, range(4),
                                  lambda ps, blk, ti=ti: P.act(sgf[:, blk, ti * 512:(ti + 1) * 512], ps[:, :], AF.Sigmoid, [ps.b], [sgf.b]))
                    for blk in range(4):
                        P.tt("dve", sgf[:, blk, :], sgf[:, blk, :], OMLB[:], ALU.mult, [sgf.b, OMLB.b], [sgf.b])
                        P.tt("dve", sgf[:, blk, :], sgf[:, blk, :], LB[:], ALU.add, [sgf.b, LB.b], [sgf.b])
                        P.act(lgf[:, blk, :], sgf[:, blk, :], AF.Ln, [sgf.b], [lgf.b])
                        P.ts("pool", sgf[:, blk, :], sgf[:, blk, :], -1.0, 1.0, ALU.mult, ALU.add, [sgf.b, lgf.b], [sgf.b])
                    for ti in range(2):
                        proj_tile(C_RI + ti * 512, 512, range(4),
                                  lambda ps, blk, ti=ti: P.copy("act", vbf[:, blk, ti * 512:(ti + 1) * 512], ps[:, :], [ps.b], [vbf.b]))
                    proj_tile(C_AK, 512, range(4), lambda ps, blk: P.copy("dve", kvt[:, blk, :], ps[:, :], [ps.b], [kvt.b]))

                    def ev_ik(ps, blk):
                        P.copy("act", ikt[:, blk, 0:64], ps[:, 0:64], [ps.b], [ikt.b])
                        P.copy("act", ikt[:, blk, 64:128], ps[:, 0:64], [ps.b], [ikt.b])
                    proj_tile(C_IK, 64, range(4), ev_ik)
                    for ti in range(2):
                        proj_tile(C_RQ + ti * 512, 512, [3],
                                  lambda ps, blk, ti=ti: P.act(qs[:, ti * 512:(ti + 1) * 512], ps[:, :], AF.Silu, [ps.b], [qs.b]))
                    for ti in range(2):
                        proj_tile(C_RG + ti * 512, 512, [3],
                                  lambda ps, blk, ti=ti: P.act(gs[:, ti * 512:(ti + 1) * 512], ps[:, :], AF.Silu, [ps.b], [gs.b]))
                    for ti in range(2):
                        proj_tile(C_AQ + ti * 512, 512, [3],
                                  lambda ps, blk, ti=ti: P.copy("dve", aqt[:, ti * 512:(ti + 1) * 512], ps[:, :], [ps.b], [aqt.b]))
                    proj_tile(C_IQ, 512, [3], lambda ps, blk: P.copy("dve", iqt[:], ps[:, :], [ps.b], [iqt.b]))
                    proj_tile(C_IW, 8, [3], lambda ps, blk: P.copy("dve", iwt[:], ps[:, 0:8], [ps.b], [iwt.b]))

                def secD(sbi):
                    for blk in range(4):
                        p = sbi * 4 + blk
                        own = (blk == 3)
                        for ti in range(2):
                            ps = psbank()
                            P.mm(ps[:, :], TU, lgf[:, blk, ti * 512:(ti + 1) * 512], True, True, [cstf.b, lgf.b], [ps.b])
                            P.act(eR[:, ti * 512:(ti + 1) * 512], ps[:, :], AF.Exp, [ps.b], [eR.b])
                        P.stt(ktb[:], sgf[:, blk, :], vT[:, p:p + 1], eR[:], ALU.mult, ALU.mult, [sgf.b, vT.b, eR.b], [ktb.b])
                        psd = psbank()
                        for hd in range(8):
                            P.mm(psd[:, 2 * hd:2 * hd + 2], lgf[:, blk, hd * 128:(hd + 1) * 128], CI, True, True, [lgf.b, cstf.b], [psd.b])
                        P.act(dec[:], psd[:, 0:16], AF.Exp, [psd.b], [dec.b])
                        stop("stopD1")
                        if own:
                            for ti in range(2):
                                ps = psbank()
                                P.mm(ps[:, :], TL, lgf[:, blk, ti * 512:(ti + 1) * 512], True, True, [cstf.b, lgf.b], [ps.b])
                                P.act(eB[:, ti * 512:(ti + 1) * 512], ps[:, :], AF.Exp, [ps.b], [eB.b])
                                P.act(eR[:, ti * 512:(ti + 1) * 512], ps[:, :], AF.Exp, [ps.b, ktb.b], [eR.b], scale=-1.0)
                            P.stt(qtb[0:64, :], qs[0:64, :], 128.0 ** -0.5, eB[0:64, :], ALU.mult, ALU.mult, [qs.b, eB.b], [qtb.b])
                            P.stt(qtb1[64:128, :], qs[64:128, :], 128.0 ** -0.5, eB[64:128, :], ALU.mult, ALU.mult, [qs.b, eB.b], [qtb1.b])
                            P.tt("dve", khb[:], sgf[:, blk, :], eR[:], ALU.mult, [sgf.b, eR.b], [khb.b])
                            stop("stopD3a")
                            psq = psbank(); psk = psbank()
                            pq = bfv(psq); pk = bfv(psk)
                            for hd in range(8):
                                P.tr(pq[:, hd * 128:(hd + 1) * 128], qtb[:, hd * 128:(hd + 1) * 128], ident, [qtb.b, cstb.b], [psq.b])
                            for hd in range(8):
                                P.tr(pk[:, hd * 128:(hd + 1) * 128], khb[:, hd * 128:(hd + 1) * 128], ident, [khb.b, cstb.b], [psk.b])
                            psq1 = psbank(); pq1 = bfv(psq1)
                            for hd in range(8):
                                P.tr(pq1[:, hd * 128:(hd + 1) * 128], qtb1[:, hd * 128:(hd + 1) * 128], ident, [qtb1.b, cstb.b], [psq1.b])
                            stop("stopD3b")
                            P.copy("act", qT0[:], pq[:, :], [psq.b], [qT0.b])
                            P.copy("dve", qT1[:], pq1[:, :], [psq1.b], [qT1.b])
                            P.copy("act", khT[:], pk[:, :], [psk.b], [khT.b])
                            stop("stopD3")
                        for c in range(2):
                            if own:
                                P.copy("act" if c == 0 else "pool", Sbf[c][:], Sst[:], [Sst.b], [Sbf[c].b])
                            for hh in range(2):
                                ps = psbank()
                                for h4 in range(4):
                                    hd = hh * 4 + h4
                                    P.mm(ps[:, h4 * 128:(h4 + 1) * 128], ktb[c * 64:(c + 1) * 64, hd * 128:(hd + 1) * 128],
                                         vbf[c * 64:(c + 1) * 64, blk, hd * 128:(hd + 1) * 128], True, True, [ktb.b, vbf.b], [ps.b])
                                for h4 in range(4):
                                    hd = hh * 4 + h4
                                    P.stt(Sst[:, hd * 128:(hd + 1) * 128], Sst[:, hd * 128:(hd + 1) * 128], dec[:, 2 * hd + c:2 * hd + c + 1], ps[:, h4 * 128:(h4 + 1) * 128],
                                          ALU.mult, ALU.add, [Sst.b, dec.b, ps.b], [Sst.b])
                        stop("stopD2")
                        if own:
                            ob = sbi
                            pso = [PS[6], PS[7]]
                            for hd in range(8):
                                pss = psbank()
                                P.mm(pss[:, 0:128], khT[:, hd * 128:(hd + 1) * 128], qT0[:, hd * 128:(hd + 1) * 128], True, False, [khT.b, qT0.b], [pss.b])
                                P.mm(pss[:, 0:128], khT[:, hd * 128:(hd + 1) * 128], qT1[:, hd * 128:(hd + 1) * 128], False, True, [khT.b, qT1.b], [pss.b])
                                pt = pT[hd % 2]
                                P.tt("dve", pt[:], pss[:, 0:128], MBD, ALU.mult, [pss.b, cstf.b], [pt.b])
                                po = pso[hd // 4]
                                oo = po[:, (hd % 4) * 128:(hd % 4 + 1) * 128]
                                P.mm(oo, pt[:], vbf[:, blk, hd * 128:(hd + 1) * 128], True, False, [pt.b, vbf.b], [po.b])
                                P.mm(oo, qT0[:, hd * 128:(hd + 1) * 128], Sbf[0][:, hd * 128:(hd + 1) * 128], False, False, [qT0.b, Sbf[0].b], [po.b])
                                P.mm(oo, qT1[:, hd * 128:(hd + 1) * 128], Sbf[1][:, hd * 128:(hd + 1) * 128], False, True, [qT1.b, Sbf[1].b], [po.b])
                            stop("stopD3c")
                            for hd in range(8):
                                po = pso[hd // 4]
                                P.act(junk[:, 0:128], po[:, (hd % 4) * 128:(hd % 4 + 1) * 128], AF.Square, [po.b], [junk.b, ssq.b],
                                      accum_out=ssq[:, hd:hd + 1])
                            stop("stopD3d")
                            rms_rstd(ssq[:], rsq[:], 8, 1.0 / 128, [ssq.b], [rsq.b])
                            P.tt("dve", GS[:], gs[:], GR[:], ALU.mult, [gs.b, GR.b], [GS.b])
                            for hd in range(8):
                                po = pso[hd // 4]
                                P.stt(recb[:, hd * 128:(hd + 1) * 128], po[:, (hd % 4) * 128:(hd % 4 + 1) * 128], rsq[:, hd:hd + 1],
                                      GS[:, hd * 128:(hd + 1) * 128], ALU.mult, ALU.mult, [po.b, rsq.b, GS.b], [recb.b])
                            if sbi == 0:
                                dump("rec0", recb, [128, 1024], BF16)
                            stop("stopD4")
                            psr = psbank(); pr = bfv(psr)
                            for hd in range(8):
                                P.tr(pr[:, hd * 128:(hd + 1) * 128], recb[:, hd * 128:(hd + 1) * 128], ident, [recb.b, cstb.b], [psr.b])
                            P.copy("act", recT[:], pr[:, :], [psr.b], [recT.b])
                            P.dma(mixT_d[0:1024, ob * 128:(ob + 1) * 128].rearrange("(h p) t -> p h t", p=128), recT[:].rearrange("p (h t) -> p h t", h=8), [recT.b], [MIXD], semb=MIXD)
                            psa = psbank(); pa = bfv(psa)
                            for hd in range(8):
                                P.tr(pa[:, hd * 128:(hd + 1) * 128], aqt[:, hd * 128:(hd + 1) * 128], ident, [aqt.b, cstb.b], [psa.b])
                            P.copy("act", aqT[:], pa[:, :], [psa.b], [aqT.b])
                            P.dma(aqT_d[:, :, ob * 128:(ob + 1) * 128].rearrange("h p t -> p h t"), aqT[:].rearrange("p (h t) -> p h t", h=8), [aqT.b], [AQD], semb=AQD)
                            P.act(aw[:], iwt[:], AF.Abs, [iwt.b], [aw.b], scale=64.0 ** -0.5 * 8.0 ** -0.5)
                            P.ts("dve", sgn[:], iwt[:], 0.0, 2.0, ALU.is_ge, ALU.mult, [iwt.b], [sgn.b])
                            P.ts("dve", sgn[:], sgn[:], -1.0, None, ALU.add, None, [sgn.b], [sgn.b])
                            for h in range(8):
                                P.ts("pool", iqs[:, h * 64:(h + 1) * 64], iqt[:, h * 64:(h + 1) * 64], aw[:, h:h + 1], None, ALU.mult, None,
                                     [iqt.b, aw.b], [iqs.b])
                            psi = psbank(); pi = bfv(psi)
                            for c4 in range(4):
                                P.tr(pi[:, c4 * 128:(c4 + 1) * 128], iqs[:, c4 * 128:(c4 + 1) * 128], ident, [iqs.b, cstb.b], [psi.b])
                            P.copy("act", iqT[:], pi[:, 0:512], [psi.b], [iqT.b])
                            P.dma(iqT_d[:, :, ob * 128:(ob + 1) * 128].rearrange("h p t -> p h t"), iqT[:].rearrange("p (h t) -> p h t", h=4), [iqT.b], [IQD], semb=IQD)
                            P.dma(sgn_d[ob * 128:(ob + 1) * 128, :], sgn[:], [sgn.b], [SGD], semb=SGD)
                def secE(sbi):
                    pst = [psbank(), psbank()]
                    for blk in range(4):
                        for w3 in range(3):
                            idx = blk * 3 + w3
                            pv = bfv(pst[idx // 8])
                            src = kvt[:, blk, w3 * 128:(w3 + 1) * 128] if w3 < 2 else ikt[:, blk, :]
                            P.tr(pv[:, (idx % 8) * 128:(idx % 8 + 1) * 128], src, ident, [kvt.b, ikt.b, cstb.b], [pst[idx // 8].b])
                    P.copy("act", trs[:, 0:1024], bfv(pst[0])[:, :], [pst[0].b], [trs.b])
                    P.copy("dve", trs[:, 1024:1536], bfv(pst[1])[:, 0:512], [pst[1].b], [trs.b])
                    trv = trs[:].rearrange("p (b w t) -> p b w t", w=3, t=128)
                    t0 = sbi * 512
                    for kv in range(2):
                        P.dma(KT_d[kv, :, t0:t0 + 512].rearrange("p (b t) -> p b t", b=4), trv[:, :, kv, :], [trs.b], [KTD], semb=KTD)
                    P.dma(kiT_d[:, t0:t0 + 512].rearrange("p (b t) -> p b t", b=4), trv[:, :, 2, :], [trs.b], [KID], semb=KID)
                    P.dma(V_d[t0:t0 + 512, :].rearrange("(b p) c -> p b c", p=128), kvt[:, :, 256:512], [kvt.b], [VD], semb=VD)
                secA(0)
                for sbi in range(NSB):
                    secB(sbi)
                    if sbi + 1 < NSB:
                        secA(sbi + 1)
                    secD(sbi)
                    secE(sbi)
                dump("Sst", Sst, [128, 1024])

            stop("stop1")

            psmod[0] = 3
            NIT = 22
            P.set_barrier()
            with contextlib.ExitStack() as es2:
                def sb2(name, shape, dt=F32):
                    t = T.__new__(T)
                    t.t = es2.enter_context(nc.sbuf_tensor(name, shape, dt))
                    t.b = P.mkbuf(name)
                    return t
                KT = sb2("KT", [128, 2 * S], BF16)
                kiT = sb2("kiT", [128, S], BF16)
                Vaug = sb2("Vaug", [128, NB * 2 * 132], BF16)
                scoreL = [sb2("score%d" % k, [128, S]) for k in range(2)]
                nbiasL = [sb2("nbias%d" % k, [128, S], BF16) for k in range(2)]
                rbuf = [sb2("rbuf%d" % i, [128, 512], BF16) for i in range(2)]
                pbuf = [sb2("pbuf%d" % i, [128, 512], BF16) for i in range(2)]
                aqTiL = [sb2("aqTi%d" % k, [128, 1024], BF16) for k in range(2)]
                iqTi = sb2("iqTi", [128, 512], BF16)
                sgni = sb2("sgni", [128, 8])
                Dh = sb2("Dh", [128, 1024], BF16)
                CBf = sb2("CBf", [128, 512], BF16)
                PBt = sb2("PBt", [128, 512], BF16)
                BIGI4 = sb2("BIGI4", [128, 512], BF16)
                accS = sb2("accS", [128, 1056])
                nv = sb2("nv", [128, NO])
                smL = [sb2("sm%d" % k, [128, 16]) for k in range(2)]
                cjL = [sb2("cj%d" % k, [128, 8], BF16) for k in range(2)]
                sm2 = sb2("sm2", [128, 8]); sm3 = sb2("sm3", [128, 8]); sm4 = sb2("sm4", [128, 8])
                junk2 = sb2("junk2", [128, 128], BF16)
                attb = sb2("attb", [128, 1024], BF16)
                attT = sb2("attT", [128, 1024], BF16)
                P.dma(KT[:, 0:S], KT_d[0], [KTD], [KT.b])
                P.dma(KT[:, S:2 * S], KT_d[1], [KTD], [KT.b])
                P.dma(kiT[:], kiT_d[:, :], [KID], [kiT.b])
                P.emit("pool", lambda: nc.gpsimd.memset(Vaug[:], 1.0), [], [Vaug.b])
                Vv = Vaug[:].rearrange("p (b k c) -> p b k c", k=2, c=132)
                V_dv = V_d.rearrange("(b p) c -> p b c", p=128)
                for b0 in range(0, NB, 16):
                    b1 = min(NB, b0 + 16)
                    for kv in range(2):
                        P.dma(Vv[:, b0:b1, kv, 0:128], V_dv[:, b0:b1, kv * 128:(kv + 1) * 128], [VD], [Vaug.b])
                P.emit("pool", lambda: nc.gpsimd.memset(CBf[:], 0.0), [], [CBf.b])
                P.copy("pool", CBf[:, 384:512], CB, [cstf.b], [CBf.b])
                P.dma(scoreL[0][:, 0:512], padb[0:1, :].partition_broadcast(128), [], [scoreL[0].b])
                P.copy("pool", PBt[:], scoreL[0][:, 0:512], [scoreL[0].b], [PBt.b])
                for r4 in range(4):
                    P.ts("dve", BIGI4[:, r4 * 128:(r4 + 1) * 128], ident_f, 29952.0, None, ALU.mult, None, [cstf.b], [BIGI4.b])
                P.dma(nv[:], nvalT[:, :], [], [nv.b])
                acc = [PS[3], PS[4], PS[5]]
                def stageA(i):
                    score = scoreL[i % 2]; nbias = nbiasL[i % 2]; cj = cjL[i % 2]; aqTi = aqTiL[i % 2]; sm = smL[i % 2]
                    LO, HI, TH, CNT, GE, DD, NGE, SEL, AA, BB, THF = [sm[:, k:k + 1] for k in range(11)]
                    SMB = [sm.b]
                    nkt = i + 1
                    n = nkt * 512
                    nk = 4 * (i + 1)
                    P.dma(aqTi[:].rearrange("p (h t) -> p h t", h=8), aqT_d[:, :, i * 128:(i + 1) * 128].rearrange("h p t -> p h t"), [AQD], [aqTi.b])
                    P.dma(iqTi[:].rearrange("p (h t) -> p h t", h=4), iqT_d[:, :, i * 128:(i + 1) * 128].rearrange("h p t -> p h t"), [IQD], [iqTi.b])
                    P.dma(sgni[:], sgn_d[i * 128:(i + 1) * 128, :], [SGD], [sgni.b])
                    for h in range(8):
                        P.ts("dve", Dh[:, h * 128:(h + 1) * 128], ident_f, sgni[:, h:h + 1], None, ALU.mult, None, [cstf.b, sgni.b], [Dh.b])
                    for kt in range(nkt):
                        psc = PS[6 + kt % 2]
                        for h in range(8):
                            ps = psbank()
                            pb = (h % 2) * 64
                            P.mm(ps[:, :], iqTi[pb:pb + 64, (h // 2) * 128:(h // 2 + 1) * 128], kiT[pb:pb + 64, kt * 512:(kt + 1) * 512],
                                 True, True, [iqTi.b, kiT.b], [ps.b])
                            rb = rbuf[h % 2]
                            P.act(rb[:], ps[:, :], AF.Relu, [ps.b], [rb.b])
                            P.mm(psc[:, :], Dh[:, h * 128:(h + 1) * 128], rb[:], h == 0, h == 7, [Dh.b, rb.b], [psc.b])
                        dst = score[:, kt * 512:(kt + 1) * 512]
                        if kt == nkt - 1:
                            P.tt("dve", dst, psc[:, :], CBf[:], ALU.add, [psc.b, CBf.b], [score.b])
                            if kt == 0:
                                P.tt("dve", dst, dst, PBt[:], ALU.add, [score.b, PBt.b], [score.b])
                        elif kt == 0:
                            P.tt("dve", dst, psc[:, :], PBt[:], ALU.add, [psc.b, PBt.b], [score.b])
                        else:
                            P.copy("act", dst, psc[:, :], [psc.b], [score.b])
                    sc = score[:, 0:n]
                    P.emit("dve", lambda sc=sc: nc.vector.reduce_max(out=HI, in_=sc, axis=AX.X), [score.b], SMB)
                    P.ts("dve", LO, HI, -64.0, None, ALU.add, None, SMB, SMB)
                    for it in range(NIT):
                        cw = 64.0 / (2.0 ** (it + 1))
                        P.ts("dve", TH, LO, cw, None, ALU.add, None, SMB, SMB)
                        P.ts("dve", cj[:, 0:1].to_broadcast([128, n]), sc, TH, 0.0, ALU.is_ge, ALU.add, [score.b] + SMB, [cj.b] + SMB, accum_out=CNT)
                        P.ts("dve", GE, CNT, float(KSEL), cw, ALU.is_ge, ALU.mult, SMB, SMB)
                        P.tt("dve", LO, LO, GE, ALU.add, SMB, SMB)
                    P.ts("dve", SEL, nv[:, i:i + 1], float(KSEL), None, ALU.is_gt, None, [nv.b], SMB)
                    P.ts("dve", AA, SEL, 1e20, -1e20, ALU.mult, ALU.add, SMB, SMB)
                    P.tt("dve", BB, LO, SEL, ALU.mult, SMB, SMB)
                    P.tt("dve", THF, AA, BB, ALU.add, SMB, SMB)
                    P.ts("dve", nbias[:, 0:n], sc, THF, 1.0, ALU.is_ge, ALU.subtract, [score.b] + SMB, [nbias.b])
                def stageB(i):
                    nbias = nbiasL[i % 2]; aqTi = aqTiL[i % 2]
                    nk = 4 * (i + 1)
                    its = [(kb, kvh) for kb in range(nk) for kvh in range(2)]

                    def Lmm(k):
                        kb, kvh = its[k]
                        psl = psbank()
                        P.mm(psl[:, :], KT[:, kvh * S + kb * 128:kvh * S + (kb + 1) * 128], aqTi[:, kvh * 512:(kvh + 1) * 512],
                             True, False, [KT.b, aqTi.b], [psl.b])
                        P.mm(psl[:, :], nbias[:, kb * 128:(kb + 1) * 128], BIGI4[:], False, True, [nbias.b, BIGI4.b], [psl.b])
                        return psl
                    psl_next = Lmm(0)
                    for k, (kb, kvh) in enumerate(its):
                        psl = psl_next
                        if k + 1 < len(its):
                            psl_next = Lmm(k + 1)
                        pb_ = pbuf[k % 2]
                        P.act(pb_[:], psl[:, :], AF.Exp, [psl.b], [pb_.b], scale=128.0 ** -0.5)
                        vo = (kb * 2 + kvh) * 132
                        for g in range(4):
                            hd = kvh * 4 + g
                            a = acc[hd // 3]
                            off = (hd % 3) * 132
                            P.mm(a[:, off:off + 129], pb_[:, g * 128:(g + 1) * 128], Vaug[:, vo:vo + 129], (kb == 0 and hd % 3 == 0), False,
                                 [pb_.b, Vaug.b], [a.b], inc=(g == 3), skip_group_check=True)
                    for b3 in range(3):
                        wcp = 396 if b3 < 2 else 264
                        P.copy("act", accS[:, b3 * 396:b3 * 396 + wcp], acc[b3][:, 0:wcp], [acc[b3].b], [accS.b])
                    for hd in range(8):
                        off = (hd // 3) * 396 + (hd % 3) * 132
                        P.emit("dve", lambda off=off, hd=hd: nc.vector.reciprocal(out=sm2[:, hd:hd + 1], in_=accS[:, off + 128:off + 129]), [accS.b], [sm2.b])
                        P.act(junk2[:], accS[:, off:off + 128], AF.Square, [accS.b, sm2.b], [junk2.b, sm3.b], scale=sm2[:, hd:hd + 1],
                              accum_out=sm3[:, hd:hd + 1])
                    rms_rstd(sm3[:], sm4[:], 8, 1.0 / 128, [sm3.b], [sm4.b])
                    P.tt("dve", sm4[:], sm4[:], sm2[:], ALU.mult, [sm4.b, sm2.b], [sm4.b])
                    for hd in range(8):
                        off = (hd // 3) * 396 + (hd % 3) * 132
                        P.ts("dve", attb[:, hd * 128:(hd + 1) * 128], accS[:, off:off + 128], sm4[:, hd:hd + 1], None, ALU.mult, None,
                             [accS.b, sm4.b], [attb.b])
                    if i == 0:
                        dump("att0", attb, [128, 1024], BF16)
                    if i == 1:
                        dump("att1", attb, [128, 1024], BF16)
                    psr = psbank(); pr = bfv(psr)
                    for hd in range(8):
                        P.tr(pr[:, hd * 128:(hd + 1) * 128], attb[:, hd * 128:(hd + 1) * 128], ident, [attb.b, cstb.b], [psr.b])
                    P.copy("act", attT[:], pr[:, :], [psr.b], [attT.b])
                    P.dma(mixT_d[1024:2048, i * 128:(i + 1) * 128].rearrange("(h p) t -> p h t", p=128), attT[:].rearrange("p (h t) -> p h t", h=8),
                          [attT.b], [MIXD], semb=MIXD)
                stageA(0)
                for i in range(NO):
                    if i + 1 < NO:
                        stageA(i + 1)
                    stageB(i)
            psmod[0] = 6
            stop("stop2")

            h2T_d = dscr("h2T_d", [D, TO], BF16)
            H2D = Buf("h2T_d"); X1D = Buf("x1_d"); OUTD = Buf("out")
            P.set_barrier()
            with contextlib.ExitStack() as es34:
                def sb34(name, shape, dt=F32, st=es34):
                    t = T.__new__(T)
                    t.t = st.enter_context(nc.sbuf_tensor(name, shape, dt))
                    t.b = P.mkbuf(name)
                    return t
                comb = sb34("comb", [128, NO * 32])
                with contextlib.ExitStack() as es3:
                    sb3 = lambda name, shape, dt=F32: sb34(name, shape, dt, es3)
                    Wo = sb3("Wo", [128, KC * 2048], BF16)
                    wst3 = [sb3("wst3_%d" % i, [128, 2048]) for i in range(2)]
                    gaTt = sb3("gaTt", [128, 8])
                    P.dma(gaTt[:], gaT_in[:, :], [], [gaTt.b])
                    for kc in range(KC):
                        w = wst3[kc % 2]
                        P.dma(w[:], w_out[kc * 128:(kc + 1) * 128, :], [], [w.b])
                        if kc < 8:
                            P.copy("act" if kc % 2 == 0 else "dve", Wo[:, kc * 2048:(kc + 1) * 2048], w[:], [w.b], [Wo.b])
                        else:
                            P.act(Wo[:, kc * 2048:(kc + 1) * 2048], w[:], AF.Identity, [w.b, gaTt.b], [Wo.b], scale=gaTt[:, kc - 8:kc - 7])
                    GT1b = sb3("GT1b", [128, 2048])
                    mod6 = mod_d.rearrange("(a n) -> a n", a=6)
                    P.dma(GT1b[:], mod6[2:3, :].partition_broadcast(128), [modD], [GT1b.b])
                    Wrt = sb3("Wrt", [128, KC * 36]); Wrtb = sb3("Wrtb", [128, KC * 36], BF16)
                    P.dma(Wrt[:].rearrange("p (k c) -> p k c", c=36), w_rt.rearrange("(k p) c -> p k c", p=128), [], [Wrt.b])
                    P.copy("dve", Wrtb[:], Wrt[:], [Wrt.b], [Wrtb.b])
                    brt = sb3("brt", [128, 36])
                    P.dma(brt[:], b_rt[0:1, :].partition_broadcast(128), [], [brt.b])
                    xo = [sb3("xo%d" % i, [128, D]) for i in range(2)]
                    x1t = [sb3("x1t%d" % i, [128, D]) for i in range(2)]
                    xn2 = sb3("xn2", [128, D], BF16)
                    mixTi = sb3("mixTi", [128, KC * 128], BF16)
                    h2Tb = sb3("h2Tb", [128, KC * 128], BF16)
                    st3 = sb3("st3", [128, 8])
                    lg = sb3("lg", [128, 36])
                    rs = sb3("rs", [128, 64])
                    for i in range(NO):
                        x_ = xo[i % 2]; x1 = x1t[i % 2]
                        p = 4 * i + 3
                        P.dma(x_[:], x_sh[p * 128:(p + 1) * 128, :], [], [x_.b])
                        P.dma(mixTi[:].rearrange("p (k t) -> p k t", t=128), mixT_d[:, i * 128:(i + 1) * 128].rearrange("(k p) t -> p k t", p=128),
                              [MIXD], [mixTi.b])
                        for dt in range(4):
                            ps = psbank()
                            for kc in range(KC):
                                P.mm(ps[:, :], mixTi[:, kc * 128:(kc + 1) * 128], Wo[:, kc * 2048 + dt * 512:kc * 2048 + (dt + 1) * 512],
                                     kc == 0, kc == KC - 1, [mixTi.b, Wo.b], [ps.b])
                            P.tt("dve", x1[:, dt * 512:(dt + 1) * 512], ps[:, :], GT1b[:, dt * 512:(dt + 1) * 512], ALU.mult, [ps.b, GT1b.b], [x1.b])
                        P.tt("dve", x1[:], x1[:], x_[:], ALU.add, [x1.b, x_.b], [x1.b])
                        if i == 0:
                            dump("x1", x1, [128, D])
                        P.dma(x1_d[i * 128:(i + 1) * 128, :], x1[:], [x1.b], [X1D], semb=X1D)
                        P.act(xn2[:], x1[:], AF.Square, [x1.b], [xn2.b, st3.b], accum_out=st3[:, 0:1])
                        rms_rstd(st3[:, 0:1], st3[:, 1:2], 1, 1.0 / D, [st3.b], [st3.b])
                        P.ts("dve", xn2[:], x1[:], st3[:, 1:2], None, ALU.mult, None, [x1.b, st3.b], [xn2.b])
                        for half in range(2):
                            ps = psbank()
                            pv = bfv(ps)
                            for k8 in range(8):
                                kc = half * 8 + k8
                                P.tr(pv[:, k8 * 128:(k8 + 1) * 128], xn2[:, kc * 128:(kc + 1) * 128], ident, [xn2.b, cstb.b], [ps.b])
                            for k8 in range(8):
                                kc = half * 8 + k8
                                o = h2Tb[:, kc * 128:(kc + 1) * 128]
                                i_ = pv[:, k8 * 128:(k8 + 1) * 128]
                                if k8 % 2 == 0:
                                    P.act(o, i_, AF.Identity, [ps.b, G2s.b, modT.b], [h2Tb.b], scale=G2s[:, kc:kc + 1], bias=SH2s[:, kc:kc + 1])
                                else:
                                    P.ts("dve", o, i_, G2s[:, kc:kc + 1], SH2s[:, kc:kc + 1], ALU.mult, ALU.add, [ps.b, G2s.b, modT.b], [h2Tb.b])
                        P.dma(h2T_d[:, i * 128:(i + 1) * 128].rearrange("(k p) t -> p k t", p=128), h2Tb[:].rearrange("p (k t) -> p k t", t=128),
                              [h2Tb.b], [H2D], semb=H2D)
                        psr = psbank()
                        for kc in range(KC):
                            P.mm(psr[:, 0:36], h2Tb[:, kc * 128:(kc + 1) * 128], Wrtb[:, kc * 36:(kc + 1) * 36], kc == 0, kc == KC - 1,
                                 [h2Tb.b, Wrtb.b], [psr.b])
                        P.tt("dve", lg[:], psr[:, 0:36], brt[:], ALU.add, [psr.b, brt.b], [lg.b])
                        RB = [rs.b]
                        GMAX, NGM, SE, PG, M1, M2, DLT, EX, DEN, W1, W2 = [rs[:, k:k + 1] for k in range(11)]
                        OHG = rs[:, 12:16]; ESEL = rs[:, 16:24]; MK1 = rs[:, 24:32]; E2 = rs[:, 32:40]; MK2 = rs[:, 40:48]; CIG = rs[:, 48:56]; EG = rs[:, 56:60]
                        P.emit("dve", lambda: nc.vector.reduce_max(out=GMAX, in_=lg[:, 0:4], axis=AX.X), [lg.b], RB)
                        P.ts("dve", NGM, GMAX, -1.0, None, ALU.mult, None, RB, RB)
                        P.act(EG, lg[:, 0:4], AF.Exp, [lg.b] + RB, RB, bias=NGM, accum_out=SE)
                        P.emit("dve", lambda: nc.vector.reciprocal(out=PG, in_=SE), RB, RB)
                        P.ts("dve", OHG, lg[:, 0:4], GMAX, None, ALU.is_ge, None, [lg.b] + RB, RB)
                        P.ts("dve", ESEL, lg[:, 4:12], rs[:, 12:13], None, ALU.mult, None, [lg.b] + RB, RB)
                        for g in range(1, 4):
                            P.stt(ESEL, lg[:, 4 + 8 * g:12 + 8 * g], rs[:, 12 + g:13 + g], ESEL, ALU.mult, ALU.add, [lg.b] + RB, RB)
                        P.emit("dve", lambda: nc.vector.reduce_max(out=M1, in_=ESEL, axis=AX.X), RB, RB)
                        P.ts("dve", MK1, ESEL, M1, None, ALU.is_ge, None, RB, RB)
                        P.stt(E2, MK1, -1e30, ESEL, ALU.mult, ALU.add, RB, RB)
                        P.emit("dve", lambda: nc.vector.reduce_max(out=M2, in_=E2, axis=AX.X), RB, RB)
                        P.ts("dve", MK2, E2, M2, None, ALU.is_ge, None, RB, RB)
                        P.tt("dve", DLT, M2, M1, ALU.subtract, RB, RB)
                        P.act(EX, DLT, AF.Exp, RB, RB)
                        P.ts("dve", DEN, EX, 1.0, None, ALU.add, None, RB, RB)
                        P.emit("dve", lambda: nc.vector.reciprocal(out=DEN, in_=DEN), RB, RB)
                        P.tt("dve", W1, DEN, PG, ALU.mult, RB, RB)
                        P.tt("dve", W2, W1, EX, ALU.mult, RB, RB)
                        P.ts("dve", CIG, MK1, W1, None, ALU.mult, None, RB, RB)
                        P.stt(CIG, MK2, W2, CIG, ALU.mult, ALU.add, RB, RB)
                        for g in range(4):
                            P.ts("dve", comb[:, i * 32 + g * 8:i * 32 + (g + 1) * 8], CIG, rs[:, 12 + g:13 + g], None, ALU.mult, None, RB, [comb.b])
                stop("stop3")
                P.set_barrier()
                with contextlib.ExitStack() as es4:
                    sb4 = lambda name, shape, dt=F32: sb34(name, shape, dt, es4)
                    CH = min(TO, 1024)
                    NCH = TO // CH
                    NT = CH // 128
                    SUB = min(512, CH)
                    h2c = sb4("h2c", [128, KC * CH], BF16)
                    yacc = sb4("yacc", [128, NT * 2048])
                    WGb = sb4("WGb", [128, KC * 512], BF16); WUb = sb4("WUb", [128, KC * 512], BF16); WDb = sb4("WDb", [128, 4 * 2048], BF16)
                    stg = [sb4("stg%d" % k, [128, 2048]) for k in range(4)]
                    sgt = [sb4("sgt%d" % k, [128, 512]) for k in range(2)]
                    heT = [sb4("heT%d" % k, [128, 4 * 512], BF16) for k in range(2)]
                    xf = sb4("xf", [128, 2048])
                    st4 = sb4("st4", [128, 8])
                    srot = [0]

                    def load_cast(dst_ap, dstb, src_ap, three=None):
                        w = stg[srot[0] % 4]
                        eng = "act" if srot[0] % 4 != 3 else "dve"
                        srot[0] += 1
                        if three is None:
                            P.dma(w[:], src_ap, [], [w.b])
                        else:
                            P.dma(w[:].rearrange("p (k c) -> p k c", c=512), src_ap, [], [w.b])
                        P.copy(eng, dst_ap, w[:], [w.b], [dstb])

                    for ch in range(NCH):
                        P.dma(h2c[:].rearrange("p (k t) -> p k t", t=CH), h2T_d[:, ch * CH:(ch + 1) * CH].rearrange("(k p) t -> p k t", p=128),
                              [H2D], [h2c.b])
                        P.emit("pool", lambda: nc.gpsimd.memset(yacc[:], 0.0), [], [yacc.b])
                        for e in range(NE):
                            for q in range(4):
                                load_cast(WGb[:, q * 2048:(q + 1) * 2048], WGb.b, w_eg[e % ne_decl, q * 512:(q + 1) * 512, :].rearrange("(k p) c -> p k c", p=128), 1)
                            for q in range(4):
                                load_cast(WUb[:, q * 2048:(q + 1) * 2048], WUb.b, w_eu[e % ne_decl, q * 512:(q + 1) * 512, :].rearrange("(k p) c -> p k c", p=128), 1)
                            for c in range(4):
                                load_cast(WDb[:, c * 2048:(c + 1) * 2048], WDb.b, w_ed[e % ne_decl, c * 128:(c + 1) * 128, :])
                            for st_ in range(CH // SUB):
                                tok0 = st_ * SUB
                                he = heT[st_ % 2]
                                for c in range(4):
                                    psg = psbank(); psu = psbank()
                                    for kc in range(KC):
                                        P.mm(psg[:, 0:SUB], WGb[:, kc * 512 + c * 128:kc * 512 + (c + 1) * 128], h2c[:, kc * CH + tok0:kc * CH + tok0 + SUB],
                                             kc == 0, kc == KC - 1, [WGb.b, h2c.b], [psg.b])
                                    for kc in range(KC):
                                        P.mm(psu[:, 0:SUB], WUb[:, kc * 512 + c * 128:kc * 512 + (c + 1) * 128], h2c[:, kc * CH + tok0:kc * CH + tok0 + SUB],
                                             kc == 0, kc == KC - 1, [WUb.b, h2c.b], [psu.b])
                                    sg = sgt[c % 2]
                                    P.act(sg[:, 0:SUB], psg[:, 0:SUB], AF.Silu, [psg.b], [sg.b])
                                    P.tt("dve", he[:, c * 512:c * 512 + SUB], sg[:, 0:SUB], psu[:, 0:SUB], ALU.mult, [sg.b, psu.b], [he.b])
                                for t_ in range(SUB // 128):
                                    tile = st_ * (SUB // 128) + t_
                                    gt = ch * NT + tile
                                    for dt in range(4):
                                        psd = psbank()
                                        for c in range(4):
                                            P.mm(psd[:, :], he[:, c * 512 + t_ * 128:c * 512 + (t_ + 1) * 128], WDb[:, c * 2048 + dt * 512:c * 2048 + (dt + 1) * 512],
                                                 c == 0, c == 3, [he.b, WDb.b], [psd.b])
                                        ya = yacc[:, tile * 2048 + dt * 512:tile * 2048 + (dt + 1) * 512]
                                        P.stt(ya, psd[:, :], comb[:, gt * 32 + e:gt * 32 + e + 1], ya, ALU.mult, ALU.add, [psd.b, comb.b, yacc.b], [yacc.b])
                        GT2b = stg[0]; GFb = stg[1]
                        P.dma(GT2b[:], mod6[5:6, :].partition_broadcast(128), [modD], [GT2b.b])
                        P.dma(GFb[:], g_fin[0:1, :].partition_broadcast(128), [], [GFb.b])
                        for tile in range(NT):
                            gt = ch * NT + tile
                            P.dma(xf[:], x1_d[gt * 128:(gt + 1) * 128, :], [X1D], [xf.b])
                            ya = yacc[:, tile * 2048:(tile + 1) * 2048]
                            P.tt("dve", ya, ya, GT2b[:], ALU.mult, [yacc.b, GT2b.b], [yacc.b])
                            P.tt("dve", xf[:], xf[:], ya, ALU.add, [xf.b, yacc.b], [xf.b])
                            P.act(ya, xf[:], AF.Square, [xf.b], [yacc.b, st4.b], accum_out=st4[:, 0:1])
                            rms_rstd(st4[:, 0:1], st4[:, 1:2], 1, 1.0 / D, [st4.b], [st4.b])
                            P.stt(xf[:], xf[:], st4[:, 1:2], GFb[:], ALU.mult, ALU.mult, [xf.b, st4.b, GFb.b], [xf.b])
                            P.dma(out_d[gt * 128:(gt + 1) * 128, :], xf[:], [xf.b], [OUTD], semb=OUTD)


        try:
            body()
        except _Stop:
            pass
        for b_ in P.dmabufs:
            P.final.append((b_.sem, b_.cnt))
        P.replay()
    return nc, dbg_out


def host_consts():
    c = np.zeros((128, 1024), np.float32)
    i = np.arange(128)
    c[:, 0:128] = np.eye(128)
    same = (i[:, None] // 64) == (i[None, :] // 64)
    c[:, 128:256] = (same & (i[:, None] <= i[None, :])).astype(np.float32)
    c[:, 256:384] = (same & (i[:, None] > i[None, :])).astype(np.float32)
    c[:, 384] = (i < 64)
    c[:, 385] = (i >= 64)
    c[:, 512:640] = np.where(i[None, :] <= i[:, None], 0.0, -1e30)
    c[:, 640:768] = np.eye(128)
    return c


def make_in_maps(inputs, S, ne=NE):
    f = lambda a: np.ascontiguousarray(np.asarray(a, dtype=np.float32))
    x = f(inputs["x"]); c = f(inputs["c"])
    NB = S // 128; NO = NB // 4
    pT = lambda v: np.ascontiguousarray(v.reshape(-1, 128).T)
    shared = {
        "w_ada": f(inputs["w_ada"][0]), "badaT": pT(f(inputs["b_ada"][0])),
        "gnmT": pT(f(inputs["g_norm_mix"][0])), "gnfT": pT(f(inputs["g_norm_ffn"][0])),
        "w_in": f(inputs["w_in"][0]), "lbl": f(inputs["lb_logits"]),
        "g_rec": f(inputs["g_rec_out"]), "g_att": f(inputs["g_att_out"]), "gaT": pT(f(inputs["g_att_out"][0])),
        "w_out": f(inputs["w_out"][0]),
        "w_rt": np.ascontiguousarray(np.concatenate([f(inputs["w_router_group"][0]), f(inputs["w_router_expert"][0])], axis=1)),
        "b_rt": np.ascontiguousarray(np.concatenate([f(inputs["b_router_group"][0]), f(inputs["b_router_expert"][0])])[None, :]),
        "w_eg": f(inputs["w_expert_gate"][0][:ne]), "w_eu": f(inputs["w_expert_up"][0][:ne]), "w_ed": f(inputs["w_expert_down"][0][:ne]),
        "g_fin": f(inputs["g_final"])[None, :], "cst": host_consts(),
    }
    maps = []
    for core in range(8):
        b, j = core // 4, core % 4
        npad = (3 - j) * 128
        xs = np.zeros((S, D), np.float32)
        xs[npad:] = x[b, :S - npad]
        valid = np.ones(S, np.float32); valid[:npad] = 0
        padb = np.zeros((1, 512), np.float32); padb[0, :npad] = -1e30
        nval = np.zeros((128, NO), np.float32)
        for i in range(NO):
            nval[:, i] = (4 * i + 3) * 128 + np.arange(128) - npad + 1
        m = dict(shared)
        m.update({"x_sh": xs, "cT": pT(c[b]), "validT": pT(valid), "padb": padb, "nvalT": nval})
        maps.append(m)
    return maps


def assemble(results, S, B=2):
    NB = S // 128; NO = NB // 4
    out = np.zeros((B, S, D), np.float32)
    for core in range(8):
        b, j = core // 4, core % 4
        o = np.asarray(results[core]["out"]).reshape(NO, 128, D)
        for i in range(NO):
            g = 4 * i + j
            out[b, g * 128:(g + 1) * 128] = o[i]
    return out


_CACHE = {}


def kernel(**inputs):
    S = int(np.asarray(inputs["x"]).shape[1])
    if S not in _CACHE:
        _CACHE[S] = build(S)[0]
    nc = _CACHE[S]
    maps = make_in_maps(inputs, S)
    res = run_bass_kernel_spmd(nc, maps, core_ids=list(range(8)))
    return assemble(res.results, S)
```

```python
import contextlib
import numpy as np
import ml_dtypes
import concourse.bass as bass
import concourse.mybir as mybir
from concourse.bass_utils import run_bass_kernel_spmd

F32 = mybir.dt.float32
BF16 = mybir.dt.bfloat16
AF = mybir.ActivationFunctionType
ALU = mybir.AluOpType
AX = mybir.AxisListType

D = 2048
KC = 16
IN_COLS = 6216
NE = 32
DE = 512
EPS = 1e-6
NEG = -30000.0
SAME_ENGINE_SYNC = True
C_RQ, C_RF, C_RI, C_RG, C_AQ, C_AK, C_AV, C_IQ, C_IK, C_IW = 0, 1024, 2048, 3072, 4096, 5120, 5376, 5632, 6144, 6208


class Buf:
    __slots__ = ("name", "w", "r", "sem", "cnt")

    def __init__(self, name):
        self.name = name
        self.w = None
        self.r = []
        self.sem = None
        self.cnt = 0


class Prog:
    ENGS = ("pe", "act", "dve", "pool", "sp")

    def __init__(self, nc, es):
        self.nc = nc
        self.es = es
        self.q = {e: [] for e in self.ENGS}
        self.cnt = {e: 0 for e in self.ENGS}
        self.esem = {e: es.enter_context(nc.semaphore("sem_" + e)) for e in ("pe", "act", "dve", "pool")}
        self.seen = {e: {} for e in self.ENGS}
        self.nsem = 0
        self.final = []
        self.dmabufs = []
        self.stopped = False
        self.barrier = []

    def set_barrier(self):
        toks = [(e, self.esem[e], self.cnt[e]) for e in ("pe", "act", "dve", "pool") if self.cnt[e] > 0]
        for b in self.dmabufs:
            toks.append(("d%d" % id(b), b.sem, b.cnt))
        self.barrier = toks

    def mkbuf(self, name):
        b = Buf(name)
        b.r = list(self.barrier)
        return b

    def newsem(self, name):
        self.nsem += 1
        return self.es.enter_context(self.nc.semaphore("d_%s_%d" % (name, self.nsem)))

    def _waits(self, eng, R, W):
        toks = []
        for b in R:
            if b.w is not None:
                toks.append(b.w)
        for b in W:
            if b.w is not None:
                toks.append(b.w)
            toks.extend(b.r)
        out = {}
        for (key, sem, v) in toks:
            if key == eng and (eng == "pe" or not SAME_ENGINE_SYNC):
                continue
            if self.seen[eng].get(key, 0) >= v:
                continue
            if out.get(key, (None, 0))[1] < v:
                out[key] = (sem, v)
        for key, (sem, v) in out.items():
            self.seen[eng][key] = v
        return list(out.values())

    def emit(self, eng, fn, R=(), W=(), inc=True):
        if self.stopped:
            return
        waits = self._waits(eng, R, W)
        sem = self.esem[eng]
        if inc:
            self.cnt[eng] += 1
            c = self.cnt[eng]
            self.q[eng].append((waits, fn, sem, 1))
        else:
            c = self.cnt[eng] + 1
            self.q[eng].append((waits, fn, sem, 0))
        tok = (eng, sem, c)
        for b in W:
            b.w = tok
            b.r = []
        for b in R:
            if b not in W:
                b.r.append(tok)

    def dma(self, out, in_, R, W, semb=None, q="sp"):
        semb = semb or W[0]
        if self.stopped:
            return (None, None, 0)
        if semb.sem is None:
            semb.sem = self.newsem(semb.name)
            self.dmabufs.append(semb)
        waits = self._waits(q, R, W)
        semb.cnt += 16
        nc = self.nc
        eng = {"sp": nc.sync, "act": nc.scalar, "pool": nc.gpsimd}[q]
        self.q[q].append((waits, lambda: eng.dma_start(out=out, in_=in_), semb.sem, 16))
        tok = ("d%d" % id(semb), semb.sem, semb.cnt)
        for b in W:
            b.w = tok
            b.r = []
        for b in R:
            b.r.append(tok)
        return tok

    def act(self, out, in_, func, R, W, **kw):
        nc = self.nc
        self.emit("act", lambda: nc.scalar.activation(out=out, in_=in_, func=func, **kw), R, W)

    def ts(self, eng, out, in0, s1, s2, op0, op1, R, W, **kw):
        e = self.nc.vector if eng == "dve" else self.nc.gpsimd
        if op1 is None:
            self.emit(eng, lambda: e.tensor_scalar(out=out, in0=in0, scalar1=s1, scalar2=None, op0=op0, **kw), R, W)
        else:
            self.emit(eng, lambda: e.tensor_scalar(out=out, in0=in0, scalar1=s1, scalar2=s2, op0=op0, op1=op1, **kw), R, W)

    def tt(self, eng, out, in0, in1, op, R, W):
        e = self.nc.vector if eng == "dve" else self.nc.gpsimd
        self.emit(eng, lambda: e.tensor_tensor(out=out, in0=in0, in1=in1, op=op), R, W)

    def stt(self, out, in0, scalar, in1, op0, op1, R, W):
        nc = self.nc
        self.emit("dve", lambda: nc.vector.scalar_tensor_tensor(out=out, in0=in0, scalar=scalar, in1=in1, op0=op0, op1=op1), R, W)

    def copy(self, eng, out, in_, R, W):
        nc = self.nc
        if eng == "act":
            self.emit("act", lambda: nc.scalar.copy(out=out, in_=in_), R, W)
        elif eng == "dve":
            self.emit("dve", lambda: nc.vector.tensor_copy(out=out, in_=in_), R, W)
        else:
            self.emit("pool", lambda: nc.gpsimd.tensor_copy(out=out, in_=in_), R, W)

    def mm(self, out, lhsT, rhs, start, stop, R, W, inc=None, **kw):
        nc = self.nc
        if inc is None:
            inc = bool(stop)
        self.emit("pe", lambda: nc.tensor.matmul(out, lhsT, rhs, start=start, stop=stop, **kw), R, W, inc=inc)

    def tr(self, out, in_, ident, R, W, inc=True):
        nc = self.nc
        self.emit("pe", lambda: nc.tensor.transpose(out, in_, ident), R, W, inc=inc)

    def replay(self):
        nc = self.nc
        engmap = {"pe": "tensor", "act": "scalar", "dve": "vector", "pool": "gpsimd", "sp": "sync"}
        with nc.Block() as blk:
            for e in self.ENGS:
                lst = self.q[e]
                final = self.final

                def body(eng, lst=lst, e=e):
                    for (waits, fn, sem, n) in lst:
                        for (s, v) in waits:
                            eng.wait_ge(s, v)
                        if n:
                            fn().then_inc(sem, n)
                        else:
                            fn()
                    if e == "sp":
                        for (s, v) in final:
                            eng.wait_ge(s, v)

                getattr(blk, engmap[e])(body)


class T:
    def __init__(self, P, kind, name, shape, dtype):
        nc = P.nc
        if kind == "sb":
            self.t = P.es.enter_context(nc.sbuf_tensor(name, shape, dtype))
        else:
            self.t = P.es.enter_context(nc.psum_tensor(name, shape, dtype))
        self.b = Buf(name)

    def __getitem__(self, k):
        return self.t[k]


def build(S, dbg=None):
    NB = S // 128
    NSB = NB // 4
    NO = NSB
    TO = NO * 128
    KSEL = min(256, S // 4)
    dbg = dbg or ()
    nc = bass.Bass("TRN2", target_bir_lowering=False)

    def din(name, shape, dt=F32):
        return nc.dram_tensor(name, list(shape), dt, kind="ExternalInput").ap()

    def dscr(name, shape, dt):
        return nc.dram_tensor(name, list(shape), dt, kind="Internal").ap()

    x_sh = din("x_sh", [S, D])
    cT = din("cT", [128, KC])
    w_ada = din("w_ada", [D, 6 * D])
    badaT = din("badaT", [128, 96])
    gnmT = din("gnmT", [128, KC])
    gnfT = din("gnfT", [128, KC])
    w_in = din("w_in", [D, IN_COLS])
    lbl = din("lbl", [2, 1024])
    g_rec = din("g_rec", [1, 1024])
    g_att = din("g_att", [1, 1024])
    gaT_in = din("gaT", [128, 8])
    w_out = din("w_out", [D, D])
    w_rt = din("w_rt", [D, 36])
    b_rt = din("b_rt", [1, 36])
    ne_decl = 1 if any(d.startswith("stop") for d in dbg) else NE
    w_eg = din("w_eg", [ne_decl, D, DE])
    w_eu = din("w_eu", [ne_decl, D, DE])
    w_ed = din("w_ed", [ne_decl, DE, D])
    g_fin = din("g_fin", [1, D])
    validT = din("validT", [128, NB])
    padb = din("padb", [1, 512])
    nvalT = din("nvalT", [128, NO])
    cst = din("cst", [128, 8 * 128])
    out_d = nc.dram_tensor("out", [TO, D], F32, kind="ExternalOutput").ap()

    win_bf = dscr("win_bf", [D, IN_COLS], BF16)
    mod_d = dscr("mod_d", [96 * 128], F32)
    KT_d = dscr("KT_d", [2, 128, S], BF16)
    V_d = dscr("V_d", [S, 256], BF16)
    kiT_d = dscr("kiT_d", [128, S], BF16)
    aqT_d = dscr("aqT_d", [8, 128, TO], BF16)
    iqT_d = dscr("iqT_d", [4, 128, TO], BF16)
    sgn_d = dscr("sgn_d", [TO, 8], F32)
    mixT_d = dscr("mixT_d", [D, TO], BF16)
    x1_d = dscr("x1_d", [TO, D], F32)

    dbg_out = {}

    class _Stop(Exception):
        pass

    es = contextlib.ExitStack()
    with es:
        P = Prog(nc, es)
        allbufs = []

        def stop(tag):
            if tag in dbg:
                P.stopped = True

        def body():

            def sb(name, shape, dt=F32):
                return T(P, "sb", name, shape, dt)

            PS = [T(P, "ps", "ps%d" % i, [128, 512], F32) for i in range(8)]
            psrot = [0]
            psmod = [6]

            def psbank():
                t = PS[psrot[0] % psmod[0]]
                psrot[0] += 1
                return t

            def bfv(ps):
                return ps.t[:, :].bitcast(BF16)

            def dump(name, tile, shape, dt=F32):
                if name not in dbg:
                    return
                o = nc.dram_tensor("dbg_" + name, list(shape), dt, kind="ExternalOutput").ap()
                b = Buf("dbg_" + name)
                tok = P.dma(o, tile.t[:] if isinstance(tile, T) else tile[0], [tile.b if isinstance(tile, T) else tile[1]], [b])
                P.final.append((tok[1], tok[2]))
                dbg_out[name] = (shape, dt)

            cstf = sb("cstf", [128, 8 * 128])
            P.dma(cstf[:], cst[:, :], [], [cstf.b])
            ident_f = cstf[:, 0:128]
            TL = cstf[:, 128:256]
            TU = cstf[:, 256:384]
            CI = cstf[:, 384:386]
            CB = cstf[:, 512:640]
            cstb = sb("cstb", [128, 8 * 128], BF16)
            P.copy("dve", cstb[:], cstf[:], [cstf.b], [cstb.b])
            ident = cstb[:, 0:128]
            MBD = cstf[:, 128:256]
            I4 = cstb[:, 640:640 + 128]

            cTt = sb("cTt", [128, KC])
            P.dma(cTt[:], cT[:, :], [], [cTt.b])
            cact = sb("cact", [128, KC])
            P.act(cact[:], cTt[:], AF.Silu, [cTt.b], [cact.b])
            modps = psbank()
            modT = sb("modT", [128, 96])
            bT = sb("bT", [128, 96])
            modTT = sb("modTT", [96, 128])
            g1t = sb("g1t", [128, KC]); g2t = sb("g2t", [128, KC])
            G1s = sb("G1s", [128, KC]); G2s = sb("G2s", [128, KC])
            with contextlib.ExitStack() as es0:
                wad = [T.__new__(T) for _ in range(2)]
                for i, w in enumerate(wad):
                    w.t = es0.enter_context(nc.sbuf_tensor("wad%d" % i, [128, KC, 512], F32))
                    w.b = Buf("wad%d" % i)
                w_ada_v = w_ada.rearrange("(kc p) c -> p kc c", p=128)
                modrow = T.__new__(T)
                modrow.t = es0.enter_context(nc.sbuf_tensor("modrow", [1, 6 * D], F32))
                modrow.b = Buf("modrow")
                for n in range(24):
                    w = wad[n % 2]
                    P.dma(w[:], w_ada_v[:, :, n * 512:(n + 1) * 512], [], [w.b])
                    psr_ = psbank()
                    for kc in range(KC):
                        P.mm(psr_[0:1, :], cact[:, kc:kc + 1], w[:, kc, :], kc == 0, kc == KC - 1, [w.b, cact.b], [psr_.b])
                    P.copy("act" if n % 2 == 0 else "dve", modrow[0:1, n * 512:(n + 1) * 512], psr_[0:1, :], [psr_.b], [modrow.b])
                for m in range(96):
                    P.mm(modps[:, m:m + 1], modrow[0:1, m * 128:(m + 1) * 128], ident_f[0:1, 0:1], True, True, [modrow.b, cstf.b], [modps.b],
                         inc=(m == 95))
                P.dma(bT[:], badaT[:, :], [], [bT.b])
                P.tt("dve", modT[:], modps[:, 0:96], bT[:], ALU.add, [modps.b, bT.b], [modT.b])
                dump("modT", modT, [128, 96])
                P.dma(g1t[:], gnmT[:, :], [], [g1t.b])
                P.dma(g2t[:], gnfT[:, :], [], [g2t.b])
                P.stt(G1s[:], modT[:, 16:32], 1.0, g1t[:], ALU.add, ALU.mult, [modT.b, g1t.b], [G1s.b])
                P.stt(G2s[:], modT[:, 64:80], 1.0, g2t[:], ALU.add, ALU.mult, [modT.b, g2t.b], [G2s.b])
                SH1s = modT[:, 0:16]
                SH2s = modT[:, 48:64]
                modD = Buf("mod_d")
                pmt = psbank()
                P.tr(pmt[0:96, 0:128], modT[:], ident_f, [modT.b, cstf.b], [pmt.b])
                P.copy("dve", modTT[:], pmt[0:96, 0:128], [pmt.b], [modTT.b])
                P.dma(mod_d.rearrange("(m p) -> m p", p=128), modTT[:], [modTT.b], [modD])

                winD = Buf("win_bf")
                wst = []
                wsb = []
                for i in range(2):
                    a = T.__new__(T); a.t = es0.enter_context(nc.sbuf_tensor("wst%d" % i, [128, IN_COLS], F32)); a.b = Buf("wst%d" % i)
                    c_ = T.__new__(T); c_.t = es0.enter_context(nc.sbuf_tensor("wsb%d" % i, [128, IN_COLS], BF16)); c_.b = Buf("wsb%d" % i)
                    wst.append(a); wsb.append(c_)
                for kc in range(KC):
                    a = wst[kc % 2]; c_ = wsb[kc % 2]
                    P.dma(a[:], w_in[kc * 128:(kc + 1) * 128, :], [], [a.b])
                    h = IN_COLS // 2
                    P.copy("act", c_[:, 0:h], a[:, 0:h], [a.b], [c_.b])
                    P.copy("dve", c_[:, h:], a[:, h:], [a.b], [c_.b])
                    P.dma(win_bf[kc * 128:(kc + 1) * 128, :], c_[:], [c_.b], [winD], semb=winD)

            stop("stop0")
            P.set_barrier()
            with contextlib.ExitStack() as es1:
                def sb1(name, shape, dt=F32):
                    t = T.__new__(T)
                    t.t = es1.enter_context(nc.sbuf_tensor(name, shape, dt))
                    t.b = P.mkbuf(name)
                    return t

                LB = sb1("LB", [128, 1024]); OMLB = sb1("OMLB", [128, 1024]); GS = sb1("GS", [128, 1024]); l1 = GS
                P.dma(LB[:], lbl[0:1, :].partition_broadcast(128), [], [LB.b])
                P.dma(l1[:], lbl[1:2, :].partition_broadcast(128), [], [l1.b])
                P.tt("dve", LB[:], LB[:], l1[:], ALU.subtract, [LB.b, l1.b], [LB.b])
                P.act(LB[:], LB[:], AF.Sigmoid, [LB.b], [LB.b])
                P.ts("dve", OMLB[:], LB[:], -1.0, 1.0, ALU.mult, ALU.add, [LB.b], [OMLB.b])
                GR = sb1("GR", [128, 1024])
                P.dma(GR[:], g_rec[0:1, :].partition_broadcast(128), [], [GR.b])
                vT = sb1("vT", [128, NB])
                P.dma(vT[:], validT[:, :], [], [vT.b])

                xt = [sb1("xt%d" % i, [128, D]) for i in range(2)]
                junk = sb1("junk", [128, 128], BF16)
                xn = [sb1("xn%d" % i, [128, D], BF16) for i in range(2)]
                st = sb1("st", [128, 8])
                hT = sb1("hT", [128, KC, 512], BF16)
                wt = [sb1("wt%d" % i, [128, KC, 512], BF16) for i in range(2)]
                wrot = [0]
                sgf = sb1("sgf", [128, 4, 1024])
                lgf = sb1("lgf", [128, 4, 1024])
                vbf = sb1("vbf", [128, 4, 1024], BF16)
                kvt = sb1("kvt", [128, 4, 512], BF16)
                ikt = sb1("ikt", [128, 4, 128], BF16)
                qs = sb1("qs", [128, 1024]); gs = sb1("gs", [128, 1024])
                aqt = sb1("aqt", [128, 1024], BF16)
                iqt = sb1("iqt", [128, 512]); iwt = sb1("iwt", [128, 8])
                eR = sb1("eR", [128, 1024]); eB = sb1("eB", [128, 1024])
                ktb = sb1("ktb", [128, 1024], BF16)
                qtb = sb1("qtb", [128, 1024], BF16); qtb1 = sb1("qtb1", [128, 1024], BF16); khb = sb1("khb", [128, 1024], BF16)
                dec = sb1("dec", [128, 16])
                Sst = sb1("Sst", [128, 1024])
                Sbf = [sb1("Sbf%d" % i, [128, 1024], BF16) for i in range(2)]
                qT0 = sb1("qT0", [128, 1024], BF16); qT1 = sb1("qT1", [128, 1024], BF16)
                khT = sb1("khT", [128, 1024], BF16)
                pT = [sb1("pT%d" % i, [128, 128], BF16) for i in range(2)]
                ssq = sb1("ssq", [128, 8]); rsq = sb1("rsq", [128, 8])
                recb = sb1("recb", [128, 1024], BF16)
                trs = sb1("trs", [128, 1536], BF16)
                recT = sb1("recT", [128, 1024], BF16)
                aqT = sb1("aqT", [128, 1024], BF16)
                iqs = sb1("iqs", [128, 512], BF16)
                iqT = sb1("iqT", [128, 512], BF16)
                aw = sb1("aw", [128, 8]); sgn = sb1("sgn", [128, 8])
                P.emit("pool", lambda: nc.gpsimd.memset(Sst[:], 0.0), [], [Sst.b])
                P.emit("pool", lambda: nc.gpsimd.memset(qtb[:], 0.0), [], [qtb.b])
                P.emit("pool", lambda: nc.gpsimd.memset(qtb1[:], 0.0), [], [qtb1.b])

                KTD = Buf("KT_d"); VD = Buf("V_d"); KID = Buf("kiT_d"); AQD = Buf("aqT_d"); IQD = Buf("iqT_d")
                SGD = Buf("sgn_d"); MIXD = Buf("mixT_d")
                win_v = win_bf.rearrange("(kc p) c -> p kc c", p=128)

                def rms_rstd(ssap, outap, n, scale, R, W):
                    P.ts("dve", outap, ssap, scale, EPS, ALU.mult, ALU.add, R, W)
                    P.act(outap, outap, AF.Sqrt, W, W)
                    P.emit("dve", lambda: nc.vector.reciprocal(out=outap, in_=outap), W, W)

                def proj_tile(c0, ncols, blks, evac):
                    w = wt[wrot[0] % 2]; wrot[0] += 1
                    P.dma(w[:, :, 0:ncols], win_v[:, :, c0:c0 + ncols], [winD], [w.b])
                    for blk in blks:
                        ps = psbank()
                        for kc in range(KC):
                            P.mm(ps[:, 0:ncols], hT[:, kc, blk * 128:(blk + 1) * 128], w[:, kc, 0:ncols],
                                 kc == 0, kc == KC - 1, [hT.b, w.b], [ps.b])
                        evac(ps, blk)

                def secA(sbi):
                    for blk in range(4):
                        p = sbi * 4 + blk
                        x_ = xt[p % 2]; xn_ = xn[p % 2]
                        P.dma(x_[:], x_sh[p * 128:(p + 1) * 128, :], [], [x_.b])
                        P.act(xn_[:], x_[:], AF.Square, [x_.b], [xn_.b, st.b], accum_out=st[:, 0:1])
                        rms_rstd(st[:, 0:1], st[:, 1:2], 1, 1.0 / D, [st.b], [st.b])
                        P.ts("dve", xn_[:], x_[:], st[:, 1:2], None, ALU.mult, None, [x_.b, st.b], [xn_.b])
                        for half in range(2):
                            ps = psbank()
                            pv = bfv(ps)
                            for k8 in range(8):
                                kc = half * 8 + k8
                                P.tr(pv[:, k8 * 128:(k8 + 1) * 128], xn_[:, kc * 128:(kc + 1) * 128], ident, [xn_.b, cstb.b], [ps.b])
                            for k8 in range(8):
                                kc = half * 8 + k8
                                o = hT[:, kc, blk * 128:(blk + 1) * 128]
                                i_ = pv[:, k8 * 128:(k8 + 1) * 128]
                                if k8 % 2 == 0:
                                    P.act(o, i_, AF.Identity, [ps.b, G1s.b, modT.b], [hT.b], scale=G1s[:, kc:kc + 1], bias=SH1s[:, kc:kc + 1])
                                else:
                                    P.ts("dve", o, i_, G1s[:, kc:kc + 1], SH1s[:, kc:kc + 1], ALU.mult, ALU.add, [ps.b, G1s.b, modT.b], [hT.b])
                    if sbi == 0:
                        dump("hT", hT, [128, KC, 512], BF16)
                def secB(sbi):
                    for ti in range(2):
                        proj_tile(C_RF + ti * 512, 512, range(4),
                                  lambda ps, blk, ti=ti: P.act(sgf[:, blk, ti * 512:(ti + 1) * 512], ps[:, :], AF.Sigmoid, [ps.b], [sgf.b]))
                    for blk in range(4):
                        P.tt("dve", sgf[:, blk, :], sgf[:, blk, :], OMLB[:], ALU.mult, [sgf.b, OMLB.b], [sgf.b])
                        P.tt("dve", sgf[:, blk, :], sgf[:, blk, :], LB[:], ALU.add, [sgf.b, LB.b], [sgf.b])
                        P.act(lgf[:, blk, :], sgf[:, blk, :], AF.Ln, [sgf.b], [lgf.b])
                        P.ts("pool", sgf[:, blk, :], sgf[:, blk, :], -1.0, 1.0, ALU.mult, ALU.add, [sgf.b, lgf.b], [sgf.b])
                    for ti in range(2):
                        proj_tile(C_RI + ti * 512, 512, range(4),
                                  lambda ps, blk, ti=ti: P.copy("act", vbf[:, blk, ti * 512:(ti + 1) * 512], ps[:, :], [ps.b], [vbf.b]))
                    proj_tile(C_AK, 512, range(4), lambda ps, blk: P.copy("dve", kvt[:, blk, :], ps[:, :], [ps.b], [kvt.b]))

                    def ev_ik(ps, blk):
                        P.copy("act", ikt[:, blk, 0:64], ps[:, 0:64], [ps.b], [ikt.b])
                        P.copy("act", ikt[:, blk, 64:128], ps[:, 0:64], [ps.b], [ikt.b])
                    proj_tile(C_IK, 64, range(4), ev_ik)
                    for ti in range(2):
                        proj_tile(C_RQ + ti * 512, 512, [3],
                                  lambda ps, blk, ti=ti: P.act(qs[:, ti * 512:(ti + 1) * 512], ps[:, :], AF.Silu, [ps.b], [qs.b]))
                    for ti in range(2):
                        proj_tile(C_RG + ti * 512, 512, [3],
                                  lambda ps, blk, ti=ti: P.act(gs[:, ti * 512:(ti + 1) * 512], ps[:, :], AF.Silu, [ps.b], [gs.b]))
                    for ti in range(2):
                        proj_tile(C_AQ + ti * 512, 512, [3],
                                  lambda ps, blk, ti=ti: P.copy("dve", aqt[:, ti * 512:(ti + 1) * 512], ps[:, :], [ps.b], [aqt.b]))
                    proj_tile(C_IQ, 512, [3], lambda ps, blk: P.copy("dve", iqt[:], ps[:, :], [ps.b], [iqt.b]))
                    proj_tile(C_IW, 8, [3], lambda ps, blk: P.copy("dve", iwt[:], ps[:, 0:8], [ps.b], [iwt.b]))

                def secD(sbi):
                    for blk in range(4):
                        p = sbi * 4 + blk
                        own = (blk == 3)
                        for ti in range(2):
                            ps = psbank()
                            P.mm(ps[:, :], TU, lgf[:, blk, ti * 512:(ti + 1) * 512], True, True, [cstf.b, lgf.b], [ps.b])
                            P.act(eR[:, ti * 512:(ti + 1) * 512], ps[:, :], AF.Exp, [ps.b], [eR.b])
                        P.stt(ktb[:], sgf[:, blk, :], vT[:, p:p + 1], eR[:], ALU.mult, ALU.mult, [sgf.b, vT.b, eR.b], [ktb.b])
                        psd = psbank()
                        for hd in range(8):
                            P.mm(psd[:, 2 * hd:2 * hd + 2], lgf[:, blk, hd * 128:(hd + 1) * 128], CI, True, True, [lgf.b, cstf.b], [psd.b])
                        P.act(dec[:], psd[:, 0:16], AF.Exp, [psd.b], [dec.b])
                        stop("stopD1")
                        if own:
                            for ti in range(2):
                                ps = psbank()
                                P.mm(ps[:, :], TL, lgf[:, blk, ti * 512:(ti + 1) * 512], True, True, [cstf.b, lgf.b], [ps.b])
                                P.act(eB[:, ti * 512:(ti + 1) * 512], ps[:, :], AF.Exp, [ps.b], [eB.b])
                                P.act(eR[:, ti * 512:(ti + 1) * 512], ps[:, :], AF.Exp, [ps.b, ktb.b], [eR.b], scale=-1.0)
                            P.stt(qtb[0:64, :], qs[0:64, :], 128.0 ** -0.5, eB[0:64, :], ALU.mult, ALU.mult, [qs.b, eB.b], [qtb.b])
                            P.stt(qtb1[64:128, :], qs[64:128, :], 128.0 ** -0.5, eB[64:128, :], ALU.mult, ALU.mult, [qs.b, eB.b], [qtb1.b])
                            P.tt("dve", khb[:], sgf[:, blk, :], eR[:], ALU.mult, [sgf.b, eR.b], [khb.b])
                            stop("stopD3a")
                            psq = psbank(); psk = psbank()
                            pq = bfv(psq); pk = bfv(psk)
                            for hd in range(8):
                                P.tr(pq[:, hd * 128:(hd + 1) * 128], qtb[:, hd * 128:(hd + 1) * 128], ident, [qtb.b, cstb.b], [psq.b])
                            for hd in range(8):
                                P.tr(pk[:, hd * 128:(hd + 1) * 128], khb[:, hd * 128:(hd + 1) * 128], ident, [khb.b, cstb.b], [psk.b])
                            psq1 = psbank(); pq1 = bfv(psq1)
                            for hd in range(8):
                                P.tr(pq1[:, hd * 128:(hd + 1) * 128], qtb1[:, hd * 128:(hd + 1) * 128], ident, [qtb1.b, cstb.b], [psq1.b])
                            stop("stopD3b")
                            P.copy("act", qT0[:], pq[:, :], [psq.b], [qT0.b])
                            P.copy("dve", qT1[:], pq1[:, :], [psq1.b], [qT1.b])
                            P.copy("act", khT[:], pk[:, :], [psk.b], [khT.b])
                            stop("stopD3")
                        for c in range(2):
                            if own:
                                P.copy("act" if c == 0 else "pool", Sbf[c][:], Sst[:], [Sst.b], [Sbf[c].b])
                            for hh in range(2):
                                ps = psbank()
                                for h4 in range(4):
                                    hd = hh * 4 + h4
                                    P.mm(ps[:, h4 * 128:(h4 + 1) * 128], ktb[c * 64:(c + 1) * 64, hd * 128:(hd + 1) * 128],
                                         vbf[c * 64:(c + 1) * 64, blk, hd * 128:(hd + 1) * 128], True, True, [ktb.b, vbf.b], [ps.b])
                                for h4 in range(4):
                                    hd = hh * 4 + h4
                                    P.stt(Sst[:, hd * 128:(hd + 1) * 128], Sst[:, hd * 128:(hd + 1) * 128], dec[:, 2 * hd + c:2 * hd + c + 1], ps[:, h4 * 128:(h4 + 1) * 128],
                                          ALU.mult, ALU.add, [Sst.b, dec.b, ps.b], [Sst.b])
                        stop("stopD2")
                        if own:
                            ob = sbi
                            pso = [PS[6], PS[7]]
                            for hd in range(8):
                                pss = psbank()
                                P.mm(pss[:, 0:128], khT[:, hd * 128:(hd + 1) * 128], qT0[:, hd * 128:(hd + 1) * 128], True, False, [khT.b, qT0.b], [pss.b])
                                P.mm(pss[:, 0:128], khT[:, hd * 128:(hd + 1) * 128], qT1[:, hd * 128:(hd + 1) * 128], False, True, [khT.b, qT1.b], [pss.b])
                                pt = pT[hd % 2]
                                P.tt("dve", pt[:], pss[:, 0:128], MBD, ALU.mult, [pss.b, cstf.b], [pt.b])
                                po = pso[hd // 4]
                                oo = po[:, (hd % 4) * 128:(hd % 4 + 1) * 128]
                                P.mm(oo, pt[:], vbf[:, blk, hd * 128:(hd + 1) * 128], True, False, [pt.b, vbf.b], [po.b])
                                P.mm(oo, qT0[:, hd * 128:(hd + 1) * 128], Sbf[0][:, hd * 128:(hd + 1) * 128], False, False, [qT0.b, Sbf[0].b], [po.b])
                                P.mm(oo, qT1[:, hd * 128:(hd + 1) * 128], Sbf[1][:, hd * 128:(hd + 1) * 128], False, True, [qT1.b, Sbf[1].b], [po.b])
                            stop("stopD3c")
                            for hd in range(8):
                                po = pso[hd // 4]
                                P.act(junk[:, 0:128], po[:, (hd % 4) * 128:(hd % 4 + 1) * 128], AF.Square, [po.b], [junk.b, ssq.b],
                                      accum_out=ssq[:, hd:hd + 1])
                            stop("stopD3d")
                            rms_rstd(ssq[:], rsq[:], 8, 1.0 / 128, [ssq.b], [rsq.b])
                            P.tt("dve", GS[:], gs[:], GR[:], ALU.mult, [gs.b, GR.b], [GS.b])
                            for hd in range(8):
                                po = pso[hd // 4]
                                P.stt(recb[:, hd * 128:(hd + 1) * 128], po[:, (hd % 4) * 128:(hd % 4 + 1) * 128], rsq[:, hd:hd + 1],
                                      GS[:, hd * 128:(hd + 1) * 128], ALU.mult, ALU.mult, [po.b, rsq.b, GS.b], [recb.b])
                            if sbi == 0:
                                dump("rec0", recb, [128, 1024], BF16)
                            stop("stopD4")
                            psr = psbank(); pr = bfv(psr)
                            for hd in range(8):
                                P.tr(pr[:, hd * 128:(hd + 1) * 128], recb[:, hd * 128:(hd + 1) * 128], ident, [recb.b, cstb.b], [psr.b])
                            P.copy("act", recT[:], pr[:, :], [psr.b], [recT.b])
                            P.dma(mixT_d[0:1024, ob * 128:(ob + 1) * 128].rearrange("(h p) t -> p h t", p=128), recT[:].rearrange("p (h t) -> p h t", h=8), [recT.b], [MIXD], semb=MIXD)
                            psa = psbank(); pa = bfv(psa)
                            for hd in range(8):
                                P.tr(pa[:, hd * 128:(hd + 1) * 128], aqt[:, hd * 128:(hd + 1) * 128], ident, [aqt.b, cstb.b], [psa.b])
                            P.copy("act", aqT[:], pa[:, :], [psa.b], [aqT.b])
                            P.dma(aqT_d[:, :, ob * 128:(ob + 1) * 128].rearrange("h p t -> p h t"), aqT[:].rearrange("p (h t) -> p h t", h=8), [aqT.b], [AQD], semb=AQD)
                            P.act(aw[:], iwt[:], AF.Abs, [iwt.b], [aw.b], scale=64.0 ** -0.5 * 8.0 ** -0.5)
                            P.ts("dve", sgn[:], iwt[:], 0.0, 2.0, ALU.is_ge, ALU.mult, [iwt.b], [sgn.b])
                            P.ts("dve", sgn[:], sgn[:], -1.0, None, ALU.add, None, [sgn.b], [sgn.b])
                            for h in range(8):
                                P.ts("pool", iqs[:, h * 64:(h + 1) * 64], iqt[:, h * 64:(h + 1) * 64], aw[:, h:h + 1], None, ALU.mult, None,
                                     [iqt.b, aw.b], [iqs.b])
                            psi = psbank(); pi = bfv(psi)
                            for c4 in range(4):
                                P.tr(pi[:, c4 * 128:(c4 + 1) * 128], iqs[:, c4 * 128:(c4 + 1) * 128], ident, [iqs.b, cstb.b], [psi.b])
                            P.copy("act", iqT[:], pi[:, 0:512], [psi.b], [iqT.b])
                            P.dma(iqT_d[:, :, ob * 128:(ob + 1) * 128].rearrange("h p t -> p h t"), iqT[:].rearrange("p (h t) -> p h t", h=4), [iqT.b], [IQD], semb=IQD)
                            P.dma(sgn_d[ob * 128:(ob + 1) * 128, :], sgn[:], [sgn.b], [SGD], semb=SGD)
                def secE(sbi):
                    pst = [psbank(), psbank()]
                    for blk in range(4):
                        for w3 in range(3):
                            idx = blk * 3 + w3
                            pv = bfv(pst[idx // 8])
                            src = kvt[:, blk, w3 * 128:(w3 + 1) * 128] if w3 < 2 else ikt[:, blk, :]
                            P.tr(pv[:, (idx % 8) * 128:(idx % 8 + 1) * 128], src, ident, [kvt.b, ikt.b, cstb.b], [pst[idx // 8].b])
                    P.copy("act", trs[:, 0:1024], bfv(pst[0])[:, :], [pst[0].b], [trs.b])
                    P.copy("dve", trs[:, 1024:1536], bfv(pst[1])[:, 0:512], [pst[1].b], [trs.b])
                    trv = trs[:].rearrange("p (b w t) -> p b w t", w=3, t=128)
                    t0 = sbi * 512
                    for kv in range(2):
                        P.dma(KT_d[kv, :, t0:t0 + 512].rearrange("p (b t) -> p b t", b=4), trv[:, :, kv, :], [trs.b], [KTD], semb=KTD)
                    P.dma(kiT_d[:, t0:t0 + 512].rearrange("p (b t) -> p b t", b=4), trv[:, :, 2, :], [trs.b], [KID], semb=KID)
                    P.dma(V_d[t0:t0 + 512, :].rearrange("(b p) c -> p b c", p=128), kvt[:, :, 256:512], [kvt.b], [VD], semb=VD)
                secA(0)
                for sbi in range(NSB):
                    secB(sbi)
                    if sbi + 1 < NSB:
                        secA(sbi + 1)
                    secD(sbi)
                    secE(sbi)
                dump("Sst", Sst, [128, 1024])

            stop("stop1")

            psmod[0] = 3
            NIT = 22
            P.set_barrier()
            with contextlib.ExitStack() as es2:
                def sb2(name, shape, dt=F32):
                    t = T.__new__(T)
                    t.t = es2.enter_context(nc.sbuf_tensor(name, shape, dt))
                    t.b = P.mkbuf(name)
                    return t
                KT = sb2("KT", [128, 2 * S], BF16)
                kiT = sb2("kiT", [128, S], BF16)
                Vaug = sb2("Vaug", [128, NB * 2 * 132], BF16)
                scoreL = [sb2("score%d" % k, [128, S - 512 if (k == 0 and NO % 2 == 0) else S]) for k in range(2)]
                nbiasL = [sb2("nbias%d" % k, [128, S], BF16) for k in range(2)]
                rbuf = [sb2("rbuf%d" % i, [128, 512], BF16) for i in range(2)]
                pbuf = [sb2("pbuf%d" % i, [128, 512], BF16) for i in range(2)]
                aqTiL = [sb2("aqTi%d" % k, [128, 1024], BF16) for k in range(2)]
                iqTi = sb2("iqTi", [128, 512], BF16)
                sgni = sb2("sgni", [128, 8])
                Dh = sb2("Dh", [128, 1024], BF16)
                CBf = sb2("CBf", [128, 512], BF16)
                PBt = sb2("PBt", [128, 512], BF16)
                BIGI4 = sb2("BIGI4", [128, 512], BF16)
                accS = sb2("accS", [128, 1056])
                nv = sb2("nv", [128, NO])
                smL = [sb2("sm%d" % k, [128, 16]) for k in range(2)]
                cjL = [sb2("cj%d" % k, [128, 8], BF16) for k in range(2)]
                sm2 = sb2("sm2", [128, 8]); sm3 = sb2("sm3", [128, 8]); sm4 = sb2("sm4", [128, 8])
                junk2 = sb2("junk2", [128, 128], BF16)
                attbL = [sb2("attb%d" % k, [128, 1024], BF16) for k in range(2)]
                attT = sb2("attT", [128, 1024], BF16)
                P.dma(KT[:, 0:S], KT_d[0], [KTD], [KT.b])
                P.dma(KT[:, S:2 * S], KT_d[1], [KTD], [KT.b])
                P.dma(kiT[:], kiT_d[:, :], [KID], [kiT.b])
                P.emit("pool", lambda: nc.gpsimd.memset(Vaug[:], 1.0), [], [Vaug.b])
                Vv = Vaug[:].rearrange("p (b k c) -> p b k c", k=2, c=132)
                V_dv = V_d.rearrange("(b p) c -> p b c", p=128)
                for b0 in range(0, NB, 16):
                    b1 = min(NB, b0 + 16)
                    for kv in range(2):
                        P.dma(Vv[:, b0:b1, kv, 0:128], V_dv[:, b0:b1, kv * 128:(kv + 1) * 128], [VD], [Vaug.b])
                P.emit("pool", lambda: nc.gpsimd.memset(CBf[:], 0.0), [], [CBf.b])
                P.copy("pool", CBf[:, 384:512], CB, [cstf.b], [CBf.b])
                P.dma(scoreL[0][:, 0:512], padb[0:1, :].partition_broadcast(128), [], [scoreL[0].b])
                P.copy("pool", PBt[:], scoreL[0][:, 0:512], [scoreL[0].b], [PBt.b])
                for r4 in range(4):
                    P.ts("dve", BIGI4[:, r4 * 128:(r4 + 1) * 128], ident_f, 29952.0, None, ALU.mult, None, [cstf.b], [BIGI4.b])
                P.dma(nv[:], nvalT[:, :], [], [nv.b])
                acc = [PS[3], PS[4], PS[5]]
                def stageA(i):
                    score = scoreL[i % 2]; nbias = nbiasL[i % 2]; cj = cjL[i % 2]; aqTi = aqTiL[i % 2]; sm = smL[i % 2]
                    LO, HI, TH, CNT, GE, DD, NGE, SEL, AA, BB, THF = [sm[:, k:k + 1] for k in range(11)]
                    SMB = [sm.b]
                    nkt = i + 1
                    n = nkt * 512
                    nk = 4 * (i + 1)
                    P.dma(aqTi[:].rearrange("p (h t) -> p h t", h=8), aqT_d[:, :, i * 128:(i + 1) * 128].rearrange("h p t -> p h t"), [AQD], [aqTi.b])
                    P.dma(iqTi[:].rearrange("p (h t) -> p h t", h=4), iqT_d[:, :, i * 128:(i + 1) * 128].rearrange("h p t -> p h t"), [IQD], [iqTi.b])
                    P.dma(sgni[:], sgn_d[i * 128:(i + 1) * 128, :], [SGD], [sgni.b])
                    for h in range(8):
                        P.ts("dve", Dh[:, h * 128:(h + 1) * 128], ident_f, sgni[:, h:h + 1], None, ALU.mult, None, [cstf.b, sgni.b], [Dh.b])
                    for kt in range(nkt):
                        psc = PS[6 + kt % 2]
                        for h in range(8):
                            ps = psbank()
                            pb = (h % 2) * 64
                            P.mm(ps[:, :], iqTi[pb:pb + 64, (h // 2) * 128:(h // 2 + 1) * 128], kiT[pb:pb + 64, kt * 512:(kt + 1) * 512],
                                 True, True, [iqTi.b, kiT.b], [ps.b])
                            rb = rbuf[h % 2]
                            P.act(rb[:], ps[:, :], AF.Relu, [ps.b], [rb.b])
                            P.mm(psc[:, :], Dh[:, h * 128:(h + 1) * 128], rb[:], h == 0, h == 7, [Dh.b, rb.b], [psc.b])
                        dst = score[:, kt * 512:(kt + 1) * 512]
                        if kt == nkt - 1:
                            P.tt("dve", dst, psc[:, :], CBf[:], ALU.add, [psc.b, CBf.b], [score.b])
                            if kt == 0:
                                P.tt("dve", dst, dst, PBt[:], ALU.add, [score.b, PBt.b], [score.b])
                        elif kt == 0:
                            P.tt("dve", dst, psc[:, :], PBt[:], ALU.add, [psc.b, PBt.b], [score.b])
                        else:
                            P.copy("act", dst, psc[:, :], [psc.b], [score.b])
                    sc = score[:, 0:n]
                    P.emit("dve", lambda sc=sc: nc.vector.reduce_max(out=HI, in_=sc, axis=AX.X), [score.b], SMB)
                    P.ts("dve", LO, HI, -64.0, None, ALU.add, None, SMB, SMB)
                    for it in range(NIT):
                        cw = 64.0 / (2.0 ** (it + 1))
                        P.ts("dve", TH, LO, cw, None, ALU.add, None, SMB, SMB)
                        P.ts("dve", cj[:, 0:1].to_broadcast([128, n]), sc, TH, 0.0, ALU.is_ge, ALU.add, [score.b] + SMB, [cj.b] + SMB, accum_out=CNT)
                        P.ts("dve", GE, CNT, float(KSEL), cw, ALU.is_ge, ALU.mult, SMB, SMB)
                        P.tt("dve", LO, LO, GE, ALU.add, SMB, SMB)
                    P.ts("dve", SEL, nv[:, i:i + 1], float(KSEL), None, ALU.is_gt, None, [nv.b], SMB)
                    P.ts("dve", AA, SEL, 1e20, -1e20, ALU.mult, ALU.add, SMB, SMB)
                    P.tt("dve", BB, LO, SEL, ALU.mult, SMB, SMB)
                    P.tt("dve", THF, AA, BB, ALU.add, SMB, SMB)
                    P.ts("dve", nbias[:, 0:n], sc, THF, 1.0, ALU.is_ge, ALU.subtract, [score.b] + SMB, [nbias.b])
                def stageB(i):
                    nbias = nbiasL[i % 2]; aqTi = aqTiL[i % 2]; attb = attbL[i % 2]
                    nk = 4 * (i + 1)
                    its = [(kb, kvh) for kb in range(nk) for kvh in range(2)]

                    def Lmm(k):
                        kb, kvh = its[k]
                        psl = psbank()
                        P.mm(psl[:, :], KT[:, kvh * S + kb * 128:kvh * S + (kb + 1) * 128], aqTi[:, kvh * 512:(kvh + 1) * 512],
                             True, False, [KT.b, aqTi.b], [psl.b])
                        P.mm(psl[:, :], nbias[:, kb * 128:(kb + 1) * 128], BIGI4[:], False, True, [nbias.b, BIGI4.b], [psl.b])
                        return psl
                    psl_next = Lmm(0)
                    for k, (kb, kvh) in enumerate(its):
                        psl = psl_next
                        if k + 1 < len(its):
                            psl_next = Lmm(k + 1)
                        pb_ = pbuf[k % 2]
                        P.act(pb_[:], psl[:, :], AF.Exp, [psl.b], [pb_.b], scale=128.0 ** -0.5)
                        vo = (kb * 2 + kvh) * 132
                        for g in range(4):
                            hd = kvh * 4 + g
                            a = acc[hd // 3]
                            off = (hd % 3) * 132
                            P.mm(a[:, off:off + 129], pb_[:, g * 128:(g + 1) * 128], Vaug[:, vo:vo + 129], (kb == 0 and hd % 3 == 0), False,
                                 [pb_.b, Vaug.b], [a.b], inc=(g == 3), skip_group_check=True)
                    for b3 in range(3):
                        wcp = 396 if b3 < 2 else 264
                        P.copy("act", accS[:, b3 * 396:b3 * 396 + wcp], acc[b3][:, 0:wcp], [acc[b3].b], [accS.b])
                    for hd in range(8):
                        off = (hd // 3) * 396 + (hd % 3) * 132
                        P.emit("dve", lambda off=off, hd=hd: nc.vector.reciprocal(out=sm2[:, hd:hd + 1], in_=accS[:, off + 128:off + 129]), [accS.b], [sm2.b])
                        P.act(junk2[:], accS[:, off:off + 128], AF.Square, [accS.b, sm2.b], [junk2.b, sm3.b], scale=sm2[:, hd:hd + 1],
                              accum_out=sm3[:, hd:hd + 1])
                    rms_rstd(sm3[:], sm4[:], 8, 1.0 / 128, [sm3.b], [sm4.b])
                    P.tt("dve", sm4[:], sm4[:], sm2[:], ALU.mult, [sm4.b, sm2.b], [sm4.b])
                    for hd in range(8):
                        off = (hd // 3) * 396 + (hd % 3) * 132
                        P.ts("dve", attb[:, hd * 128:(hd + 1) * 128], accS[:, off:off + 128], sm4[:, hd:hd + 1], None, ALU.mult, None,
                             [accS.b, sm4.b], [attb.b])
                    if i == 0:
                        dump("att0", attb, [128, 1024], BF16)
                    if i == 1:
                        dump("att1", attb, [128, 1024], BF16)
                def stageT(i):
                    attb = attbL[i % 2]
                    psr = psbank(); pr = bfv(psr)
                    for hd in range(8):
                        P.tr(pr[:, hd * 128:(hd + 1) * 128], attb[:, hd * 128:(hd + 1) * 128], ident, [attb.b, cstb.b], [psr.b])
                    P.copy("act", attT[:], pr[:, :], [psr.b], [attT.b])
                    P.dma(mixT_d[1024:2048, i * 128:(i + 1) * 128].rearrange("(h p) t -> p h t", p=128), attT[:].rearrange("p (h t) -> p h t", h=8),
                          [attT.b], [MIXD], semb=MIXD)
                stageA(0)
                for i in range(NO):
                    if i + 1 < NO:
                        stageA(i + 1)
                    stageB(i)
                    if i >= 1:
                        stageT(i - 1)
                stageT(NO - 1)
            psmod[0] = 6
            stop("stop2")

            h2T_d = dscr("h2T_d", [D, TO], BF16)
            H2D = Buf("h2T_d"); X1D = Buf("x1_d"); OUTD = Buf("out")
            P.set_barrier()
            with contextlib.ExitStack() as es34:
                def sb34(name, shape, dt=F32, st=es34):
                    t = T.__new__(T)
                    t.t = st.enter_context(nc.sbuf_tensor(name, shape, dt))
                    t.b = P.mkbuf(name)
                    return t
                comb = sb34("comb", [128, NO * 32])
                with contextlib.ExitStack() as es3:
                    sb3 = lambda name, shape, dt=F32: sb34(name, shape, dt, es3)
                    Wo = sb3("Wo", [128, KC * 2048], BF16)
                    wst3 = [sb3("wst3_%d" % i, [128, 2048]) for i in range(2)]
                    gaTt = sb3("gaTt", [128, 8])
                    P.dma(gaTt[:], gaT_in[:, :], [], [gaTt.b])
                    for kc in range(KC):
                        w = wst3[kc % 2]
                        P.dma(w[:], w_out[kc * 128:(kc + 1) * 128, :], [], [w.b])
                        if kc < 8:
                            P.copy("act" if kc % 2 == 0 else "dve", Wo[:, kc * 2048:(kc + 1) * 2048], w[:], [w.b], [Wo.b])
                        else:
                            P.act(Wo[:, kc * 2048:(kc + 1) * 2048], w[:], AF.Identity, [w.b, gaTt.b], [Wo.b], scale=gaTt[:, kc - 8:kc - 7])
                    GT1b = sb3("GT1b", [128, 2048])
                    mod6 = mod_d.rearrange("(a n) -> a n", a=6)
                    P.dma(GT1b[:], mod6[2:3, :].partition_broadcast(128), [modD], [GT1b.b])
                    Wrt = sb3("Wrt", [128, KC * 36]); Wrtb = sb3("Wrtb", [128, KC * 36], BF16)
                    P.dma(Wrt[:].rearrange("p (k c) -> p k c", c=36), w_rt.rearrange("(k p) c -> p k c", p=128), [], [Wrt.b])
                    P.copy("dve", Wrtb[:], Wrt[:], [Wrt.b], [Wrtb.b])
                    brt = sb3("brt", [128, 36])
                    P.dma(brt[:], b_rt[0:1, :].partition_broadcast(128), [], [brt.b])
                    xo = [sb3("xo%d" % i, [128, D]) for i in range(2)]
                    x1t = [sb3("x1t%d" % i, [128, D]) for i in range(2)]
                    xn2 = sb3("xn2", [128, D], BF16)
                    mixTi = sb3("mixTi", [128, KC * 128], BF16)
                    h2Tb = sb3("h2Tb", [128, KC * 128], BF16)
                    st3 = sb3("st3", [128, 8])
                    lg = sb3("lg", [128, 36])
                    rs = sb3("rs", [128, 64])
                    for i in range(NO):
                        x_ = xo[i % 2]; x1 = x1t[i % 2]
                        p = 4 * i + 3
                        P.dma(x_[:], x_sh[p * 128:(p + 1) * 128, :], [], [x_.b])
                        P.dma(mixTi[:].rearrange("p (k t) -> p k t", t=128), mixT_d[:, i * 128:(i + 1) * 128].rearrange("(k p) t -> p k t", p=128),
                              [MIXD], [mixTi.b])
                        for dt in range(4):
                            ps = psbank()
                            for kc in range(KC):
                                P.mm(ps[:, :], mixTi[:, kc * 128:(kc + 1) * 128], Wo[:, kc * 2048 + dt * 512:kc * 2048 + (dt + 1) * 512],
                                     kc == 0, kc == KC - 1, [mixTi.b, Wo.b], [ps.b])
                            P.tt("dve", x1[:, dt * 512:(dt + 1) * 512], ps[:, :], GT1b[:, dt * 512:(dt + 1) * 512], ALU.mult, [ps.b, GT1b.b], [x1.b])
                        P.tt("dve", x1[:], x1[:], x_[:], ALU.add, [x1.b, x_.b], [x1.b])
                        if i == 0:
                            dump("x1", x1, [128, D])
                        P.dma(x1_d[i * 128:(i + 1) * 128, :], x1[:], [x1.b], [X1D], semb=X1D)
                        P.act(xn2[:], x1[:], AF.Square, [x1.b], [xn2.b, st3.b], accum_out=st3[:, 0:1])
                        rms_rstd(st3[:, 0:1], st3[:, 1:2], 1, 1.0 / D, [st3.b], [st3.b])
                        P.ts("dve", xn2[:], x1[:], st3[:, 1:2], None, ALU.mult, None, [x1.b, st3.b], [xn2.b])
                        for half in range(2):
                            ps = psbank()
                            pv = bfv(ps)
                            for k8 in range(8):
                                kc = half * 8 + k8
                                P.tr(pv[:, k8 * 128:(k8 + 1) * 128], xn2[:, kc * 128:(kc + 1) * 128], ident, [xn2.b, cstb.b], [ps.b])
                            for k8 in range(8):
                                kc = half * 8 + k8
                                o = h2Tb[:, kc * 128:(kc + 1) * 128]
                                i_ = pv[:, k8 * 128:(k8 + 1) * 128]
                                if k8 % 2 == 0:
                                    P.act(o, i_, AF.Identity, [ps.b, G2s.b, modT.b], [h2Tb.b], scale=G2s[:, kc:kc + 1], bias=SH2s[:, kc:kc + 1])
                                else:
                                    P.ts("dve", o, i_, G2s[:, kc:kc + 1], SH2s[:, kc:kc + 1], ALU.mult, ALU.add, [ps.b, G2s.b, modT.b], [h2Tb.b])
                        P.dma(h2T_d[:, i * 128:(i + 1) * 128].rearrange("(k p) t -> p k t", p=128), h2Tb[:].rearrange("p (k t) -> p k t", t=128),
                              [h2Tb.b], [H2D], semb=H2D)
                        psr = psbank()
                        for kc in range(KC):
                            P.mm(psr[:, 0:36], h2Tb[:, kc * 128:(kc + 1) * 128], Wrtb[:, kc * 36:(kc + 1) * 36], kc == 0, kc == KC - 1,
                                 [h2Tb.b, Wrtb.b], [psr.b])
                        P.tt("dve", lg[:], psr[:, 0:36], brt[:], ALU.add, [psr.b, brt.b], [lg.b])
                        RB = [rs.b]
                        GMAX, NGM, SE, PG, M1, M2, DLT, EX, DEN, W1, W2 = [rs[:, k:k + 1] for k in range(11)]
                        OHG = rs[:, 12:16]; ESEL = rs[:, 16:24]; MK1 = rs[:, 24:32]; E2 = rs[:, 32:40]; MK2 = rs[:, 40:48]; CIG = rs[:, 48:56]; EG = rs[:, 56:60]
                        P.emit("dve", lambda: nc.vector.reduce_max(out=GMAX, in_=lg[:, 0:4], axis=AX.X), [lg.b], RB)
                        P.ts("dve", NGM, GMAX, -1.0, None, ALU.mult, None, RB, RB)
                        P.act(EG, lg[:, 0:4], AF.Exp, [lg.b] + RB, RB, bias=NGM, accum_out=SE)
                        P.emit("dve", lambda: nc.vector.reciprocal(out=PG, in_=SE), RB, RB)
                        P.ts("dve", OHG, lg[:, 0:4], GMAX, None, ALU.is_ge, None, [lg.b] + RB, RB)
                        P.ts("dve", ESEL, lg[:, 4:12], rs[:, 12:13], None, ALU.mult, None, [lg.b] + RB, RB)
                        for g in range(1, 4):
                            P.stt(ESEL, lg[:, 4 + 8 * g:12 + 8 * g], rs[:, 12 + g:13 + g], ESEL, ALU.mult, ALU.add, [lg.b] + RB, RB)
                        P.emit("dve", lambda: nc.vector.reduce_max(out=M1, in_=ESEL, axis=AX.X), RB, RB)
                        P.ts("dve", MK1, ESEL, M1, None, ALU.is_ge, None, RB, RB)
                        P.stt(E2, MK1, -1e30, ESEL, ALU.mult, ALU.add, RB, RB)
                        P.emit("dve", lambda: nc.vector.reduce_max(out=M2, in_=E2, axis=AX.X), RB, RB)
                        P.ts("dve", MK2, E2, M2, None, ALU.is_ge, None, RB, RB)
                        P.tt("dve", DLT, M2, M1, ALU.subtract, RB, RB)
                        P.act(EX, DLT, AF.Exp, RB, RB)
                        P.ts("dve", DEN, EX, 1.0, None, ALU.add, None, RB, RB)
                        P.emit("dve", lambda: nc.vector.reciprocal(out=DEN, in_=DEN), RB, RB)
                        P.tt("dve", W1, DEN, PG, ALU.mult, RB, RB)
                        P.tt("dve", W2, W1, EX, ALU.mult, RB, RB)
                        P.ts("dve", CIG, MK1, W1, None, ALU.mult, None, RB, RB)
                        P.stt(CIG, MK2, W2, CIG, ALU.mult, ALU.add, RB, RB)
                        for g in range(4):
                            P.ts("dve", comb[:, i * 32 + g * 8:i * 32 + (g + 1) * 8], CIG, rs[:, 12 + g:13 + g], None, ALU.mult, None, RB, [comb.b])
                stop("stop3")
                P.set_barrier()
                with contextlib.ExitStack() as es4:
                    sb4 = lambda name, shape, dt=F32: sb34(name, shape, dt, es4)
                    CH = min(TO, 1024)
                    NCH = TO // CH
                    NT = CH // 128
                    SUB = min(512, CH)
                    h2c = sb4("h2c", [128, KC * CH], BF16)
                    yacc = sb4("yacc", [128, NT * 2048])
                    WGb = sb4("WGb", [128, KC * 512], BF16); WUb = sb4("WUb", [128, KC * 512], BF16); WDb = sb4("WDb", [128, 4 * 2048], BF16)
                    stg = [sb4("stg%d" % k, [128, 2048]) for k in range(4)]
                    sgt = [sb4("sgt%d" % k, [128, 512]) for k in range(2)]
                    heT = [sb4("heT%d" % k, [128, 4 * 512], BF16) for k in range(2)]
                    xf = sb4("xf", [128, 2048])
                    st4 = sb4("st4", [128, 8])
                    srot = [0]

                    def load_cast(dst_ap, dstb, src_ap, three=None):
                        w = stg[srot[0] % 4]
                        eng = "act" if srot[0] % 4 != 3 else "dve"
                        srot[0] += 1
                        if three is None:
                            P.dma(w[:], src_ap, [], [w.b])
                        else:
                            P.dma(w[:].rearrange("p (k c) -> p k c", c=512), src_ap, [], [w.b])
                        P.copy(eng, dst_ap, w[:], [w.b], [dstb])

                    for ch in range(NCH):
                        P.dma(h2c[:].rearrange("p (k t) -> p k t", t=CH), h2T_d[:, ch * CH:(ch + 1) * CH].rearrange("(k p) t -> p k t", p=128),
                              [H2D], [h2c.b])
                        P.emit("pool", lambda: nc.gpsimd.memset(yacc[:], 0.0), [], [yacc.b])
                        for e in range(NE):
                            for q in range(4):
                                load_cast(WGb[:, q * 2048:(q + 1) * 2048], WGb.b, w_eg[e % ne_decl, q * 512:(q + 1) * 512, :].rearrange("(k p) c -> p k c", p=128), 1)
                            for q in range(4):
                                load_cast(WUb[:, q * 2048:(q + 1) * 2048], WUb.b, w_eu[e % ne_decl, q * 512:(q + 1) * 512, :].rearrange("(k p) c -> p k c", p=128), 1)
                            for c in range(4):
                                load_cast(WDb[:, c * 2048:(c + 1) * 2048], WDb.b, w_ed[e % ne_decl, c * 128:(c + 1) * 128, :])
                            for st_ in range(CH // SUB):
                                tok0 = st_ * SUB
                                he = heT[st_ % 2]
                                for c in range(4):
                                    psg = psbank(); psu = psbank()
                                    for kc in range(KC):
                                        P.mm(psg[:, 0:SUB], WGb[:, kc * 512 + c * 128:kc * 512 + (c + 1) * 128], h2c[:, kc * CH + tok0:kc * CH + tok0 + SUB],
                                             kc == 0, kc == KC - 1, [WGb.b, h2c.b], [psg.b])
                                    for kc in range(KC):
                                        P.mm(psu[:, 0:SUB], WUb[:, kc * 512 + c * 128:kc * 512 + (c + 1) * 128], h2c[:, kc * CH + tok0:kc * CH + tok0 + SUB],
                                             kc == 0, kc == KC - 1, [WUb.b, h2c.b], [psu.b])
                                    sg = sgt[c % 2]
                                    P.act(sg[:, 0:SUB], psg[:, 0:SUB], AF.Silu, [psg.b], [sg.b])
                                    P.tt("dve", he[:, c * 512:c * 512 + SUB], sg[:, 0:SUB], psu[:, 0:SUB], ALU.mult, [sg.b, psu.b], [he.b])
                                for t_ in range(SUB // 128):
                                    tile = st_ * (SUB // 128) + t_
                                    gt = ch * NT + tile
                                    for dt in range(4):
                                        psd = psbank()
                                        for c in range(4):
                                            P.mm(psd[:, :], he[:, c * 512 + t_ * 128:c * 512 + (t_ + 1) * 128], WDb[:, c * 2048 + dt * 512:c * 2048 + (dt + 1) * 512],
                                                 c == 0, c == 3, [he.b, WDb.b], [psd.b])
                                        ya = yacc[:, tile * 2048 + dt * 512:tile * 2048 + (dt + 1) * 512]
                                        P.stt(ya, psd[:, :], comb[:, gt * 32 + e:gt * 32 + e + 1], ya, ALU.mult, ALU.add, [psd.b, comb.b, yacc.b], [yacc.b])
                        GT2b = stg[0]; GFb = stg[1]
                        P.dma(GT2b[:], mod6[5:6, :].partition_broadcast(128), [modD], [GT2b.b])
                        P.dma(GFb[:], g_fin[0:1, :].partition_broadcast(128), [], [GFb.b])
                        for tile in range(NT):
                            gt = ch * NT + tile
                            P.dma(xf[:], x1_d[gt * 128:(gt + 1) * 128, :], [X1D], [xf.b])
                            ya = yacc[:, tile * 2048:(tile + 1) * 2048]
                            P.tt("dve", ya, ya, GT2b[:], ALU.mult, [yacc.b, GT2b.b], [yacc.b])
                            P.tt("dve", xf[:], xf[:], ya, ALU.add, [xf.b, yacc.b], [xf.b])
                            P.act(ya, xf[:], AF.Square, [xf.b], [yacc.b, st4.b], accum_out=st4[:, 0:1])
                            rms_rstd(st4[:, 0:1], st4[:, 1:2], 1, 1.0 / D, [st4.b], [st4.b])
                            P.stt(xf[:], xf[:], st4[:, 1:2], GFb[:], ALU.mult, ALU.mult, [xf.b, st4.b, GFb.b], [xf.b])
                            P.dma(out_d[gt * 128:(gt + 1) * 128, :], xf[:], [xf.b], [OUTD], semb=OUTD)


        try:
            body()
        except _Stop:
            pass
        for b_ in P.dmabufs:
            P.final.append((b_.sem, b_.cnt))
        P.replay()
    return nc, dbg_out


def host_consts():
    c = np.zeros((128, 1024), np.float32)
    i = np.arange(128)
    c[:, 0:128] = np.eye(128)
    same = (i[:, None] // 64) == (i[None, :] // 64)
    c[:, 128:256] = (same & (i[:, None] <= i[None, :])).astype(np.float32)
    c[:, 256:384] = (same & (i[:, None] > i[None, :])).astype(np.float32)
    c[:, 384] = (i < 64)
    c[:, 385] = (i >= 64)
    c[:, 512:640] = np.where(i[None, :] <= i[:, None], 0.0, -1e30)
    c[:, 640:768] = np.eye(128)
    return c


def make_in_maps(inputs, S, ne=NE):
    f = lambda a: np.ascontiguousarray(np.asarray(a, dtype=np.float32))
    x = f(inputs["x"]); c = f(inputs["c"])
    NB = S // 128; NO = NB // 4
    pT = lambda v: np.ascontiguousarray(v.reshape(-1, 128).T)
    shared = {
        "w_ada": f(inputs["w_ada"][0]), "badaT": pT(f(inputs["b_ada"][0])),
        "gnmT": pT(f(inputs["g_norm_mix"][0])), "gnfT": pT(f(inputs["g_norm_ffn"][0])),
        "w_in": f(inputs["w_in"][0]), "lbl": f(inputs["lb_logits"]),
        "g_rec": f(inputs["g_rec_out"]), "g_att": f(inputs["g_att_out"]), "gaT": pT(f(inputs["g_att_out"][0])),
        "w_out": f(inputs["w_out"][0]),
        "w_rt": np.ascontiguousarray(np.concatenate([f(inputs["w_router_group"][0]), f(inputs["w_router_expert"][0])], axis=1)),
        "b_rt": np.ascontiguousarray(np.concatenate([f(inputs["b_router_group"][0]), f(inputs["b_router_expert"][0])])[None, :]),
        "w_eg": f(inputs["w_expert_gate"][0][:ne]), "w_eu": f(inputs["w_expert_up"][0][:ne]), "w_ed": f(inputs["w_expert_down"][0][:ne]),
        "g_fin": f(inputs["g_final"])[None, :], "cst": host_consts(),
    }
    maps = []
    for core in range(8):
        b, j = core // 4, core % 4
        npad = (3 - j) * 128
        xs = np.zeros((S, D), np.float32)
        xs[npad:] = x[b, :S - npad]
        valid = np.ones(S, np.float32); valid[:npad] = 0
        padb = np.zeros((1, 512), np.float32); padb[0, :npad] = -1e30
        nval = np.zeros((128, NO), np.float32)
        for i in range(NO):
            nval[:, i] = (4 * i + 3) * 128 + np.arange(128) - npad + 1
        m = dict(shared)
        m.update({"x_sh": xs, "cT": pT(c[b]), "validT": pT(valid), "padb": padb, "nvalT": nval})
        maps.append(m)
    return maps


def assemble(results, S, B=2):
    NB = S // 128; NO = NB // 4
    out = np.zeros((B, S, D), np.float32)
    for core in range(8):
        b, j = core // 4, core % 4
        o = np.asarray(results[core]["out"]).reshape(NO, 128, D)
        for i in range(NO):
            g = 4 * i + j
            out[b, g * 128:(g + 1) * 128] = o[i]
    return out


_CACHE = {}


def kernel(**inputs):
    S = int(np.asarray(inputs["x"]).shape[1])
    if S not in _CACHE:
        _CACHE[S] = build(S)[0]
    nc = _CACHE[S]
    maps = make_in_maps(inputs, S)
    res = run_bass_kernel_spmd(nc, maps, core_ids=list(range(8)))
    return assemble(res.results, S)
```
